# Optimizing a Trainium2 kernel written in Bass

```python
import math
import jax, jax.numpy as jnp
from jax import lax
import numpy as np

D_MODEL = 1024
BATCH = 16
SEQ = 4096
DEPTH = 4

GRID_W = 64
CTX_LEN = 256
EPS = 1e-6
CONV_W = 4
N_BRANCH = 3
S5_WIDTH = D_MODEL // 2
S5_GROUP = 16
S5_GROUPS = S5_WIDTH // S5_GROUP
S5_STATE = 64
M2_HEAD_DIM = 64
M2_INNER = D_MODEL
M2_HEADS = M2_INNER // M2_HEAD_DIM
M2_GROUPS = 4
M2_HPG = M2_HEADS // M2_GROUPS
M2_STATE = 64
M2_CHUNK = 128
M2_CONV_DIM = M2_INNER + 2 * M2_GROUPS * M2_STATE
LRU_WIDTH = D_MODEL // 2
LRU_BLOCKS = 8
LRU_BLOCK = LRU_WIDTH // LRU_BLOCKS
LRU_C = 8.0
N_EXPERTS = 16
CAPACITY = 2
D_EXPERT = D_MODEL
D_IN = S5_WIDTH + M2_INNER + M2_CONV_DIM + 2 * M2_HEADS + 2 * LRU_WIDTH + N_BRANCH * D_MODEL

kernel_name = 'hybrid_s5_ssd_rglru_ecmoe_diffusion'


def _rmsnorm(x, g):
    x32 = x.astype(jnp.float32)
    y = x32 * lax.rsqrt(jnp.mean(x32 * x32, axis=-1, keepdims=True) + EPS)
    return (y * g.astype(jnp.float32)).astype(x.dtype)


def _modulate(x, g, shift, scale):
    return _rmsnorm(x, g) * (1 + scale) + shift


def _rev(t):
    return jnp.flip(t, axis=1)


def _dwconv(x, w, b):
    k = w.shape[0]
    y = lax.conv_general_dilated(x, w.astype(x.dtype)[:, None, :], window_strides=(1,), padding=[(k // 2, k - 1 - k // 2)], dimension_numbers=('NWC', 'WIO', 'NWC'), feature_group_count=x.shape[-1])
    return y + b.astype(x.dtype)


def _to_col_major(t, rows):
    b, l, ch = t.shape
    return t.reshape(b, rows, GRID_W, ch).transpose(0, 2, 1, 3).reshape(b, l, ch)


def _to_row_major(t, rows):
    b, l, ch = t.shape
    return t.reshape(b, GRID_W, rows, ch).transpose(0, 2, 1, 3).reshape(b, l, ch)


def _segsum(a):
    t = a.shape[-1]
    cs = jnp.cumsum(a, axis=-1)
    diff = cs[..., :, None] - cs[..., None, :]
    mask = jnp.tril(jnp.ones((t, t), dtype=bool))
    return jnp.where(mask, diff, -jnp.inf)


def _linear_scan(a, v, h0):
    if h0 is not None:
        v = v.at[:, 0].add(a[:, 0] * h0)
    def comb(e1, e2):
        a1, b1 = e1
        a2, b2 = e2
        return a1 * a2, a2 * b1 + b2
    _, h = lax.associative_scan(comb, (a, v), axis=1)
    return h, h[:, -1]


def _s5_scan(u, lam_re, lam_im, log_step, b_re, b_im, c_re, c_im, h0):
    f32 = jnp.float32
    lam_re, lam_im = lam_re.astype(f32), lam_im.astype(f32)
    step = jnp.exp(log_step.astype(f32))[:, None]
    ar, ai = lam_re * step, lam_im * step
    mag = jnp.exp(ar)
    bar_re, bar_im = mag * jnp.cos(ai), mag * jnp.sin(ai)
    den = lam_re * lam_re + lam_im * lam_im
    nr, ni = bar_re - 1.0, bar_im
    coef_re = (nr * lam_re + ni * lam_im) / den
    coef_im = (ni * lam_re - nr * lam_im) / den
    bu_re = jnp.einsum('blgi,gpi->blgp', u, b_re.astype(f32))
    bu_im = jnp.einsum('blgi,gpi->blgp', u, b_im.astype(f32))
    v_re = coef_re * bu_re - coef_im * bu_im
    v_im = coef_re * bu_im + coef_im * bu_re
    if h0 is not None:
        h_re, h_im = h0
        v_re = v_re.at[:, 0].add(bar_re * h_re - bar_im * h_im)
        v_im = v_im.at[:, 0].add(bar_re * h_im + bar_im * h_re)
    cnt = jnp.ones((1, u.shape[1], 1, 1), f32)
    def comb(e1, e2):
        n1, r1, i1 = e1
        n2, r2, i2 = e2
        m = jnp.exp(n2 * ar)
        pr, pi = m * jnp.cos(n2 * ai), m * jnp.sin(n2 * ai)
        return n1 + n2, pr * r1 - pi * i1 + r2, pr * i1 + pi * r1 + i2
    _, s_re, s_im = lax.associative_scan(comb, (cnt, v_re, v_im), axis=1)
    y = jnp.einsum('blgp,gip->blgi', s_re, c_re.astype(f32)) - jnp.einsum('blgp,gip->blgi', s_im, c_im.astype(f32))
    return y, (s_re[:, -1], s_im[:, -1])


def _s5_branch(u, p, h0, rows):
    bsz, l, _ = u.shape
    u32 = u.astype(jnp.float32)
    us = u32 if rows is None else _to_col_major(u32, rows)
    ug = us.reshape(bsz, l, S5_GROUPS, S5_GROUP)
    y = jnp.zeros_like(ug)
    finals = []
    for d in range(2):
        inp = ug if d == 0 else _rev(ug)
        yd, fin = _s5_scan(inp, p['s5_lam_re'][d], p['s5_lam_im'][d], p['s5_log_step'][d], p['s5_b_re'][d], p['s5_b_im'][d], p['s5_c_re'][d], p['s5_c_im'][d], None if h0 is None else h0[d])
        y = y + (yd if d == 0 else _rev(yd))
        finals.append(fin)
    y = y.reshape(bsz, l, S5_WIDTH)
    if rows is not None:
        y = _to_row_major(y, rows)
    y = jax.nn.gelu(y + p['s5_d'].astype(jnp.float32) * u32).astype(u.dtype)
    val, gate = jnp.split(y @ p['s5_w_glu'], 2, axis=-1)
    return val * jax.nn.sigmoid(gate), finals


def _ssd_scan(xs, la, bm, cm, h0):
    bsz, l, g, j, hp = xs.shape
    n = bm.shape[-1]
    nc = l // M2_CHUNK
    xs = xs.reshape(bsz, nc, M2_CHUNK, g, j, hp)
    bm = bm.reshape(bsz, nc, M2_CHUNK, g, n)
    cm = cm.reshape(bsz, nc, M2_CHUNK, g, n)
    a = la.reshape(bsz, nc, M2_CHUNK, g, j).transpose(0, 3, 4, 1, 2)
    a_cum = jnp.cumsum(a, axis=-1)
    cb = jnp.einsum('bcqgn,bcsgn->bgcqs', cm, bm)
    lmat = jnp.exp(_segsum(a))
    y_diag = jnp.einsum('bgjcqs,bcsgjp->bcqgjp', cb[:, :, None] * lmat, xs)
    decay_in = jnp.exp(a_cum[..., -1:] - a_cum).transpose(0, 3, 4, 1, 2)
    st = jnp.einsum('bcsgn,bcsgjp->bcgjpn', bm, xs * decay_in[..., None])
    if h0 is None:
        h0 = jnp.zeros((bsz, g, j, hp, n), xs.dtype)
    st = jnp.concatenate([h0[:, None], st], axis=1)
    chunk_a = jnp.pad(a_cum[..., -1], ((0, 0), (0, 0), (0, 0), (1, 0)))
    decay_chunk = jnp.exp(_segsum(chunk_a))
    st = jnp.einsum('bgjzc,bcgjpn->bzgjpn', decay_chunk, st)
    entering, final = st[:, :-1], st[:, -1]
    decay_out = jnp.exp(a_cum).transpose(0, 3, 4, 1, 2)
    y_off = jnp.einsum('bcqgn,bcgjpn->bcqgjp', cm, entering) * decay_out[..., None]
    return (y_diag + y_off).reshape(bsz, l, g, j, hp), final


def _mamba2_branch(z, xbc, dt_raw, p, h0):
    bsz, l, _ = z.shape
    f32 = jnp.float32
    xbc = jax.nn.silu(_dwconv(xbc, p['m2_conv_w'], p['m2_conv_b']).astype(f32))
    gn = M2_GROUPS * M2_STATE
    xm = xbc[..., :M2_INNER].reshape(bsz, l, M2_GROUPS, M2_HPG, M2_HEAD_DIM)
    bm = xbc[..., M2_INNER:M2_INNER + gn].reshape(bsz, l, M2_GROUPS, M2_STATE)
    cm = xbc[..., M2_INNER + gn:].reshape(bsz, l, M2_GROUPS, M2_STATE)
    dtv = jax.nn.softplus(dt_raw.astype(f32).reshape(bsz, l, 2, M2_GROUPS, M2_HPG) + p['m2_dt_bias'].astype(f32).reshape(2, M2_GROUPS, M2_HPG))
    a = -jnp.exp(p['m2_a_log'].astype(f32)).reshape(2, M2_GROUPS, M2_HPG)
    y = p['m2_d'].astype(f32).reshape(M2_GROUPS, M2_HPG, 1) * xm
    finals = []
    for d in range(2):
        dtd = dtv[:, :, d]
        xs, la, bs, cs = xm * dtd[..., None], dtd * a[d], bm, cm
        if d == 1:
            xs, la, bs, cs = _rev(xs), _rev(la), _rev(bs), _rev(cs)
        yd, fin = _ssd_scan(xs, la, bs, cs, None if h0 is None else h0[d])
        y = y + (yd if d == 0 else _rev(yd))
        finals.append(fin)
    y = y.reshape(bsz, l, M2_INNER) * jax.nn.silu(z.astype(f32))
    y = _rmsnorm(y, p['m2_norm_g']).astype(z.dtype)
    return y @ p['m2_w_out'], finals


def _rglru_branch(xc, gc, p, h0):
    bsz, l, _ = xc.shape
    f32 = jnp.float32
    xc32 = _dwconv(xc, p['lru_conv_w'], p['lru_conv_b']).astype(f32)
    xb = xc32.reshape(bsz, l, LRU_BLOCKS, LRU_BLOCK)
    h = jnp.zeros_like(xc32)
    finals = []
    for d in range(2):
        r = jax.nn.sigmoid(jnp.einsum('blhi,hij->blhj', xb, p['lru_w_a'][d].astype(f32)).reshape(bsz, l, LRU_WIDTH) + p['lru_b_a'][d].astype(f32))
        ig = jax.nn.sigmoid(jnp.einsum('blhi,hij->blhj', xb, p['lru_w_x'][d].astype(f32)).reshape(bsz, l, LRU_WIDTH) + p['lru_b_x'][d].astype(f32))
        log_a = -LRU_C * r * jax.nn.softplus(-p['lru_lam'][d].astype(f32))
        a_t = jnp.exp(log_a)
        v = jnp.sqrt(jnp.maximum(-jnp.expm1(2.0 * log_a), EPS)) * (ig * xc32)
        if d == 1:
            a_t, v = _rev(a_t), _rev(v)
        hd, fin = _linear_scan(a_t, v, None if h0 is None else h0[d])
        h = h + (hd if d == 0 else _rev(hd))
        finals.append(fin)
    y = (h * jax.nn.gelu(gc.astype(f32))).astype(xc.dtype)
    return y @ p['lru_w_out'], finals


def _token_mixer(h, p, h0, rows):
    sizes = (S5_WIDTH, M2_INNER, M2_CONV_DIM, 2 * M2_HEADS, LRU_WIDTH, LRU_WIDTH)
    idx = np.cumsum(sizes).tolist()
    proj = h @ p['w_in']
    u_s5, z, xbc, dt_raw, x_lru, g_lru, gates = jnp.split(proj, idx, axis=-1)
    h0a, h0b, h0c = (None, None, None) if h0 is None else h0
    ya, sa = _s5_branch(u_s5, p, h0a, rows)
    yb, sb = _mamba2_branch(z, xbc, dt_raw, p, h0b)
    yc, sc = _rglru_branch(x_lru, g_lru, p, h0c)
    f32 = jnp.float32
    g = jax.nn.sigmoid(gates.astype(f32)).reshape(gates.shape[0], gates.shape[1], N_BRANCH, D_MODEL)
    merged = g[:, :, 0] * ya.astype(f32) + g[:, :, 1] * yb.astype(f32) + g[:, :, 2] * yc.astype(f32)
    return merged.astype(h.dtype) @ p['w_o'], (sa, sb, sc)


def _expert_choice_moe(h, p):
    bsz, n, d = h.shape
    cap = CAPACITY * n // N_EXPERTS
    aff = jax.nn.softmax((h @ p['moe_w_router']).astype(jnp.float32), axis=-1)
    wts, idx = lax.top_k(aff.transpose(0, 2, 1), cap)
    xs = jax.vmap(lambda hb, ib: hb[ib])(h, idx)
    hid = jax.nn.silu(jnp.einsum('becd,edf->becf', xs, p['moe_w1'])) * jnp.einsum('becd,edf->becf', xs, p['moe_w3'])
    ys = jnp.einsum('becf,efd->becd', hid, p['moe_w2']) * wts[..., None].astype(h.dtype)
    return jax.vmap(lambda yb, ib: jnp.zeros((n, d), yb.dtype).at[ib.reshape(-1)].add(yb.reshape(-1, d)))(ys, idx)


def setup_inputs(seed: int = 0) -> dict:
    key = jax.random.key(seed)
    keys = iter(jax.random.split(key, 64))
    f32 = jnp.float32
    def nrm(shape, scale):
        return jax.random.normal(next(keys), shape, f32) * scale
    def uni(shape, lo, hi):
        return jax.random.uniform(next(keys), shape, f32, lo, hi)
    L, D = DEPTH, D_MODEL
    G, P, I = S5_GROUPS, S5_STATE, S5_GROUP
    x = nrm((BATCH, SEQ, D), 1.0)
    c = nrm((BATCH, D), 1.0)
    ctx = nrm((BATCH, CTX_LEN, D), 1.0)
    c_ctx = nrm((D,), 1.0)
    w_mod = nrm((L, D, 6 * D), 0.5 * D ** -0.5)
    b_mod = nrm((L, 6 * D), 0.01)
    norm1_g = 1.0 + nrm((L, D), 0.01)
    norm2_g = 1.0 + nrm((L, D), 0.01)
    w_in = nrm((L, D, D_IN), D ** -0.5)
    s5_lam_re = -0.5 + nrm((L, 2, G, P), 0.01)
    s5_lam_im = math.pi * jnp.arange(P, dtype=f32) + nrm((L, 2, G, P), 0.01)
    s5_log_step = uni((L, 2, G), math.log(1e-3), math.log(1e-1))
    s5_b_re = nrm((L, 2, G, P, I), (2 * I) ** -0.5)
    s5_b_im = nrm((L, 2, G, P, I), (2 * I) ** -0.5)
    s5_c_re = nrm((L, 2, G, I, P), (2 * P) ** -0.5)
    s5_c_im = nrm((L, 2, G, I, P), (2 * P) ** -0.5)
    s5_d = nrm((L, S5_WIDTH), 0.5)
    s5_w_glu = nrm((L, S5_WIDTH, 2 * D), S5_WIDTH ** -0.5)
    m2_conv_w = nrm((L, CONV_W, M2_CONV_DIM), CONV_W ** -0.5)
    m2_conv_b = nrm((L, M2_CONV_DIM), 0.01)
    dt0 = jnp.exp(uni((L, 2, M2_HEADS), math.log(1e-3), math.log(1e-1)))
    m2_dt_bias = dt0 + jnp.log(-jnp.expm1(-dt0))
    m2_a_log = jnp.log(uni((L, 2, M2_HEADS), 1.0, 16.0))
    m2_d = 1.0 + nrm((L, M2_HEADS), 0.1)
    m2_norm_g = 1.0 + nrm((L, M2_INNER), 0.01)
    m2_w_out = nrm((L, M2_INNER, D), M2_INNER ** -0.5)
    lru_conv_w = nrm((L, CONV_W, LRU_WIDTH), CONV_W ** -0.5)
    lru_conv_b = nrm((L, LRU_WIDTH), 0.01)
    lru_w_a = nrm((L, 2, LRU_BLOCKS, LRU_BLOCK, LRU_BLOCK), LRU_BLOCK ** -0.5)
    lru_b_a = nrm((L, 2, LRU_WIDTH), 0.01)
    lru_w_x = nrm((L, 2, LRU_BLOCKS, LRU_BLOCK, LRU_BLOCK), LRU_BLOCK ** -0.5)
    lru_b_x = nrm((L, 2, LRU_WIDTH), 0.01)
    a0 = uni((L, 2, LRU_WIDTH), 0.9, 0.999)
    s = a0 ** (1.0 / LRU_C)
    lru_lam = jnp.log(s) - jnp.log1p(-s)
    lru_w_out = nrm((L, LRU_WIDTH, D), LRU_WIDTH ** -0.5)
    w_o = nrm((L, D, D), D ** -0.5)
    moe_w_router = nrm((L, D, N_EXPERTS), D ** -0.5)
    moe_w1 = nrm((L, N_EXPERTS, D, D_EXPERT), D ** -0.5)
    moe_w3 = nrm((L, N_EXPERTS, D, D_EXPERT), D ** -0.5)
    moe_w2 = nrm((L, N_EXPERTS, D_EXPERT, D), D_EXPERT ** -0.5)
    final_norm_g = 1.0 + nrm((D,), 0.01)
    return {'x': x, 'c': c, 'ctx': ctx, 'c_ctx': c_ctx, 'w_mod': w_mod, 'b_mod': b_mod, 'norm1_g': norm1_g, 'norm2_g': norm2_g, 'w_in': w_in, 's5_lam_re': s5_lam_re, 's5_lam_im': s5_lam_im, 's5_log_step': s5_log_step, 's5_b_re': s5_b_re, 's5_b_im': s5_b_im, 's5_c_re': s5_c_re, 's5_c_im': s5_c_im, 's5_d': s5_d, 's5_w_glu': s5_w_glu, 'm2_conv_w': m2_conv_w, 'm2_conv_b': m2_conv_b, 'm2_dt_bias': m2_dt_bias, 'm2_a_log': m2_a_log, 'm2_d': m2_d, 'm2_norm_g': m2_norm_g, 'm2_w_out': m2_w_out, 'lru_conv_w': lru_conv_w, 'lru_conv_b': lru_conv_b, 'lru_w_a': lru_w_a, 'lru_b_a': lru_b_a, 'lru_w_x': lru_w_x, 'lru_b_x': lru_b_x, 'lru_lam': lru_lam, 'lru_w_out': lru_w_out, 'w_o': w_o, 'moe_w_router': moe_w_router, 'moe_w1': moe_w1, 'moe_w3': moe_w3, 'moe_w2': moe_w2, 'final_norm_g': final_norm_g}


def reference(x, c, ctx, c_ctx, w_mod, b_mod, norm1_g, norm2_g, w_in, s5_lam_re, s5_lam_im, s5_log_step, s5_b_re, s5_b_im, s5_c_re, s5_c_im, s5_d, s5_w_glu, m2_conv_w, m2_conv_b, m2_dt_bias, m2_a_log, m2_d, m2_norm_g, m2_w_out, lru_conv_w, lru_conv_b, lru_w_a, lru_b_a, lru_w_x, lru_b_x, lru_lam, lru_w_out, w_o, moe_w_router, moe_w1, moe_w3, moe_w2, final_norm_g):
    rows = x.shape[1] // GRID_W
    sc = jax.nn.silu(c)
    scc = jax.nn.silu(c_ctx)
    for i in range(DEPTH):
        p = {'w_in': w_in[i], 's5_lam_re': s5_lam_re[i], 's5_lam_im': s5_lam_im[i], 's5_log_step': s5_log_step[i], 's5_b_re': s5_b_re[i], 's5_b_im': s5_b_im[i], 's5_c_re': s5_c_re[i], 's5_c_im': s5_c_im[i], 's5_d': s5_d[i], 's5_w_glu': s5_w_glu[i], 'm2_conv_w': m2_conv_w[i], 'm2_conv_b': m2_conv_b[i], 'm2_dt_bias': m2_dt_bias[i], 'm2_a_log': m2_a_log[i], 'm2_d': m2_d[i], 'm2_norm_g': m2_norm_g[i], 'm2_w_out': m2_w_out[i], 'lru_conv_w': lru_conv_w[i], 'lru_conv_b': lru_conv_b[i], 'lru_w_a': lru_w_a[i], 'lru_b_a': lru_b_a[i], 'lru_w_x': lru_w_x[i], 'lru_b_x': lru_b_x[i], 'lru_lam': lru_lam[i], 'lru_w_out': lru_w_out[i], 'w_o': w_o[i], 'moe_w_router': moe_w_router[i], 'moe_w1': moe_w1[i], 'moe_w3': moe_w3[i], 'moe_w2': moe_w2[i]}
        mod_x = jnp.split((sc @ w_mod[i] + b_mod[i])[:, None, :], 6, axis=-1)
        mod_c = jnp.split((scc @ w_mod[i] + b_mod[i])[None, None, :], 6, axis=-1)
        mc, ctx_states = _token_mixer(_modulate(ctx, norm1_g[i], mod_c[0], mod_c[1]), p, None, None)
        if i < DEPTH - 1:
            ctx = ctx + mod_c[2] * mc
            ctx = ctx + mod_c[5] * _expert_choice_moe(_modulate(ctx, norm2_g[i], mod_c[3], mod_c[4]), p)
        mx, _ = _token_mixer(_modulate(x, norm1_g[i], mod_x[0], mod_x[1]), p, ctx_states, rows)
        x = x + mod_x[2] * mx
        x = x + mod_x[5] * _expert_choice_moe(_modulate(x, norm2_g[i], mod_x[3], mod_x[4]), p)
    return _rmsnorm(x, final_norm_g)
```

```python
import numpy as np
from contextlib import ExitStack
import concourse.bass as bass
import concourse.mybir as mybir

F32 = mybir.dt.float32
BF16 = mybir.dt.bfloat16
I32 = mybir.dt.int32
ALU = mybir.AluOpType
AF = mybir.ActivationFunctionType
AX = mybir.AxisListType


class Buf:
    __slots__ = ("name", "w", "r")

    def __init__(self, name=""):
        self.name = name
        self.w = None
        self.r = {}


class Eng:
    def __init__(self, fw, name, handle, sem, step=1):
        self.fw = fw
        self.name = name
        self.h = handle
        self.sem = sem
        self.count = 0
        self.step = step
        self.seen = {}


class FW:
    def __init__(self, nc, n_dma_sems=24, n_gdma_sems=8):
        self.nc = nc
        self.es = ExitStack()
        mk = lambda n: self.es.enter_context(nc.semaphore(n))
        self.pe = Eng(self, "pe", nc.tensor, mk("s_pe"))
        self.dve = Eng(self, "dve", nc.vector, mk("s_dve"))
        self.act = Eng(self, "act", nc.scalar, mk("s_act"))
        self.pool = Eng(self, "pool", nc.gpsimd, mk("s_pool"))
        self.sp = Eng(self, "sp", nc.sync, mk("s_sp"))
        self.engs = [self.pe, self.dve, self.act, self.pool, self.sp]
        self.dq = [Eng(self, f"dq{i}", None, mk(f"s_dq{i}"), step=16) for i in range(n_dma_sems)]
        self.gq = [Eng(self, f"gq{i}", None, mk(f"s_gq{i}"), step=16) for i in range(n_gdma_sems)]
        self.aq = [Eng(self, f"aq{i}", None, mk(f"s_aq{i}"), step=16) for i in range(8)]
        self.dq_i = 0
        self.gq_i = 0
        self.aq_i = 0
        self.n_wait = 0
        self.n_inst = 0

    def _wait(self, eng, e2, c, raw=False):
        if e2 is eng:
            if (not raw) or eng is self.pe or eng is self.sp or eng.count - c > 2:
                return
        if eng.seen.get(e2, 0) >= c:
            return
        eng.h.wait_ge(e2.sem, c)
        eng.seen[e2] = c
        self.n_wait += 1

    def _deps(self, eng, reads, writes):
        for b in reads:
            if b.w is not None:
                self._wait(eng, b.w[0], b.w[1], raw=True)
        for b in writes:
            if b.w is not None:
                self._wait(eng, *b.w)
            for e2, c in b.r.items():
                self._wait(eng, e2, c)

    def _mark(self, tok_eng, tok_c, reads, writes):
        for b in reads:
            if b.r.get(tok_eng, 0) < tok_c:
                b.r[tok_eng] = tok_c
        for b in writes:
            b.w = (tok_eng, tok_c)
            b.r = {}

    def op(self, eng, fn, reads=(), writes=()):
        self._deps(eng, reads, writes)
        inst = fn()
        eng.count += 1
        inst.then_inc(eng.sem, 1)
        self._mark(eng, eng.count, reads, writes)
        self.n_inst += 1
        return inst

    def dma(self, out, in_, reads=(), writes=(), q="sp", **kw):
        if q == "sp":
            issuer = self.sp; pool = self.dq; i = self.dq_i; self.dq_i = (i + 1) % len(pool)
        elif q == "act":
            issuer = self.act; pool = self.aq; i = self.aq_i; self.aq_i = (i + 1) % len(pool)
        else:
            issuer = self.pool; pool = self.gq; i = self.gq_i; self.gq_i = (i + 1) % len(pool)
        tok = pool[i]
        if tok.count > 0:
            self._wait(issuer, tok, tok.count)
        self._deps(issuer, reads, writes)
        inst = issuer.h.dma_start(out=out, in_=in_, **kw)
        tok.count += 16
        inst.then_inc(tok.sem, 16)
        self._mark(tok, tok.count, reads, writes)
        self.n_inst += 1
        return inst

    def idma(self, reads=(), writes=(), **kw):
        issuer = self.pool; pool = self.gq; i = self.gq_i; self.gq_i = (i + 1) % len(pool)
        tok = pool[i]
        if tok.count > 0:
            self._wait(issuer, tok, tok.count)
        self._deps(issuer, reads, writes)
        inst = issuer.h.indirect_dma_start(**kw)
        tok.count += 16
        inst.then_inc(tok.sem, 16)
        self._mark(tok, tok.count, reads, writes)
        self.n_inst += 1
        return inst

    def barrier(self):
        allq = self.engs + self.dq + self.gq + self.aq
        for e in self.engs:
            for e2 in allq:
                if e2 is not e and e2.count > 0:
                    self._wait(e, e2, e2.count)

    def finish(self):
        self.barrier()

    def close(self):
        self.es.close()


D = 1024
NPJ = 4128
C_U, C_Z, C_XBC, C_DT, C_XL, C_GL, C_G = 0, 512, 1536, 3072, 3104, 3616, 4128
GELU_K = 1.5957691216057308


class Stream:
    pass


class KB:
    def __init__(self, nc, n_samp=2, dbg=False):
        self.nc = nc
        self.fw = FW(nc)
        self.ges = ExitStack()
        self.n_samp = n_samp
        self.din = {}
        self.dbg = dbg
        self._uid = 0

    def inp(self, name, shape, dt=F32):
        t = self.nc.dram_tensor(name, list(shape), dt, kind="ExternalInput")
        self.din[name] = (tuple(shape), dt)
        return t.ap()

    def scratch(self, name, shape, dt=F32, out=False):
        kind = "ExternalOutput" if (out or self.dbg) else "Internal"
        return self.nc.dram_tensor(name, list(shape), dt, kind=kind).ap()

    def sb(self, es, name, shape, dt=F32):
        self._uid += 1
        return es.enter_context(self.nc.sbuf_tensor(f"{name}_{self._uid}", list(shape), dt))

    def ps(self, es, name, shape, dt=F32):
        self._uid += 1
        return es.enter_context(self.nc.psum_tensor(f"{name}_{self._uid}", list(shape), dt))

    def V(self, fn, r=(), w=()):
        return self.fw.op(self.fw.dve, fn, r, w)

    def A(self, fn, r=(), w=()):
        return self.fw.op(self.fw.act, fn, r, w)

    def G(self, fn, r=(), w=()):
        return self.fw.op(self.fw.pool, fn, r, w)

    def T(self, fn, r=(), w=()):
        return self.fw.op(self.fw.pe, fn, r, w)

    def dma(self, out, in_, r=(), w=(), q="sp", **kw):
        return self.fw.dma(out, in_, r, w, q=q, **kw)

    def setup(self):
        nc = self.nc
        es = self.ges
        S = self.n_samp
        self.x_in = self.inp("x", [S, 4096, D])
        self.c_in = self.inp("ctx", [S, 256, D])
        self.cvec_in = self.inp("cvec", [128, 8, 3])
        self.w_mod = self.inp("w_mod", [4, D, 6144])
        self.b_modT = self.inp("b_modT", [4, 128, 48])
        self.n1gT = self.inp("n1gT", [4, 128, 8])
        self.n2gT = self.inp("n2gT", [4, 128, 8])
        self.w_in = self.inp("w_in", [4, D, 7200])
        self.ident_in = self.inp("ident_in", [128, 128])
        self.lru_cw = self.inp("lru_cw", [4, 128, 4, 4])
        self.lru_cb = self.inp("lru_cb", [4, 128, 4])
        self.lru_wa = self.inp("lru_wa", [4, 2, 4, 128, 128])
        self.lru_wx = self.inp("lru_wx", [4, 2, 4, 128, 128])
        self.lru_bal = self.inp("lru_bal", [4, 128, 3, 2, 4])
        self.ident_f = self.sb(es, "ident_f", [128, 128]); self.b_const = Buf("const")
        self.ident_b = self.sb(es, "ident_b", [128, 128], BF16)
        self.ones_f = self.sb(es, "ones_f", [128, 128])
        self.dma(self.ident_f[:], self.ident_in, w=[self.b_const])
        self.V(lambda: nc.vector.tensor_copy(self.ident_b[:], self.ident_f[:]), r=[self.b_const], w=[self.b_const])
        self.V(lambda: nc.vector.memset(self.ones_f[:], 1.0), w=[self.b_const])
        self.scv = self.sb(es, "scv", [128, 8, 3]); self.b_scv = Buf("scv")
        self.modc = self.sb(es, "modc", [128, 48, 3]); self.b_modc = Buf("modc")
        self.nsc = self.sb(es, "nsc", [128, 3, 4, 8]); self.b_nsc = Buf("nsc")
        self.gb = self.sb(es, "gb", [128, 3, 2, D]); self.b_gb = Buf("gb")
        self.dma(self.scv[:], self.cvec_in, w=[self.b_scv])
        self.A(lambda: nc.scalar.activation(out=self.scv[:], in_=self.scv[:], func=AF.Silu), r=[self.b_scv], w=[self.b_scv])
        self.lru_st = self.sb(es, "lru_st", [128, 4, 2, S]); self.b_lru_st = Buf("lru_st")
        self.streams = {}
        for kind, L, src in (("c", 256, self.c_in), ("x", 4096, self.x_in)):
            for s in range(S):
                st = Stream()
                st.kind, st.s, st.L = kind, s, L
                st.NT = L // 128
                st.TB = min(512, L)
                st.NB = L // st.TB
                st.v = 2 if kind == "c" else s
                st.src = src[s]
                st.res = self.scratch(f"res_{kind}{s}", [L, D])
                st.b_res = [Buf() for _ in range(st.NT)]
                st.first = True
                st.PJ = self.scratch(f"pj_{kind}{s}", [NPJ, L])
                st.b_pj = {}
                st.hTs = self.scratch(f"hTs_{kind}{s}", [128, 8, L], BF16)
                st.b_hTs = Buf()
                st.ylru = self.scratch(f"ylru_{kind}{s}", [512, L], BF16)
                st.b_ylru = [Buf() for _ in range(4)]
                st.ys5 = self.scratch(f"ys5_{kind}{s}", [512, L], BF16)
                st.b_ys5 = [Buf() for _ in range(4)]
                st.yssd = self.scratch(f"yssd_{kind}{s}", [1024, L], BF16)
                st.b_yssd = [Buf() for _ in range(8)]
                self.streams[(kind, s)] = st

    def pjbuf(self, st, r0):
        if r0 not in st.b_pj:
            st.b_pj[r0] = Buf(f"pj{r0}")
        return st.b_pj[r0]

    def ph_mod(self, i):
        nc = self.nc
        with ExitStack() as es:
            wm = [self.sb(es, f"wm{k}", [128, 8, 512]) for k in range(2)]
            b_wm = [Buf() for _ in range(2)]
            pm = [self.ps(es, f"pmod{k}", [128, 4, 4]) for k in range(2)]
            b_pm = [Buf() for _ in range(2)]
            bm = self.sb(es, "bm", [128, 48]); b_bm = Buf()
            g12 = self.sb(es, "g12", [128, 2, 8]); b_g12 = Buf()
            self.dma(bm[:], self.b_modT[i], w=[b_bm])
            self.dma(g12[:, 0, :], self.n1gT[i], w=[b_g12])
            self.dma(g12[:, 1, :], self.n2gT[i], w=[b_g12])
            for nb in range(12):
                k = nb % 2
                self.dma(wm[k][:], self.w_mod[i][:, nb * 512:(nb + 1) * 512].rearrange("(kc p) n -> p kc n", p=128), w=[b_wm[k]])
                for j in range(4):
                    for kc in range(8):
                        self.T(lambda: nc.tensor.matmul(pm[k][:, j, 0:3], wm[k][:, kc, j * 128:(j + 1) * 128], self.scv[:, kc, :],
                                                        start=(kc == 0), stop=(kc == 7)),
                               r=[b_wm[k], self.b_scv], w=[b_pm[k]])
                for j in range(4):
                    jj = nb * 4 + j
                    self.V(lambda: nc.vector.tensor_scalar(self.modc[:, jj, :], pm[k][:, j, 0:3], bm[:, jj:jj + 1], None, ALU.add),
                           r=[b_pm[k], b_bm], w=[self.b_modc])
            for v in range(3):
                for (o, seg_sc, seg_sh, gi) in ((0, 1, 0, 0), (2, 4, 3, 1)):
                    self.V(lambda: nc.vector.scalar_tensor_tensor(self.nsc[:, v, o, :], self.modc[:, seg_sc * 8:seg_sc * 8 + 8, v], 1.0,
                                                                  g12[:, gi, :], ALU.add, ALU.mult),
                           r=[self.b_modc, b_g12], w=[self.b_nsc])
                    self.V(lambda: nc.vector.tensor_copy(self.nsc[:, v, o + 1, :], self.modc[:, seg_sh * 8:seg_sh * 8 + 8, v]),
                           r=[self.b_modc], w=[self.b_nsc])
            dg = [self.sb(es, f"dg{k}", [128, 128]) for k in range(2)]; b_dg = [Buf() for _ in range(2)]
            pb = [self.ps(es, f"pb{k}", [128, 512]) for k in range(2)]; b_pb = [Buf() for _ in range(2)]
            n = 0
            for v in range(3):
                for gi, seg in ((0, 2), (1, 5)):
                    for half in range(2):
                        k = n % 2; n += 1
                        for q in range(4):
                            kc = half * 4 + q
                            d = (n * 4 + q) % 2
                            self.G(lambda: nc.gpsimd.tensor_scalar(dg[d][:], self.ident_f[:], self.modc[:, seg * 8 + kc, v:v + 1], None, ALU.mult),
                                   r=[self.b_const, self.b_modc], w=[b_dg[d]])
                            self.T(lambda: nc.tensor.matmul(pb[k][:, q * 128:(q + 1) * 128], self.ones_f[:], dg[d][:], start=True, stop=True),
                                   r=[self.b_const, b_dg[d]], w=[b_pb[k]])
                        self.A(lambda: nc.scalar.copy(self.gb[:, v, gi, half * 512:(half + 1) * 512], pb[k][:]), r=[b_pb[k]], w=[self.b_gb])
        self.fw.barrier()

    def ph_norm_proj(self, i, st):
        nc = self.nc
        L, NT, TB, NB = st.L, st.NT, st.TB, st.NB
        src = st.src if st.first else st.res
        with ExitStack() as es:
            hT = self.sb(es, "hT", [128, 8, L], BF16)
            b_hT = [Buf() for _ in range(NT)]
            with ExitStack() as es1:
                xt = [self.sb(es1, f"xt{k}", [128, D]) for k in range(2)]; b_xt = [Buf() for _ in range(2)]
                xn = [self.sb(es1, f"xn{k}", [128, D], BF16) for k in range(2)]; b_xn = [Buf() for _ in range(2)]
                junk = self.sb(es1, "junk", [128, D], BF16); b_junk = Buf()
                ssq = [self.sb(es1, f"ssq{k}", [128, 1]) for k in range(2)]; b_ssq = [Buf() for _ in range(2)]
                ptr = [self.ps(es1, f"ptr{k}", [128, 8, 128], BF16) for k in range(2)]; b_ptr = [Buf() for _ in range(2)]
                for t in range(NT):
                    k = t % 2
                    self.dma(xt[k][:], src[t * 128:(t + 1) * 128, :], r=[st.b_res[t]], w=[b_xt[k]])
                    self.A(lambda: nc.scalar.activation(out=junk[:], in_=xt[k][:], func=AF.Square, accum_out=ssq[k][:]),
                           r=[b_xt[k]], w=[b_junk, b_ssq[k]])
                    self.A(lambda: nc.scalar.activation(out=ssq[k][:], in_=ssq[k][:], func=AF.Sqrt, scale=1.0 / D, bias=1e-6),
                           r=[b_ssq[k]], w=[b_ssq[k]])
                    self.V(lambda: nc.vector.reciprocal(ssq[k][:], ssq[k][:]), r=[b_ssq[k]], w=[b_ssq[k]])
                    self.A(lambda: nc.scalar.activation(out=xn[k][:], in_=xt[k][:], func=AF.Copy, scale=ssq[k][:, 0:1]),
                           r=[b_xt[k], b_ssq[k]], w=[b_xn[k]])
                    for kc in range(8):
                        self.T(lambda: nc.tensor.transpose(ptr[k][:, kc, :], xn[k][:, kc * 128:(kc + 1) * 128], self.ident_b[:]),
                               r=[b_xn[k], self.b_const], w=[b_ptr[k]])
                    for kc in range(8):
                        o = hT[:, kc, t * 128:(t + 1) * 128]
                        sc = self.nsc[:, st.v, 0, kc:kc + 1]; sh = self.nsc[:, st.v, 1, kc:kc + 1]
                        if kc % 2 == 0:
                            self.V(lambda: nc.vector.tensor_scalar(o, ptr[k][:, kc, :], sc, sh, ALU.mult, ALU.add),
                                   r=[b_ptr[k], self.b_nsc], w=[b_hT[t]])
                        else:
                            self.A(lambda: nc.scalar.activation(out=o, in_=ptr[k][:, kc, :], func=AF.Identity, scale=sc, bias=sh),
                                   r=[b_ptr[k], self.b_nsc], w=[b_hT[t]])
                self.dma(st.hTs, hT[:], r=b_hT, w=[st.b_hTs])
            wf = self.sb(es, "wf", [128, 8, 512]); b_wf = Buf()
            wb = [self.sb(es, f"wb{k}", [128, 8, 512], BF16) for k in range(2)]; b_wb = [Buf() for _ in range(2)]
            stg = [self.sb(es, f"stg{k}", [128, L]) for k in range(2)]; b_stg = [Buf() for _ in range(2)]
            pm = [self.ps(es, f"pm{k}", [128, 512]) for k in range(4)]; b_pm = [Buf() for _ in range(4)]
            zst = [self.sb(es, f"zst{k}", [128, 512]) for k in range(2)]; b_zst = [Buf() for _ in range(2)]
            dts = self.sb(es, "dts", [128, NT, 32]); b_dts = Buf()
            groups = [(c0, 512) for c0 in range(0, 3072, 512)] + [(3072, 32)] + [(c0, 512) for c0 in (3104, 3616)]
            n_ev = 0; n_st = 0
            for gi, (c0, ncol) in enumerate(groups):
                k = gi % 2
                self.dma(wf[:, :, :ncol], self.w_in[i][:, c0:c0 + ncol].rearrange("(kc p) n -> p kc n", p=128), w=[b_wf])
                self.G(lambda: nc.gpsimd.tensor_copy(wb[k][:, :, :ncol], wf[:, :, :ncol]), r=[b_wf], w=[b_wb[k]])
                tokmajor = hasattr(st, "z_tm") and c0 in (512, 1024, 3072)
                if tokmajor:
                    for t in range(NT):
                        pk = n_ev % 4
                        for kc in range(8):
                            self.T(lambda: nc.tensor.matmul(pm[pk][:, :ncol], hT[:, kc, t * 128:(t + 1) * 128], wb[k][:, kc, :ncol],
                                                            start=(kc == 0), stop=(kc == 7)),
                                   r=[b_wb[k], b_hT[t]], w=[b_pm[pk]])
                        if c0 == 3072:
                            self.V(lambda: nc.vector.tensor_copy(dts[:, t, :], pm[pk][:, :32]), r=[b_pm[pk]], w=[b_dts])
                        else:
                            zk = n_ev % 2
                            if n_ev % 2 == 0:
                                self.A(lambda: nc.scalar.copy(zst[zk][:], pm[pk][:, :512]), r=[b_pm[pk]], w=[b_zst[zk]])
                            else:
                                self.V(lambda: nc.vector.tensor_copy(zst[zk][:], pm[pk][:, :512]), r=[b_pm[pk]], w=[b_zst[zk]])
                            self.dma(st.z_tm[t * 128:(t + 1) * 128, c0 - 512:c0], zst[zk][:], r=[b_zst[zk]], w=[st.b_z_tm[t]])
                        n_ev += 1
                    if c0 == 3072:
                        self.dma(st.dt_tm.rearrange("(t p) c -> p t c", p=128), dts[:], r=[b_dts], w=[st.b_dt_tm])
                    continue
                for r0 in range(0, ncol, 128):
                    m = min(128, ncol - r0)
                    sk = n_st % 2; n_st += 1
                    for tb in range(NB):
                        pk = n_ev % 4
                        tiles = range(tb * TB // 128, (tb + 1) * TB // 128)
                        for kc in range(8):
                            self.T(lambda: nc.tensor.matmul(pm[pk][:m, :TB], wb[k][:, kc, r0:r0 + m], hT[:, kc, tb * TB:(tb + 1) * TB],
                                                            start=(kc == 0), stop=(kc == 7)),
                                   r=[b_wb[k]] + [b_hT[t] for t in tiles], w=[b_pm[pk]])
                        if n_ev % 2 == 0:
                            self.A(lambda: nc.scalar.copy(stg[sk][:m, tb * TB:(tb + 1) * TB], pm[pk][:m, :TB]), r=[b_pm[pk]], w=[b_stg[sk]])
                        else:
                            self.V(lambda: nc.vector.tensor_copy(stg[sk][:m, tb * TB:(tb + 1) * TB], pm[pk][:m, :TB]), r=[b_pm[pk]], w=[b_stg[sk]])
                        n_ev += 1
                    self.dma(st.PJ[c0 + r0:c0 + r0 + m, :], stg[sk][:m, :], r=[b_stg[sk]], w=[self.pjbuf(st, c0 + r0)])
        self.fw.barrier()

    def ph_lru(self, i, kind):
        nc = self.nc
        sts = [self.streams[(kind, s)] for s in range(self.n_samp)]
        L = sts[0].L; TB = sts[0].TB; NB = sts[0].NB
        EPS1 = float(np.float32(1.0) - np.float32(1e-6))
        with ExitStack() as es:
            cw = self.sb(es, "cw", [128, 4, 4]); cb = self.sb(es, "cb", [128, 4]); b_cp = Buf()
            bal = self.sb(es, "bal", [128, 3, 2, 4]); b_bal = Buf()
            cc = self.sb(es, "cc", [128, 2, 2, 4]); b_cc = Buf()
            self.dma(cw[:], self.lru_cw[i], w=[b_cp]); self.dma(cb[:], self.lru_cb[i], w=[b_cp])
            self.dma(bal[:], self.lru_bal[i], w=[b_bal])
            self.A(lambda: nc.scalar.activation(out=cc[:, 0], in_=bal[:, 2], func=AF.Exp, scale=-1.0), r=[b_bal], w=[b_cc])
            self.A(lambda: nc.scalar.activation(out=cc[:, 0], in_=cc[:, 0], func=AF.Ln, bias=1.0), r=[b_cc], w=[b_cc])
            self.V(lambda: nc.vector.tensor_scalar(cc[:, 1], cc[:, 0], -16.0, None, ALU.mult), r=[b_cc], w=[b_cc])
            self.V(lambda: nc.vector.tensor_scalar(cc[:, 0], cc[:, 0], -8.0, None, ALU.mult), r=[b_cc], w=[b_cc])
            wa = self.sb(es, "wa", [128, 2, 2, 128]); b_wa = Buf()
            xlp = self.sb(es, "xlp", [128, L + 3]); b_xlp = Buf()
            gg = self.sb(es, "gg", [128, L]); b_gg = Buf()
            xc = self.sb(es, "xc", [128, L]); b_xc = Buf()
            hf = self.sb(es, "hf", [128, L]); b_hf = Buf()
            A1 = self.sb(es, "A1", [128, L]); b_A1 = Buf()
            A2 = self.sb(es, "A2", [128, L]); b_A2 = Buf()
            A3 = self.sb(es, "A3", [128, L]); b_A3 = Buf()
            yb = self.sb(es, "yb", [128, L], BF16); b_yb = Buf()
            pg = [self.ps(es, f"pg{k}", [128, 512]) for k in range(4)]; b_pg = [Buf() for _ in range(4)]
            self.V(lambda: nc.vector.memset(xlp[:, 0:2], 0.0), w=[b_xlp])
            self.V(lambda: nc.vector.memset(xlp[:, L + 2:L + 3], 0.0), w=[b_xlp])
            npg = 0
            for ch in range(4):
                for d in range(2):
                    self.dma(wa[:, d, 0, :], self.lru_wa[i, d, ch], w=[b_wa])
                    self.dma(wa[:, d, 1, :], self.lru_wx[i, d, ch], w=[b_wa])
                for st in sts:
                    r_xl = C_XL + ch * 128; r_gl = C_GL + ch * 128
                    self.dma(xlp[:, 2:L + 2], st.PJ[r_xl:r_xl + 128, :], r=[self.pjbuf(st, r_xl)], w=[b_xlp])
                    self.dma(gg[:], st.PJ[r_gl:r_gl + 128, :], r=[self.pjbuf(st, r_gl)], w=[b_gg])
                    self.V(lambda: nc.vector.tensor_scalar(xc[:], xlp[:, 0:L], cw[:, ch, 0:1], cb[:, ch:ch + 1], ALU.mult, ALU.add),
                           r=[b_xlp, b_cp], w=[b_xc])
                    for j in range(1, 4):
                        self.V(lambda: nc.vector.scalar_tensor_tensor(xc[:], xlp[:, j:j + L], cw[:, ch, j:j + 1], xc[:], ALU.mult, ALU.add),
                               r=[b_xlp, b_cp, b_xc], w=[b_xc])
                    self.G(lambda: nc.gpsimd.tensor_tensor(A1[:], gg[:], gg[:], ALU.mult), r=[b_gg], w=[b_A1])
                    self.G(lambda: nc.gpsimd.tensor_scalar(A1[:], A1[:], 0.044715, 1.0, ALU.mult, ALU.add), r=[b_A1], w=[b_A1])
                    self.G(lambda: nc.gpsimd.tensor_tensor(A1[:], A1[:], gg[:], ALU.mult), r=[b_A1, b_gg], w=[b_A1])
                    self.A(lambda: nc.scalar.activation(out=A1[:], in_=A1[:], func=AF.Sigmoid, scale=GELU_K), r=[b_A1], w=[b_A1])
                    self.G(lambda: nc.gpsimd.tensor_tensor(gg[:], gg[:], A1[:], ALU.mult), r=[b_A1, b_gg], w=[b_gg])
                    for d in range(2):
                        for tb in range(NB):
                            sl = slice(tb * TB, (tb + 1) * TB)
                            ka = npg % 4; kx = (npg + 1) % 4; npg += 2
                            self.T(lambda: nc.tensor.matmul(pg[ka][:, :TB], wa[:, d, 0, :], xc[:, sl], start=True, stop=True),
                                   r=[b_wa, b_xc], w=[b_pg[ka]])
                            self.T(lambda: nc.tensor.matmul(pg[kx][:, :TB], wa[:, d, 1, :], xc[:, sl], start=True, stop=True),
                                   r=[b_wa, b_xc], w=[b_pg[kx]])
                            self.A(lambda: nc.scalar.activation(out=A1[:, sl], in_=pg[ka][:, :TB], func=AF.Sigmoid, bias=bal[:, 0, d, ch:ch + 1]),
                                   r=[b_pg[ka], b_bal], w=[b_A1])
                            self.A(lambda: nc.scalar.activation(out=A3[:, sl], in_=pg[kx][:, :TB], func=AF.Sigmoid, bias=bal[:, 1, d, ch:ch + 1]),
                                   r=[b_pg[kx], b_bal], w=[b_A3])
                        self.A(lambda: nc.scalar.activation(out=A2[:], in_=A1[:], func=AF.Exp, scale=cc[:, 1, d, ch:ch + 1]), r=[b_A1, b_cc], w=[b_A2])
                        self.A(lambda: nc.scalar.activation(out=A1[:], in_=A1[:], func=AF.Exp, scale=cc[:, 0, d, ch:ch + 1]), r=[b_A1, b_cc], w=[b_A1])
                        self.V(lambda: nc.vector.tensor_scalar(A2[:], A2[:], EPS1, -1.0, ALU.min, ALU.mult), r=[b_A2], w=[b_A2])
                        self.A(lambda: nc.scalar.activation(out=A2[:], in_=A2[:], func=AF.Sqrt, bias=1.0), r=[b_A2], w=[b_A2])
                        self.G(lambda: nc.gpsimd.tensor_tensor(A3[:], A3[:], xc[:], ALU.mult), r=[b_A3, b_xc], w=[b_A3])
                        self.G(lambda: nc.gpsimd.tensor_tensor(A3[:], A3[:], A2[:], ALU.mult), r=[b_A3, b_A2], w=[b_A3])
                        if kind == "c":
                            init = 0.0; rd = []
                        else:
                            init = self.lru_st[:, ch, d, st.s:st.s + 1]; rd = [self.b_lru_st]
                        if d == 0:
                            self.V(lambda: nc.vector.tensor_tensor_scan(hf[:], A1[:], A3[:], init, ALU.mult, ALU.add),
                                   r=[b_A1, b_A3] + rd, w=[b_hf])
                            if kind == "c":
                                self.G(lambda: nc.gpsimd.tensor_copy(self.lru_st[:, ch, 0, st.s:st.s + 1], hf[:, L - 1:L]), r=[b_hf], w=[self.b_lru_st])
                        else:
                            rev = lambda t: bass.AP(t, L - 1, [[t[:].ap[0][0], 128], [-1, L]])
                            self.V(lambda: nc.vector.tensor_tensor_scan(rev(A2), rev(A1), rev(A3), init, ALU.mult, ALU.add),
                                   r=[b_A1, b_A3] + rd, w=[b_A2])
                            if kind == "c":
                                self.G(lambda: nc.gpsimd.tensor_copy(self.lru_st[:, ch, 1, st.s:st.s + 1], A2[:, 0:1]), r=[b_A2], w=[self.b_lru_st])
                    self.V(lambda: nc.vector.tensor_tensor(A2[:], A2[:], hf[:], ALU.add), r=[b_A2, b_hf], w=[b_A2])
                    self.V(lambda: nc.vector.tensor_tensor(yb[:], A2[:], gg[:], ALU.mult), r=[b_A2, b_gg], w=[b_yb])
                    self.dma(st.ylru[ch * 128:(ch + 1) * 128, :], yb[:], r=[b_yb], w=[st.b_ylru[ch]])
        self.fw.barrier()


def colform(v, nchunk):
    return np.ascontiguousarray(np.asarray(v, np.float32).reshape(nchunk, 128).T)


def prep_common(inp):
    o = {}
    o["w_mod"] = np.ascontiguousarray(inp["w_mod"], np.float32)
    o["b_modT"] = np.stack([colform(inp["b_mod"][i], 48) for i in range(4)])
    o["n1gT"] = np.stack([colform(inp["norm1_g"][i], 8) for i in range(4)])
    o["n2gT"] = np.stack([colform(inp["norm2_g"][i], 8) for i in range(4)])
    o["w_in"] = np.ascontiguousarray(inp["w_in"], np.float32)
    o["ident_in"] = np.eye(128, dtype=np.float32)
    cw = np.asarray(inp["lru_conv_w"], np.float32)
    o["lru_cw"] = np.ascontiguousarray(cw.reshape(4, 4, 4, 128).transpose(0, 3, 2, 1))
    o["lru_cb"] = np.ascontiguousarray(np.asarray(inp["lru_conv_b"], np.float32).reshape(4, 4, 128).transpose(0, 2, 1))
    for nm, key in (("lru_wa", "lru_w_a"), ("lru_wx", "lru_w_x")):
        w = np.asarray(inp[key], np.float32)
        bd = np.zeros((4, 2, 4, 128, 128), np.float32)
        for ch in range(4):
            bd[:, :, ch, 0:64, 0:64] = w[:, :, 2 * ch]
            bd[:, :, ch, 64:128, 64:128] = w[:, :, 2 * ch + 1]
        o[nm] = bd
    bal = np.stack([np.asarray(inp[k], np.float32) for k in ("lru_b_a", "lru_b_x", "lru_lam")], axis=1)
    o["lru_bal"] = np.ascontiguousarray(bal.reshape(4, 3, 2, 4, 128).transpose(0, 4, 1, 2, 3))
    return o


def prep_core(inp, core, n_samp=2):
    o = {}
    sl = slice(core * n_samp, (core + 1) * n_samp)
    o["x"] = np.ascontiguousarray(inp["x"][sl], np.float32)
    o["ctx"] = np.ascontiguousarray(inp["ctx"][sl], np.float32)
    cs = [np.asarray(inp["c"][core * n_samp + s], np.float32) for s in range(n_samp)]
    while len(cs) < 2:
        cs.append(cs[0])
    cs.append(np.asarray(inp["c_ctx"], np.float32))
    o["cvec"] = np.ascontiguousarray(np.stack([colform(c, 8) for c in cs], axis=-1))
    return o


TWO_PI = 6.283185307179586


def _s5_setup(self):
    es = self.ges
    S = self.n_samp
    self.s5_lam = self.inp("s5_lam", [4, 128, 3, 32])
    self.s5_B = self.inp("s5_B", [4, 2, 16, 128, 2, 128])
    self.s5_C = self.inp("s5_C", [4, 2, 16, 128, 2, 128])
    self.s5_dT = self.inp("s5_dT", [4, 128, 4])
    self.s5_st = self.sb(es, "s5_st", [128, 2, 32, S]); self.b_s5_st = Buf("s5_st")
    self.s5_par = self.sb(es, "s5_par", [128, 8, 32]); self.b_s5_par = Buf("s5_par")
    self.s5_W = self.sb(es, "s5_W", [128, 2, 32, 12]); self.b_s5_W = Buf("s5_W")


def _sincos(self, es, out_sin, out_cos, ang, shape, bufs_r, buf_w, tagn):
    nc = self.nc
    kf = self.sb(es, f"kf{tagn}", shape); ki = self.sb(es, f"ki{tagn}", shape, I32); rr = self.sb(es, f"rr{tagn}", shape)
    b = Buf()
    for out, shift in ((out_sin, 0.0), (out_cos, np.pi / 2)):
        self.V(lambda: nc.vector.tensor_scalar(kf[:], ang, shift, 1.0 / TWO_PI, ALU.add, ALU.mult), r=bufs_r, w=[b])
        self.V(lambda: nc.vector.tensor_copy(ki[:], kf[:]), r=[b], w=[b])
        self.V(lambda: nc.vector.tensor_copy(kf[:], ki[:]), r=[b], w=[b])
        self.V(lambda: nc.vector.tensor_scalar(rr[:], ang, shift, None, ALU.add), r=bufs_r + [b], w=[b])
        self.V(lambda: nc.vector.scalar_tensor_tensor(rr[:], kf[:], -TWO_PI, rr[:], ALU.mult, ALU.add), r=[b], w=[b])
        self.V(lambda: nc.vector.tensor_scalar(rr[:], rr[:], -np.pi, np.pi, ALU.max, ALU.min), r=[b], w=[b])
        self.A(lambda: nc.scalar.activation(out=out, in_=rr[:], func=AF.Sin), r=[b], w=[buf_w, b])


def _ph_s5_params(self, i):
    nc = self.nc
    with ExitStack() as es:
        lam = self.sb(es, "lam", [128, 3, 32]); b_lam = Buf()
        t = [self.sb(es, f"s5t{k}", [128, 32]) for k in range(8)]; bt = Buf()
        self.dma(lam[:], self.s5_lam[i], w=[b_lam])
        P = self.s5_par; bP = self.b_s5_par
        step, ar, th, cth, sth, den, nr = t[0], t[1], t[2], t[3], t[4], t[5], t[6]
        self.A(lambda: nc.scalar.activation(out=step[:], in_=lam[:, 2, :], func=AF.Exp), r=[b_lam], w=[bt])
        self.V(lambda: nc.vector.tensor_tensor(ar[:], lam[:, 0, :], step[:], ALU.mult), r=[b_lam, bt], w=[bt])
        self.V(lambda: nc.vector.tensor_tensor(th[:], lam[:, 1, :], step[:], ALU.mult), r=[b_lam, bt], w=[bt])
        self.A(lambda: nc.scalar.activation(out=P[:, 0, :], in_=ar[:], func=AF.Exp), r=[bt], w=[bP])
        _sincos(self, es, P[:, 2, :], P[:, 1, :], th[:], [128, 32], [bt], bP, "a")
        self.V(lambda: nc.vector.tensor_tensor(cth[:], P[:, 0, :], P[:, 1, :], ALU.mult), r=[bP], w=[bt])
        self.V(lambda: nc.vector.tensor_scalar(cth[:], cth[:], -1.0, None, ALU.add), r=[bt], w=[bt])
        self.V(lambda: nc.vector.tensor_tensor(sth[:], P[:, 0, :], P[:, 2, :], ALU.mult), r=[bP], w=[bt])
        self.V(lambda: nc.vector.tensor_tensor(den[:], lam[:, 0, :], lam[:, 0, :], ALU.mult), r=[b_lam], w=[bt])
        self.V(lambda: nc.vector.tensor_tensor(nr[:], lam[:, 1, :], lam[:, 1, :], ALU.mult), r=[b_lam], w=[bt])
        self.V(lambda: nc.vector.tensor_tensor(den[:], den[:], nr[:], ALU.add), r=[bt], w=[bt])
        self.V(lambda: nc.vector.reciprocal(den[:], den[:]), r=[bt], w=[bt])
        a, b2 = t[6], t[7]
        self.V(lambda: nc.vector.tensor_tensor(a[:], cth[:], lam[:, 0, :], ALU.mult), r=[bt, b_lam], w=[bt])
        self.V(lambda: nc.vector.tensor_tensor(b2[:], sth[:], lam[:, 1, :], ALU.mult), r=[bt, b_lam], w=[bt])
        self.V(lambda: nc.vector.tensor_tensor(a[:], a[:], b2[:], ALU.add), r=[bt], w=[bt])
        self.V(lambda: nc.vector.tensor_tensor(P[:, 3, :], a[:], den[:], ALU.mult), r=[bt], w=[bP])
        self.V(lambda: nc.vector.tensor_tensor(a[:], sth[:], lam[:, 0, :], ALU.mult), r=[bt, b_lam], w=[bt])
        self.V(lambda: nc.vector.tensor_tensor(b2[:], cth[:], lam[:, 1, :], ALU.mult), r=[bt, b_lam], w=[bt])
        self.V(lambda: nc.vector.tensor_tensor(a[:], a[:], b2[:], ALU.subtract), r=[bt], w=[bt])
        self.V(lambda: nc.vector.tensor_tensor(P[:, 4, :], a[:], den[:], ALU.mult), r=[bt], w=[bP])
        self.V(lambda: nc.vector.tensor_scalar(P[:, 5, :], P[:, 4, :], -1.0, None, ALU.mult), r=[bP], w=[bP])
        ang = self.sb(es, "angW", [128, 32, 12]); b_ang = Buf()
        for k in range(12):
            self.V(lambda: nc.vector.tensor_scalar(ang[:, :, k], th[:], float(2 ** k), None, ALU.mult), r=[bt], w=[b_ang])
        _sincos(self, es, self.s5_W[:, 1], self.s5_W[:, 0], ang[:], [128, 32, 12], [b_ang], self.b_s5_W, "w")
    self.fw.barrier()


def _ph_s5(self, i, kind):
    nc = self.nc
    sts = [self.streams[(kind, s)] for s in range(self.n_samp)]
    L = sts[0].L
    TBS = min(1024, L); NTS = L // TBS
    TB = min(512, L)
    NLV = int(np.log2(L))
    P = self.s5_par; bP = self.b_s5_par
    perm = (kind == "x")
    with ExitStack() as es:
        TC = self.sb(es, "TC", [128, L]); TS = self.sb(es, "TS", [128, L]); b_T = Buf()
        tmpT = self.sb(es, "tmpT", [128, L // 2]); b_tmpT = Buf()
        u = [self.sb(es, f"u{s}", [128, L]) for s in range(len(sts))]; b_u = [Buf() for _ in sts]
        yacc = [self.sb(es, f"yacc{s}", [128, L]) for s in range(len(sts))]; b_yacc = [Buf() for _ in sts]
        Bt = self.sb(es, "Bt", [128, 2, 2, 128]); b_Bt = Buf()
        Cf = self.sb(es, "Cf", [128, 2, 128]); b_Cf = Buf()
        Cb = self.sb(es, "Cb", [128, 2, 128], BF16); b_Cb = Buf()
        ctmp = self.sb(es, "ctmp", [128, 128]); b_ctmp = Buf()
        W4 = [self.sb(es, f"W4_{k}", [128, TBS]) for k in range(4)]; b_W4 = [Buf() for _ in range(4)]
        sb16 = self.sb(es, "sb16", [128, 2, TBS], BF16); b_sb16 = Buf()
        car = self.sb(es, "car", [128, 4]); b_car = Buf()
        ini = self.sb(es, "ini", [128, 4]); b_ini = Buf()
        dcol = self.sb(es, "dcol", [128, 4]); b_dcol = Buf()
        ybf = self.sb(es, "ybf", [128, L], BF16); b_ybf = Buf()
        pv = [self.ps(es, f"pv{k}", [128, 512]) for k in range(4)]; b_pv = [Buf() for _ in range(4)]
        py = [self.ps(es, f"py{k}", [128, 512]) for k in range(2)]; b_py = [Buf() for _ in range(2)]
        self.dma(dcol[:], self.s5_dT[i], w=[b_dcol])
        npv = 0; npy = 0
        for chunk in range(4):
            for s, st in enumerate(sts):
                self.dma(u[s][:], st.PJ[C_U + chunk * 128:C_U + (chunk + 1) * 128, :], r=[self.pjbuf(st, C_U + chunk * 128)], w=[b_u[s]])
            for gl4 in range(4):
                gp = chunk * 4 + gl4
                pr = slice(gl4 * 32, gl4 * 32 + 32)
                for d in range(2):
                    self.dma(Bt[:, d], self.s5_B[i, d, gp], w=[b_Bt])
                for d in range(2):
                    dg = d * 16 + gp
                    self.dma(Cf[:], self.s5_C[i, d, gp], w=[b_Cf])
                    cre, cim, ncim = P[:, 3, dg:dg + 1], P[:, 4, dg:dg + 1], P[:, 5, dg:dg + 1]
                    self.V(lambda: nc.vector.tensor_scalar(ctmp[:], Cf[:, 1, :], cim, None, ALU.mult), r=[b_Cf, bP], w=[b_ctmp])
                    self.V(lambda: nc.vector.scalar_tensor_tensor(Cb[:, 0, :], Cf[:, 0, :], cre, ctmp[:], ALU.mult, ALU.subtract),
                           r=[b_Cf, bP, b_ctmp], w=[b_Cb])
                    self.V(lambda: nc.vector.tensor_scalar(ctmp[:], Cf[:, 1, :], cre, None, ALU.mult), r=[b_Cf, bP], w=[b_ctmp])
                    self.V(lambda: nc.vector.scalar_tensor_tensor(Cb[:, 1, :], Cf[:, 0, :], ncim, ctmp[:], ALU.mult, ALU.subtract),
                           r=[b_Cf, bP, b_ctmp], w=[b_Cb])
                    self.V(lambda: nc.vector.memset(TC[:, 0:1], 1.0), w=[b_T])
                    self.V(lambda: nc.vector.memset(TS[:, 0:1], 0.0), w=[b_T])
                    for k in range(NLV):
                        n = 2 ** k
                        wr = self.s5_W[:, 0, dg, k:k + 1]; wi = self.s5_W[:, 1, dg, k:k + 1]
                        rW = [b_T, self.b_s5_W]
                        if k % 2 == 0:
                            self.V(lambda: nc.vector.tensor_scalar(tmpT[:, 0:n], TS[:, 0:n], wi, None, ALU.mult), r=rW, w=[b_tmpT])
                            self.V(lambda: nc.vector.scalar_tensor_tensor(TC[:, n:2 * n], TC[:, 0:n], wr, tmpT[:, 0:n], ALU.mult, ALU.subtract),
                                   r=rW + [b_tmpT], w=[b_T])
                            self.V(lambda: nc.vector.tensor_scalar(tmpT[:, 0:n], TC[:, 0:n], wi, None, ALU.mult), r=rW, w=[b_tmpT])
                            self.V(lambda: nc.vector.scalar_tensor_tensor(TS[:, n:2 * n], TS[:, 0:n], wr, tmpT[:, 0:n], ALU.mult, ALU.add),
                                   r=rW + [b_tmpT], w=[b_T])
                        else:
                            self.G(lambda: nc.gpsimd.tensor_scalar(tmpT[:, 0:n], TS[:, 0:n], wi, None, ALU.mult), r=rW, w=[b_tmpT])
                            self.G(lambda: nc.gpsimd.tensor_scalar(TC[:, n:2 * n], TC[:, 0:n], wr, None, ALU.mult), r=rW, w=[b_T])
                            self.G(lambda: nc.gpsimd.tensor_tensor(TC[:, n:2 * n], TC[:, n:2 * n], tmpT[:, 0:n], ALU.subtract), r=[b_T, b_tmpT], w=[b_T])
                            self.G(lambda: nc.gpsimd.tensor_scalar(tmpT[:, 0:n], TC[:, 0:n], wi, None, ALU.mult), r=rW, w=[b_tmpT])
                            self.G(lambda: nc.gpsimd.tensor_scalar(TS[:, n:2 * n], TS[:, 0:n], wr, None, ALU.mult), r=rW, w=[b_T])
                            self.G(lambda: nc.gpsimd.tensor_tensor(TS[:, n:2 * n], TS[:, n:2 * n], tmpT[:, 0:n], ALU.add), r=[b_T, b_tmpT], w=[b_T])
                    rho_b = P[:, 0, dg:dg + 1].to_broadcast([128, TBS])
                    for s, st in enumerate(sts):
                        if kind == "x":
                            hre = self.s5_st[:, 0, dg, s:s + 1]; him = self.s5_st[:, 1, dg, s:s + 1]
                            c1 = P[:, 1, dg:dg + 1]; s1 = P[:, 2, dg:dg + 1]
                            self.V(lambda: nc.vector.tensor_tensor(ini[:, 2:3], him, s1, ALU.mult), r=[self.b_s5_st, bP], w=[b_ini])
                            self.V(lambda: nc.vector.scalar_tensor_tensor(ini[:, 0:1], hre, c1, ini[:, 2:3], ALU.mult, ALU.subtract),
                                   r=[self.b_s5_st, bP, b_ini], w=[b_ini])
                            self.V(lambda: nc.vector.tensor_tensor(ini[:, 2:3], hre, s1, ALU.mult), r=[self.b_s5_st, bP], w=[b_ini])
                            self.V(lambda: nc.vector.scalar_tensor_tensor(ini[:, 1:2], him, c1, ini[:, 2:3], ALU.mult, ALU.add),
                                   r=[self.b_s5_st, bP, b_ini], w=[b_ini])
                        tbs_order = range(NTS) if d == 0 else range(NTS - 1, -1, -1)
                        for ti, tb in enumerate(tbs_order):
                            n0 = tb * TBS
                            if d == 0:
                                tc = TC[:, n0:n0 + TBS]; ts = TS[:, n0:n0 + TBS]
                            else:
                                o = L - 1 - n0
                                tc = bass.AP(TC, o, [[TC[:].ap[0][0], 128], [-1, TBS]])
                                ts = bass.AP(TS, o, [[TS[:].ap[0][0], 128], [-1, TBS]])
                            Vre, Vim, Abuf, Bbuf = W4
                            bVre, bVim, bA, bB = b_W4
                            for sb_ in range(TBS // TB):
                                c0 = n0 + sb_ * TB
                                if perm:
                                    w0 = c0 // 64
                                    rhs = bass.AP(u[s], w0, [[u[s][:].ap[0][0], 128], [1, TB // 64], [64, 64]])
                                else:
                                    rhs = u[s][:, c0:c0 + TB]
                                for ri, (dst, bdst) in enumerate(((Vre, bVre), (Vim, bVim))):
                                    k = npv % 4; npv += 1
                                    self.T(lambda: nc.tensor.matmul(pv[k][:, :TB], Bt[:, d, ri, :], rhs, start=True, stop=True),
                                           r=[b_Bt, b_u[s]], w=[b_pv[k]])
                                    self.A(lambda: nc.scalar.copy(dst[:, sb_ * TB:(sb_ + 1) * TB], pv[k][:, :TB]), r=[b_pv[k]], w=[bdst])
                            self.V(lambda: nc.vector.tensor_tensor(Abuf[:], Vre[:], tc, ALU.mult), r=[bVre, b_T], w=[bA])
                            self.G(lambda: nc.gpsimd.tensor_tensor(Bbuf[:], Vim[:], ts, ALU.mult), r=[bVim, b_T], w=[bB])
                            self.V(lambda: nc.vector.tensor_tensor(Abuf[:], Abuf[:], Bbuf[:], ALU.add), r=[bA, bB], w=[bA])
                            self.G(lambda: nc.gpsimd.tensor_tensor(Bbuf[:], Vim[:], tc, ALU.mult), r=[bVim, b_T], w=[bB])
                            self.V(lambda: nc.vector.tensor_tensor(Vre[:], Vre[:], ts, ALU.mult), r=[bVre, b_T], w=[bVre])
                            self.G(lambda: nc.gpsimd.tensor_tensor(Bbuf[:], Bbuf[:], Vre[:], ALU.subtract), r=[bB, bVre], w=[bB])
                            if ti == 0:
                                if kind == "x":
                                    i_re, i_im, rd = ini[:, 0:1], ini[:, 1:2], [b_ini]
                                else:
                                    i_re, i_im, rd = 0.0, 0.0, []
                            else:
                                i_re, i_im, rd = car[:, 0:1], car[:, 1:2], [b_car]
                            if d == 0:
                                rv = lambda t_: t_[:]
                                last = lambda t_: t_[:, TBS - 1:TBS]
                            else:
                                rv = lambda t_: bass.AP(t_, TBS - 1, [[t_[:].ap[0][0], 128], [-1, TBS]])
                                last = lambda t_: t_[:, 0:1]
                            self.V(lambda: nc.vector.tensor_tensor_scan(rv(Vim), rho_b, rv(Abuf), i_re, ALU.mult, ALU.add), r=[bA, bP] + rd, w=[bVim])
                            self.V(lambda: nc.vector.tensor_tensor_scan(rv(Vre), rho_b, rv(Bbuf), i_im, ALU.mult, ALU.add), r=[bB, bP] + rd, w=[bVre])
                            self.G(lambda: nc.gpsimd.tensor_copy(car[:, 0:1], last(Vim)), r=[bVim], w=[b_car])
                            self.G(lambda: nc.gpsimd.tensor_copy(car[:, 1:2], last(Vre)), r=[bVre], w=[b_car])
                            qre, qim, bqre, bqim = Vim, Vre, bVim, bVre
                            self.G(lambda: nc.gpsimd.tensor_tensor(Abuf[:], qim[:], ts, ALU.mult), r=[bqim, b_T], w=[bA])
                            self.V(lambda: nc.vector.tensor_tensor(Bbuf[:], qre[:], tc, ALU.mult), r=[bqre, b_T], w=[bB])
                            self.G(lambda: nc.gpsimd.tensor_tensor(sb16[:, 0, :], Bbuf[:], Abuf[:], ALU.subtract), r=[bA, bB], w=[b_sb16])
                            self.V(lambda: nc.vector.tensor_tensor(Abuf[:], qre[:], ts, ALU.mult), r=[bqre, b_T], w=[bA])
                            self.G(lambda: nc.gpsimd.tensor_tensor(Bbuf[:], qim[:], tc, ALU.mult), r=[bqim, b_T], w=[bB])
                            self.V(lambda: nc.vector.tensor_tensor(sb16[:, 1, :], Abuf[:], Bbuf[:], ALU.add), r=[bA, bB], w=[b_sb16])
                            if kind == "c" and ti == NTS - 1:
                                e = (TBS - 1) if d == 0 else 0
                                tce = TC[:, L - 1:L]; tse = TS[:, L - 1:L]
                                qr_e = qre[:, e:e + 1]; qi_e = qim[:, e:e + 1]
                                self.V(lambda: nc.vector.tensor_tensor(car[:, 2:3], qi_e, tse, ALU.mult), r=[bqim, b_T], w=[b_car])
                                self.V(lambda: nc.vector.scalar_tensor_tensor(self.s5_st[:, 0, dg, s:s + 1], qr_e, tce, car[:, 2:3], ALU.mult, ALU.subtract),
                                       r=[bqre, b_T, b_car], w=[self.b_s5_st])
                                self.V(lambda: nc.vector.tensor_tensor(car[:, 2:3], qr_e, tse, ALU.mult), r=[bqre, b_T], w=[b_car])
                                self.V(lambda: nc.vector.scalar_tensor_tensor(self.s5_st[:, 1, dg, s:s + 1], qi_e, tce, car[:, 2:3], ALU.mult, ALU.add),
                                       r=[bqim, b_T, b_car], w=[self.b_s5_st])
                            for sb_ in range(TBS // TB):
                                c0 = n0 + sb_ * TB
                                k = npy % 2; npy += 1
                                self.T(lambda: nc.tensor.matmul(py[k][:, :TB], Cb[:, 0, :], sb16[:, 0, sb_ * TB:(sb_ + 1) * TB], start=True, stop=False),
                                       r=[b_Cb, b_sb16], w=[b_py[k]])
                                self.T(lambda: nc.tensor.matmul(py[k][:, :TB], Cb[:, 1, :], sb16[:, 1, sb_ * TB:(sb_ + 1) * TB], start=False, stop=True),
                                       r=[b_Cb, b_sb16], w=[b_py[k]])
                                if d == 0:
                                    self.A(lambda: nc.scalar.copy(yacc[s][pr, c0:c0 + TB], py[k][pr, :TB]), r=[b_py[k]], w=[b_yacc[s]])
                                else:
                                    self.V(lambda: nc.vector.tensor_tensor(yacc[s][pr, c0:c0 + TB], yacc[s][pr, c0:c0 + TB], py[k][pr, :TB], ALU.add),
                                           r=[b_py[k], b_yacc[s]], w=[b_yacc[s]])
            for s, st in enumerate(sts):
                t1 = W4[0]; t2 = W4[1]
                ya = yacc[s]; uu = u[s]
                if perm:
                    ps_ = uu[:].ap[0][0]
                    nat = lambda t_: bass.AP(t_, 0, [[ps_, 128], [1, 64], [64, 64]])
                    seq = lambda t_: bass.AP(t_, 0, [[ps_, 128], [64, 64], [1, 64]])
                    self.V(lambda: nc.vector.scalar_tensor_tensor(seq(uu), seq(uu), dcol[:, chunk:chunk + 1], nat(ya), ALU.mult, ALU.add),
                           r=[b_u[s], b_yacc[s], b_dcol], w=[b_u[s]])
                else:
                    self.V(lambda: nc.vector.scalar_tensor_tensor(uu[:], uu[:], dcol[:, chunk:chunk + 1], ya[:], ALU.mult, ALU.add),
                           r=[b_u[s], b_yacc[s], b_dcol], w=[b_u[s]])
                self.G(lambda: nc.gpsimd.tensor_tensor(ya[:], uu[:], uu[:], ALU.mult), r=[b_u[s]], w=[b_yacc[s]])
                self.G(lambda: nc.gpsimd.tensor_scalar(ya[:], ya[:], 0.044715, 1.0, ALU.mult, ALU.add), r=[b_yacc[s]], w=[b_yacc[s]])
                self.G(lambda: nc.gpsimd.tensor_tensor(ya[:], ya[:], uu[:], ALU.mult), r=[b_yacc[s], b_u[s]], w=[b_yacc[s]])
                self.A(lambda: nc.scalar.activation(out=ya[:], in_=ya[:], func=AF.Sigmoid, scale=GELU_K), r=[b_yacc[s]], w=[b_yacc[s]])
                self.V(lambda: nc.vector.tensor_tensor(ybf[:], uu[:], ya[:], ALU.mult), r=[b_yacc[s], b_u[s]], w=[b_ybf])
                self.dma(st.ys5[chunk * 128:(chunk + 1) * 128, :], ybf[:], r=[b_ybf], w=[st.b_ys5[chunk]])
    self.fw.barrier()


KB.s5_setup = _s5_setup
KB.ph_s5_params = _ph_s5_params
KB.ph_s5 = _ph_s5


def prep_s5(inp):
    o = {}
    lam = np.zeros((4, 128, 3, 32), np.float32)
    Bm = np.zeros((4, 2, 16, 128, 2, 128), np.float32)
    Cm = np.zeros((4, 2, 16, 128, 2, 128), np.float32)
    lre, lim, lst = (np.asarray(inp[k], np.float32) for k in ("s5_lam_re", "s5_lam_im", "s5_log_step"))
    bre, bim, cre, cim = (np.asarray(inp[k], np.float32) for k in ("s5_b_re", "s5_b_im", "s5_c_re", "s5_c_im"))
    for d in range(2):
        for gp in range(16):
            dg = d * 16 + gp
            chunk, gl4 = gp // 4, gp % 4
            for gl in range(2):
                g = 2 * gp + gl
                rows = slice(gl * 64, gl * 64 + 64)
                lam[:, rows, 0, dg] = lre[:, d, g, :]
                lam[:, rows, 1, dg] = lim[:, d, g, :]
                lam[:, rows, 2, dg] = lst[:, d, g][:, None]
                r0 = gl4 * 32 + gl * 16
                Bm[:, d, gp, r0:r0 + 16, 0, rows] = bre[:, d, g].transpose(0, 2, 1)
                Bm[:, d, gp, r0:r0 + 16, 1, rows] = bim[:, d, g].transpose(0, 2, 1)
                Cm[:, d, gp, rows, 0, r0:r0 + 16] = cre[:, d, g].transpose(0, 2, 1)
                Cm[:, d, gp, rows, 1, r0:r0 + 16] = cim[:, d, g].transpose(0, 2, 1)
    o["s5_lam"] = lam; o["s5_B"] = Bm; o["s5_C"] = Cm
    o["s5_dT"] = np.stack([colform(inp["s5_d"][i], 4) for i in range(4)])
    return o


def _ssd_setup(self):
    es = self.ges
    S = self.n_samp
    self.m2_cw = self.inp("m2_cw", [4, 128, 12, 4])
    self.m2_cb = self.inp("m2_cb", [4, 128, 12])
    self.m2_row = self.inp("m2_row", [4, 1, 64])
    self.m2_drow = self.inp("m2_drow", [4, 1, 2048])
    self.ssd_const = self.inp("ssd_const", [128, 4, 128])
    self.ssd_c = self.sb(es, "ssd_c", [128, 4, 128]); self.b_ssd_c = Buf("ssd_c")
    self.dma(self.ssd_c[:], self.ssd_const, w=[self.b_ssd_c])
    self.ssd_st = self.sb(es, "ssd_st", [64, 2, S, 1024]); self.b_ssd_st = [[Buf() for _ in range(S)] for _ in range(2)]
    for st in self.streams.values():
        L = st.L
        st.xbc_tm = self.scratch(f"xbctm_{st.kind}{st.s}", [L, 1280])
        st.b_xbc_tm = [Buf() for _ in range(10)]
        st.bc_fm = self.scratch(f"bcfm_{st.kind}{st.s}", [512, L])
        st.b_bc_fm = [Buf() for _ in range(4)]
        st.z_tm = self.scratch(f"ztm_{st.kind}{st.s}", [L, 1024])
        st.b_z_tm = [Buf() for _ in range(st.NT)]
        st.dt_tm = self.scratch(f"dttm_{st.kind}{st.s}", [L, 32])
        st.b_dt_tm = Buf()
        st.yf = self.scratch(f"yf_{st.kind}{st.s}", [L, 1024])
        st.b_yf = [Buf() for _ in range(st.NT)]


def _ph_ssd(self, i, st):
    nc = self.nc
    L, NT = st.L, st.NT
    s = st.s
    C = self.ssd_c
    triU, triL, mbF, mbB = C[:, 0, :], C[:, 1, :], C[:, 2, :], C[:, 3, :]
    with ExitStack() as es:
        cw = self.sb(es, "m2cw", [128, 12, 4]); cb = self.sb(es, "m2cb", [128, 12]); b_cp = Buf()
        self.dma(cw[:], self.m2_cw[i], w=[b_cp]); self.dma(cb[:], self.m2_cb[i], w=[b_cp])
        xp = [self.sb(es, f"xp{k}", [128, L + 3]) for k in range(2)]; b_xp = [Buf() for _ in range(2)]
        acc = self.sb(es, "cacc", [128, L]); b_acc = Buf()
        act = [self.sb(es, f"cact{k}", [128, L]) for k in range(2)]; b_act = [Buf() for _ in range(2)]
        tms = self.sb(es, "tms", [128, NT, 128]); b_tms = Buf()
        pt = [self.ps(es, f"ptt{k}", [128, 4, 128]) for k in range(2)]; b_pt = [Buf() for _ in range(2)]
        for k in range(2):
            self.V(lambda: nc.vector.memset(xp[k][:, 0:2], 0.0), w=[b_xp[k]])
            self.V(lambda: nc.vector.memset(xp[k][:, L + 2:L + 3], 0.0), w=[b_xp[k]])
        npt = 0
        for ch in range(12):
            k = ch % 2
            r0 = C_XBC + ch * 128
            self.dma(xp[k][:, 2:L + 2], st.PJ[r0:r0 + 128, :], r=[self.pjbuf(st, r0)], w=[b_xp[k]])
            self.V(lambda: nc.vector.tensor_scalar(acc[:], xp[k][:, 0:L], cw[:, ch, 0:1], cb[:, ch:ch + 1], ALU.mult, ALU.add),
                   r=[b_xp[k], b_cp], w=[b_acc])
            for j in range(1, 4):
                self.V(lambda: nc.vector.scalar_tensor_tensor(acc[:], xp[k][:, j:j + L], cw[:, ch, j:j + 1], acc[:], ALU.mult, ALU.add),
                       r=[b_xp[k], b_cp, b_acc], w=[b_acc])
            self.A(lambda: nc.scalar.activation(out=act[k][:], in_=acc[:], func=AF.Silu), r=[b_acc], w=[b_act[k]])
            if ch >= 8:
                self.dma(st.bc_fm[(ch - 8) * 128:(ch - 7) * 128, :], act[k][:], r=[b_act[k]], w=[st.b_bc_fm[ch - 8]])
            if ch < 10:
                for t0 in range(0, NT, 4):
                    pk = npt % 2; npt += 1
                    nt4 = min(4, NT - t0)
                    for q in range(nt4):
                        t = t0 + q
                        self.T(lambda: nc.tensor.matmul(pt[pk][:, q, :], act[k][:, t * 128:(t + 1) * 128], self.ident_f[:], start=True, stop=True),
                               r=[b_act[k], self.b_const], w=[b_pt[pk]])
                    if npt % 2 == 0:
                        self.A(lambda: nc.scalar.copy(tms[:, t0:t0 + nt4, :], pt[pk][:, 0:nt4, :]), r=[b_pt[pk]], w=[b_tms])
                    else:
                        self.G(lambda: nc.gpsimd.tensor_copy(tms[:, t0:t0 + nt4, :], pt[pk][:, 0:nt4, :]), r=[b_pt[pk]], w=[b_tms]) if False else \
                            self.V(lambda: nc.vector.tensor_copy(tms[:, t0:t0 + nt4, :], pt[pk][:, 0:nt4, :]), r=[b_pt[pk]], w=[b_tms])
                self.dma(st.xbc_tm[:, ch * 128:(ch + 1) * 128].rearrange("(t p) c -> p t c", p=128), tms[:], r=[b_tms], w=[st.b_xbc_tm[ch]])
    self.fw.barrier()
    with ExitStack() as es:
        rowp = self.sb(es, "rowp", [128, 64]); b_rowp = Buf()
        drow = self.sb(es, "drow", [128, 2048]); b_drow = Buf()
        self.dma(rowp[:], self.m2_row[i].partition_broadcast(128), w=[b_rowp])
        self.dma(drow[:], self.m2_drow[i].partition_broadcast(128), w=[b_drow])
        self.A(lambda: nc.scalar.activation(out=rowp[:, 32:64], in_=rowp[:, 32:64], func=AF.Exp), r=[b_rowp], w=[b_rowp])
        self.V(lambda: nc.vector.tensor_scalar(rowp[:, 32:64], rowp[:, 32:64], -1.0, None, ALU.mult), r=[b_rowp], w=[b_rowp])
        DT = self.sb(es, "DT", [128, NT, 32]); b_DT = Buf()
        LA = self.sb(es, "LA", [128, NT, 32]); b_LA = Buf()
        self.dma(DT[:], st.dt_tm.rearrange("(t p) c -> p t c", p=128), r=[st.b_dt_tm], w=[b_DT])
        self.V(lambda: nc.vector.tensor_tensor(DT[:], DT[:], rowp[:, 0:32].unsqueeze(1).to_broadcast([128, NT, 32]), ALU.add), r=[b_DT, b_rowp], w=[b_DT])
        self.A(lambda: nc.scalar.activation(out=DT[:], in_=DT[:], func=AF.Exp), r=[b_DT], w=[b_DT])
        self.A(lambda: nc.scalar.activation(out=DT[:], in_=DT[:], func=AF.Ln, bias=1.0), r=[b_DT], w=[b_DT])
        self.V(lambda: nc.vector.tensor_tensor(LA[:], DT[:], rowp[:, 32:64].unsqueeze(1).to_broadcast([128, NT, 32]), ALU.mult), r=[b_DT, b_rowp], w=[b_LA])
        XT = [self.sb(es, f"XT{k}", [128, 1280]) for k in range(2)]; b_XT = [Buf() for _ in range(2)]
        BCf = [self.sb(es, f"BCf{k}", [64, 8, 128]) for k in range(2)]; b_BCf = [Buf() for _ in range(2)]
        acs = self.sb(es, "acs", [128, 48]); b_acs = Buf()
        din = self.sb(es, "din", [128, 16]); b_din = Buf()
        dch = self.sb(es, "dch", [64, 16]); b_dch = Buf()
        CBs = self.sb(es, "CBs", [128, 4, 128]); b_CBs = Buf()
        arg = self.sb(es, "arg", [128, 8, 128]); b_arg = Buf()
        MT = self.sb(es, "MT", [128, 8, 128], BF16); b_MT = Buf()
        Bd = self.sb(es, "Bd", [128, 16, 64], BF16); b_Bd = Buf()
        xs = self.sb(es, "xs", [128, 1024], BF16); b_xs = Buf()
        yt = self.sb(es, "yt", [128, 1024]); b_yt = Buf()
        yfl = self.sb(es, "yfl", [128, 1024]); b_yfl = Buf()
        zt = self.sb(es, "zt", [128, 1024]); b_zt = Buf()
        ybf = self.sb(es, "ybf16", [128, 1024], BF16); b_ybf = Buf()
        ssq = self.sb(es, "ssqm", [128, 2]); b_ssq = Buf()
        ystg = self.sb(es, "ystg", [128, 8, 512], BF16); b_ystg = Buf()
        etmp = self.sb(es, "etmp", [64, 512]); b_etmp = Buf()
        pc = self.ps(es, "pc", [128, 32]); b_pc = Buf()
        cbp = self.ps(es, "cbp", [128, 4, 128]); b_cbp = Buf()
        abc = self.ps(es, "abc", [128, 8, 128]); b_abc = Buf()
        yd = self.ps(es, "yd", [128, 512]); b_yd = Buf()
        yo = self.ps(es, "yo", [128, 512]); b_yo = Buf()
        stp = self.ps(es, "stp", [64, 512]); b_stp = Buf()
        ptr = self.ps(es, "ptr2", [128, 8, 128], BF16); b_ptr = Buf()
        grp = min(4, NT)
        for d in range(2):
            ent = self.ssd_st[:, d, s, :]
            b_ent = self.b_ssd_st[d][s]
            if st.kind == "c":
                self.V(lambda: nc.vector.memset(ent, 0.0), w=[b_ent])
            tri = triU if d == 0 else triL
            mb = mbF if d == 0 else mbB
            order = range(NT) if d == 0 else range(NT - 1, -1, -1)
            for ci, c in enumerate(order):
                k = ci % 2
                tok = slice(c * 128, (c + 1) * 128)
                self.dma(XT[k][:], st.xbc_tm[tok, :], r=st.b_xbc_tm, w=[b_XT[k]])
                self.dma(BCf[k][:], st.bc_fm[:, tok].rearrange("(k n) t -> n k t", n=64), r=st.b_bc_fm, w=[b_BCf[k]])
                la = LA[:, c, d * 16:(d + 1) * 16]
                dtv = DT[:, c, d * 16:(d + 1) * 16]
                self.T(lambda: nc.tensor.matmul(pc[:, 0:16], tri, la, start=True, stop=True), r=[self.b_ssd_c, b_LA], w=[b_pc])
                self.T(lambda: nc.tensor.matmul(pc[:, 16:32], self.ones_f[:], la, start=True, stop=True), r=[self.b_const, b_LA], w=[b_pc])
                for g in range(4):
                    self.T(lambda: nc.tensor.matmul(cbp[:, g, :], BCf[k][:, g, :], BCf[k][:, 4 + g, :], start=True, stop=True),
                           r=[b_BCf[k]], w=[b_cbp])
                self.V(lambda: nc.vector.tensor_copy(acs[:, 0:32], pc[:, 0:32]), r=[b_pc], w=[b_acs])
                self.A(lambda: nc.scalar.copy(CBs[:], cbp[:]), r=[b_cbp], w=[b_CBs])
                self.V(lambda: nc.vector.tensor_tensor(din[:], acs[:, 16:32], acs[:, 0:16], ALU.subtract), r=[b_acs], w=[b_din])
                self.A(lambda: nc.scalar.activation(out=din[:], in_=din[:], func=AF.Exp), r=[b_din], w=[b_din])
                self.A(lambda: nc.scalar.activation(out=acs[:, 32:48], in_=acs[:, 0:16], func=AF.Exp), r=[b_acs], w=[b_acs])
                self.A(lambda: nc.scalar.activation(out=dch[:], in_=acs[0:64, 16:32], func=AF.Exp), r=[b_acs], w=[b_dch])
                Btm = XT[k][:, 1024:1280].rearrange("p (g n) -> p g n", g=4).unsqueeze(2).to_broadcast([128, 4, 4, 64])
                self.V(lambda: nc.vector.tensor_tensor(Bd[:].rearrange("p (g j) n -> p g j n", g=4), Btm,
                                                       din[:].rearrange("p (g j) -> p g j", g=4).unsqueeze(3).to_broadcast([128, 4, 4, 64]), ALU.mult),
                       r=[b_XT[k], b_din], w=[b_Bd])
                self.G(lambda: nc.gpsimd.tensor_tensor(xs[:].rearrange("p (h c) -> p h c", h=16), XT[k][:, 0:1024].rearrange("p (h c) -> p h c", h=16),
                                                       dtv.unsqueeze(2).to_broadcast([128, 16, 64]), ALU.mult),
                       r=[b_XT[k], b_DT], w=[b_xs])
                for hh in range(2):
                    hs = slice(hh * 8, hh * 8 + 8)
                    for hq in range(8):
                        h = hh * 8 + hq
                        self.T(lambda: nc.tensor.matmul(abc[:, hq, :], LA[:, c, d * 16 + h:d * 16 + h + 1].to_broadcast([128, 128]), tri, start=True, stop=False),
                               r=[b_LA, self.b_ssd_c], w=[b_abc])
                        self.T(lambda: nc.tensor.matmul(abc[:, hq, :], self.ident_f[:], mb, start=False, stop=True),
                               r=[self.b_const, self.b_ssd_c], w=[b_abc])
                    self.V(lambda: nc.vector.tensor_tensor(arg[:], abc[:], acs[:, hh * 8:hh * 8 + 8].unsqueeze(2).to_broadcast([128, 8, 128]), ALU.subtract),
                           r=[b_abc, b_acs], w=[b_arg])
                    self.A(lambda: nc.scalar.activation(out=arg[:], in_=arg[:], func=AF.Exp), r=[b_arg], w=[b_arg])
                    self.G(lambda: nc.gpsimd.tensor_tensor(MT[:].rearrange("p (g j) q -> p g j q", g=2), arg[:].rearrange("p (g j) q -> p g j q", g=2),
                                                           CBs[:, hh * 2:hh * 2 + 2, :].unsqueeze(2).to_broadcast([128, 2, 4, 128]), ALU.mult),
                           r=[b_arg, b_CBs], w=[b_MT])
                    for hq in range(8):
                        h = hh * 8 + hq
                        self.T(lambda: nc.tensor.matmul(yd[:, hq * 64:(hq + 1) * 64], MT[:, hq, :], xs[:, h * 64:(h + 1) * 64], start=True, stop=True),
                               r=[b_MT, b_xs], w=[b_yd])
                    for gq in range(2):
                        g = hh * 2 + gq
                        self.T(lambda: nc.tensor.matmul(yo[:, gq * 256:(gq + 1) * 256], BCf[k][:, 4 + g, :], ent[:, g * 256:(g + 1) * 256], start=True, stop=True),
                               r=[b_BCf[k], b_ent], w=[b_yo])
                    for hq in range(8):
                        h = hh * 8 + hq
                        self.T(lambda: nc.tensor.matmul(stp[:, hq * 64:(hq + 1) * 64], Bd[:, h, :], xs[:, h * 64:(h + 1) * 64], start=True, stop=True),
                               r=[b_Bd, b_xs], w=[b_stp])
                    ysl = yt[:, hh * 512:(hh + 1) * 512]
                    self.V(lambda: nc.vector.tensor_tensor(ysl.rearrange("p (h c) -> p h c", h=8), yo[:].rearrange("p (h c) -> p h c", h=8),
                                                           acs[:, 32 + hh * 8:32 + hh * 8 + 8].unsqueeze(2).to_broadcast([128, 8, 64]), ALU.mult),
                           r=[b_yo, b_acs], w=[b_yt])
                    self.V(lambda: nc.vector.tensor_tensor(ysl, ysl, yd[:], ALU.add), r=[b_yt, b_yd], w=[b_yt])
                    esl = ent[:, hh * 512:(hh + 1) * 512]
                    self.V(lambda: nc.vector.tensor_tensor(etmp[:].rearrange("p (h c) -> p h c", h=8), esl.rearrange("p (h c) -> p h c", h=8),
                                                           dch[:, hs].unsqueeze(2).to_broadcast([64, 8, 64]), ALU.mult),
                           r=[b_ent, b_dch], w=[b_etmp])
                    self.V(lambda: nc.vector.tensor_tensor(esl, etmp[:], stp[:], ALU.add), r=[b_etmp, b_stp], w=[b_ent])
                if d == 0:
                    self.dma(st.yf[tok, :], yt[:], r=[b_yt], w=[st.b_yf[c]])
                else:
                    self.dma(yfl[:], st.yf[tok, :], r=[st.b_yf[c]], w=[b_yfl])
                    self.dma(zt[:], st.z_tm[tok, :], r=[st.b_z_tm[c]], w=[b_zt])
                    self.G(lambda: nc.gpsimd.tensor_tensor(yfl[:], yfl[:], yt[:], ALU.add), r=[b_yfl, b_yt], w=[b_yfl])
                    self.G(lambda: nc.gpsimd.tensor_tensor(yt[:], XT[k][:, 0:1024], drow[:, 0:1024], ALU.mult), r=[b_XT[k], b_drow], w=[b_yt])
                    self.G(lambda: nc.gpsimd.tensor_tensor(yfl[:], yfl[:], yt[:], ALU.add), r=[b_yfl, b_yt], w=[b_yfl])
                    self.A(lambda: nc.scalar.activation(out=zt[:], in_=zt[:], func=AF.Silu), r=[b_zt], w=[b_zt])
                    self.G(lambda: nc.gpsimd.tensor_tensor(yfl[:], yfl[:], zt[:], ALU.mult), r=[b_yfl, b_zt], w=[b_yfl])
                    self.A(lambda: nc.scalar.activation(out=zt[:], in_=yfl[:], func=AF.Square, accum_out=ssq[:, 0:1]), r=[b_yfl], w=[b_zt, b_ssq])
                    self.A(lambda: nc.scalar.activation(out=ssq[:, 1:2], in_=ssq[:, 0:1], func=AF.Sqrt, scale=1.0 / 1024, bias=1e-6), r=[b_ssq], w=[b_ssq])
                    self.V(lambda: nc.vector.reciprocal(ssq[:, 1:2], ssq[:, 1:2]), r=[b_ssq], w=[b_ssq])
                    self.V(lambda: nc.vector.scalar_tensor_tensor(ybf[:], yfl[:], ssq[:, 1:2], drow[:, 1024:2048], ALU.mult, ALU.mult),
                           r=[b_yfl, b_ssq, b_drow], w=[b_ybf])
                    for kc in range(8):
                        self.T(lambda: nc.tensor.transpose(ptr[:, kc, :], ybf[:, kc * 128:(kc + 1) * 128], self.ident_b[:]),
                               r=[b_ybf, self.b_const], w=[b_ptr])
                    q = c % grp
                    self.A(lambda: nc.scalar.copy(ystg[:, :, q * 128:(q + 1) * 128], ptr[:]), r=[b_ptr], w=[b_ystg])
                    if q == 0:
                        c0 = c * 128
                        self.dma(st.yssd[:, c0:c0 + grp * 128].rearrange("(kc p) t -> p kc t", p=128), ystg[:, :, 0:grp * 128],
                                 r=[b_ystg], w=st.b_yssd)
    self.fw.barrier()


KB.ssd_setup = _ssd_setup
KB.ph_ssd = _ph_ssd


def prep_ssd(inp):
    o = {}
    cw = np.asarray(inp["m2_conv_w"], np.float32)
    o["m2_cw"] = np.ascontiguousarray(cw.reshape(4, 4, 12, 128).transpose(0, 3, 2, 1))
    o["m2_cb"] = np.ascontiguousarray(np.asarray(inp["m2_conv_b"], np.float32).reshape(4, 12, 128).transpose(0, 2, 1))
    o["m2_row"] = np.ascontiguousarray(np.concatenate([np.asarray(inp["m2_dt_bias"], np.float32).reshape(4, 1, 32),
                                                       np.asarray(inp["m2_a_log"], np.float32).reshape(4, 1, 32)], axis=2))
    drep = np.repeat(np.asarray(inp["m2_d"], np.float32), 64, axis=1)
    o["m2_drow"] = np.ascontiguousarray(np.concatenate([drep, np.asarray(inp["m2_norm_g"], np.float32)], axis=1).reshape(4, 1, 2048))
    q = np.arange(128)
    triU = (q[:, None] <= q[None, :]).astype(np.float32)
    triL = (q[:, None] >= q[None, :]).astype(np.float32)
    mbF = np.where(q[None, :] >= q[:, None], 0.0, -30000.0).astype(np.float32)
    mbB = np.where(q[None, :] <= q[:, None], 0.0, -30000.0).astype(np.float32)
    o["ssd_const"] = np.ascontiguousarray(np.stack([triU, triL, mbF, mbB], axis=1))
    return o


def _m5_setup(self):
    self.s5_wglu = self.inp("s5_w_glu", [4, 512, 2048])
    self.m2_wout = self.inp("m2_w_out", [4, 1024, 1024])
    self.lru_wout = self.inp("lru_w_out", [4, 512, 1024])
    self.w_o = self.inp("w_o", [4, 1024, 1024])
    self.wg_bf = self.scratch("wg_bf", [128, 8, 3072], BF16)
    self.b_wg_bf = Buf("wg_bf")


def _ph_castgates(self, i):
    nc = self.nc
    with ExitStack() as es:
        wf = [self.sb(es, f"cgf{k}", [128, 8, 512]) for k in range(2)]; b_wf = [Buf() for _ in range(2)]
        wb = [self.sb(es, f"cgb{k}", [128, 8, 512], BF16) for k in range(2)]; b_wb = [Buf() for _ in range(2)]
        for n in range(6):
            k = n % 2
            c0 = C_G + n * 512
            self.dma(wf[k][:], self.w_in[i][:, c0:c0 + 512].rearrange("(kc p) n -> p kc n", p=128), w=[b_wf[k]])
            if k == 0:
                self.G(lambda: nc.gpsimd.tensor_copy(wb[k][:], wf[k][:]), r=[b_wf[k]], w=[b_wb[k]])
            else:
                self.A(lambda: nc.scalar.copy(wb[k][:], wf[k][:]), r=[b_wf[k]], w=[b_wb[k]])
            self.dma(self.wg_bf[:, :, n * 512:(n + 1) * 512], wb[k][:], r=[b_wb[k]], w=[self.b_wg_bf])
    self.fw.barrier()


def _load_cast(self, es, dst, b_dst, src_ap, nk, ncols, stg, b_stg, cnt):
    nc = self.nc
    for c0 in range(0, ncols, 512):
        k = cnt[0] % 2; cnt[0] += 1
        self.dma(stg[k][:, :nk, :], src_ap[:, c0:c0 + 512].rearrange("(kc p) n -> p kc n", p=128), w=[b_stg[k]])
        if k == 0:
            self.G(lambda: nc.gpsimd.tensor_copy(dst[:, :nk, c0:c0 + 512], stg[k][:, :nk, :]), r=[b_stg[k]], w=[b_dst])
        else:
            self.A(lambda: nc.scalar.copy(dst[:, :nk, c0:c0 + 512], stg[k][:, :nk, :]), r=[b_stg[k]], w=[b_dst])


def _ph_m5(self, i, kind):
    nc = self.nc
    sts = [self.streams[(kind, s)] for s in range(self.n_samp)]
    L = sts[0].L; TB = sts[0].TB; NB = sts[0].NB
    with ExitStack() as es:
        Wglu = self.sb(es, "Wglu", [128, 4, 2048], BF16); b_Wglu = Buf()
        Wm2 = self.sb(es, "Wm2", [128, 8, 1024], BF16); b_Wm2 = Buf()
        Wlru = self.sb(es, "Wlru", [128, 4, 1024], BF16); b_Wlru = Buf()
        Wo = self.sb(es, "Wo", [128, 8, 1024], BF16); b_Wo = Buf()
        with ExitStack() as es_w:
            stg = [self.sb(es_w, f"m5stg{k}", [128, 8, 512]) for k in range(2)]; b_stg = [Buf() for _ in range(2)]
            cnt = [0]
            _load_cast(self, es_w, Wglu, b_Wglu, self.s5_wglu[i], 4, 2048, stg, b_stg, cnt)
            _load_cast(self, es_w, Wm2, b_Wm2, self.m2_wout[i], 8, 1024, stg, b_stg, cnt)
            _load_cast(self, es_w, Wlru, b_Wlru, self.lru_wout[i], 4, 1024, stg, b_stg, cnt)
            _load_cast(self, es_w, Wo, b_Wo, self.w_o[i], 8, 1024, stg, b_stg, cnt)
            self.fw.barrier()
        Wg = [self.sb(es, f"Wg{k}", [128, 8, 3, 128], BF16) for k in range(2)]; b_Wg = [Buf() for _ in range(2)]
        hTb = self.sb(es, "hTb", [128, 8, TB], BF16); b_hTb = Buf()
        y5b = self.sb(es, "y5b", [128, 4, TB], BF16); b_y5b = Buf()
        ymb = self.sb(es, "ymb", [128, 8, TB], BF16); b_ymb = Buf()
        ylb = self.sb(es, "ylb", [128, 4, TB], BF16); b_ylb = Buf()
        gt = [self.sb(es, f"gt{k}", [128, TB]) for k in range(3)]; b_gt = [Buf() for _ in range(3)]
        sg = self.sb(es, "sgm5", [128, TB]); b_sg = Buf()
        mt = self.sb(es, "mt", [128, TB]); b_mt = Buf()
        tt = self.sb(es, "ttm5", [128, TB]); b_tt = Buf()
        mrg = self.sb(es, "mrg", [128, 8, TB], BF16); b_mrg = Buf()
        xt = [self.sb(es, f"xtm5{k}", [128, D]) for k in range(2)]; b_xt = [Buf() for _ in range(2)]
        tmp = self.sb(es, "tmpm5", [128, 512]); b_tmp = Buf()
        pp = [self.ps(es, f"pp{k}", [128, 512]) for k in range(8)]; b_pp = [Buf() for _ in range(8)]
        npp = [0]

        def nxt():
            k = npp[0] % 8; npp[0] += 1
            return pp[k], b_pp[k]

        ng = 0; nx = 0
        for st in sts:
            src = st.src if st.first else st.res
            gate_row = self.gb[:, st.v, 0, :]
            for tb in range(NB):
                ts_ = slice(tb * TB, (tb + 1) * TB)
                self.dma(hTb[:], st.hTs[:, :, ts_], r=[st.b_hTs], w=[b_hTb])
                self.dma(y5b[:], st.ys5[:, ts_].rearrange("(kc p) t -> p kc t", p=128), r=st.b_ys5, w=[b_y5b])
                self.dma(ymb[:], st.yssd[:, ts_].rearrange("(kc p) t -> p kc t", p=128), r=st.b_yssd, w=[b_ymb])
                self.dma(ylb[:], st.ylru[:, ts_].rearrange("(kc p) t -> p kc t", p=128), r=st.b_ylru, w=[b_ylb])
                for nch in range(8):
                    kg = ng % 2; ng += 1
                    ncs = slice(nch * 128, (nch + 1) * 128)
                    self.dma(Wg[kg][:], self.wg_bf.rearrange("p k (j n) -> p k j n", j=3)[:, :, :, ncs], r=[self.b_wg_bf], w=[b_Wg[kg]])
                    for j in range(3):
                        p_, bp_ = nxt()
                        for kc in range(8):
                            self.T(lambda: nc.tensor.matmul(p_[:, :TB], Wg[kg][:, kc, j, :], hTb[:, kc, :], start=(kc == 0), stop=(kc == 7)),
                                   r=[b_Wg[kg], b_hTb], w=[bp_])
                        self.A(lambda: nc.scalar.activation(out=gt[j][:], in_=p_[:, :TB], func=AF.Sigmoid), r=[bp_], w=[b_gt[j]])
                    pv_, bpv = nxt(); pg_, bpg = nxt()
                    for kc in range(4):
                        self.T(lambda: nc.tensor.matmul(pv_[:, :TB], Wglu[:, kc, ncs], y5b[:, kc, :], start=(kc == 0), stop=(kc == 3)),
                               r=[b_Wglu, b_y5b], w=[bpv])
                    for kc in range(4):
                        self.T(lambda: nc.tensor.matmul(pg_[:, :TB], Wglu[:, kc, 1024 + nch * 128:1024 + (nch + 1) * 128], y5b[:, kc, :],
                                                        start=(kc == 0), stop=(kc == 3)),
                               r=[b_Wglu, b_y5b], w=[bpg])
                    self.A(lambda: nc.scalar.activation(out=sg[:], in_=pg_[:, :TB], func=AF.Sigmoid), r=[bpg], w=[b_sg])
                    self.V(lambda: nc.vector.tensor_tensor(mt[:], pv_[:, :TB], sg[:], ALU.mult), r=[bpv, b_sg], w=[b_mt])
                    self.G(lambda: nc.gpsimd.tensor_tensor(mt[:], mt[:], gt[0][:], ALU.mult), r=[b_mt, b_gt[0]], w=[b_mt])
                    pb_, bpb = nxt()
                    for kc in range(8):
                        self.T(lambda: nc.tensor.matmul(pb_[:, :TB], Wm2[:, kc, ncs], ymb[:, kc, :], start=(kc == 0), stop=(kc == 7)),
                               r=[b_Wm2, b_ymb], w=[bpb])
                    self.V(lambda: nc.vector.tensor_tensor(tt[:], pb_[:, :TB], gt[1][:], ALU.mult), r=[bpb, b_gt[1]], w=[b_tt])
                    self.G(lambda: nc.gpsimd.tensor_tensor(mt[:], mt[:], tt[:], ALU.add), r=[b_mt, b_tt], w=[b_mt])
                    pc_, bpc = nxt()
                    for kc in range(4):
                        self.T(lambda: nc.tensor.matmul(pc_[:, :TB], Wlru[:, kc, ncs], ylb[:, kc, :], start=(kc == 0), stop=(kc == 3)),
                               r=[b_Wlru, b_ylb], w=[bpc])
                    self.V(lambda: nc.vector.tensor_tensor(tt[:], pc_[:, :TB], gt[2][:], ALU.mult), r=[bpc, b_gt[2]], w=[b_tt])
                    self.G(lambda: nc.gpsimd.tensor_tensor(mrg[:, nch, :], mt[:], tt[:], ALU.add), r=[b_mt, b_tt], w=[b_mrg])
                for q in range(TB // 128):
                    t = tb * (TB // 128) + q
                    kx = nx % 2; nx += 1
                    self.dma(xt[kx][:], src[t * 128:(t + 1) * 128, :], r=[st.b_res[t]], w=[b_xt[kx]])
                    for nb in range(2):
                        po_, bpo = nxt()
                        for kc in range(8):
                            self.T(lambda: nc.tensor.matmul(po_[:], mrg[:, kc, q * 128:(q + 1) * 128], Wo[:, kc, nb * 512:(nb + 1) * 512],
                                                            start=(kc == 0), stop=(kc == 7)),
                                   r=[b_mrg, b_Wo], w=[bpo])
                        self.V(lambda: nc.vector.tensor_tensor(tmp[:], po_[:], gate_row[:, nb * 512:(nb + 1) * 512], ALU.mult), r=[bpo, self.b_gb], w=[b_tmp])
                        self.G(lambda: nc.gpsimd.tensor_tensor(xt[kx][:, nb * 512:(nb + 1) * 512], xt[kx][:, nb * 512:(nb + 1) * 512], tmp[:], ALU.add),
                               r=[b_xt[kx], b_tmp], w=[b_xt[kx]])
                    self.dma(st.res[t * 128:(t + 1) * 128, :], xt[kx][:], r=[b_xt[kx]], w=[st.b_res[t]])
            st.first = False
    self.fw.barrier()


KB.m5_setup = _m5_setup
KB.ph_castgates = _ph_castgates
KB.ph_m5 = _ph_m5


def _moe_setup(self):
    self.w_router = self.inp("w_routerT", [4, 128, 8, 16])
    self.moe_w1 = self.inp("moe_w1", [4, 16, 1024, 1024])
    self.moe_w3 = self.inp("moe_w3", [4, 16, 1024, 1024])
    self.moe_w2 = self.inp("moe_w2", [4, 16, 1024, 1024])
    self.iota_in = self.inp("iota_c", [128, 4])
    self.reg_bc = {256: self.nc.gpsimd.to_reg(255), 4096: self.nc.gpsimd.to_reg(4095)}
    for st in self.streams.values():
        L = st.L
        st.h2 = self.scratch(f"h2_{st.kind}{st.s}", [L, 1024], BF16)
        st.b_h2 = Buf()
        st.affd = self.scratch(f"affd_{st.kind}{st.s}", [L, 16])
        st.b_affd = Buf()
        st.cum = self.scratch(f"cum_{st.kind}{st.s}", [16, L])
        st.b_cum = Buf()


def _rowbcast(self, es, dst, b_dst, col8, rbufs, pbank, b_pbank, dgs, b_dgs):
    nc = self.nc
    for half in range(2):
        for q in range(4):
            kc = half * 4 + q
            d = q % 2
            self.G(lambda: nc.gpsimd.tensor_scalar(dgs[d][:], self.ident_f[:], col8[:, kc:kc + 1], None, ALU.mult),
                   r=[self.b_const] + rbufs, w=[b_dgs[d]])
            self.T(lambda: nc.tensor.matmul(pbank[:, q * 128:(q + 1) * 128], self.ones_f[:], dgs[d][:], start=True, stop=True),
                   r=[self.b_const, b_dgs[d]], w=[b_pbank])
        self.A(lambda: nc.scalar.copy(dst[:, half * 512:(half + 1) * 512], pbank[:]), r=[b_pbank], w=[b_dst])


def _ph_moe(self, i, kind):
    nc = self.nc
    sts = [self.streams[(kind, s)] for s in range(self.n_samp)]
    L = sts[0].L; NT = sts[0].NT
    KCAP = L // 8
    NCT = max(1, KCAP // 128)
    with ExitStack() as es0:
        affT = self.sb(es0, "affT", [64, L]); b_affT = Buf()
        self.V(lambda: nc.vector.memset(affT[:], 0.0), w=[b_affT])
        with ExitStack() as es:
            Wr = self.sb(es, "Wr", [128, 8, 16]); b_Wr = Buf()
            self.dma(Wr[:], self.w_router[i], w=[b_Wr])
            scrow = self.sb(es, "scrow", [128, D]); shrow = self.sb(es, "shrow", [128, D]); b_rows = Buf()
            dgs = [self.sb(es, f"dgs{k}", [128, 128]) for k in range(2)]; b_dgs = [Buf() for _ in range(2)]
            pbk = self.ps(es, "pbk", [128, 512]); b_pbk = Buf()
            xt = [self.sb(es, f"xte{k}", [128, D]) for k in range(2)]; b_xt = [Buf() for _ in range(2)]
            h2b = [self.sb(es, f"h2b{k}", [128, D], BF16) for k in range(2)]; b_h2b = [Buf() for _ in range(2)]
            junk = self.sb(es, "junke", [128, D]); b_junk = Buf()
            ssq = [self.sb(es, f"ssqe{k}", [128, 2]) for k in range(2)]; b_ssq = [Buf() for _ in range(2)]
            h2T = self.sb(es, "h2T", [128, 8, 128]); b_h2T = Buf()
            ptr = [self.ps(es, f"ptre{k}", [128, 4, 128]) for k in range(2)]; b_ptr = [Buf() for _ in range(2)]
            plg = self.ps(es, "plg", [128, 16]); b_plg = Buf()
            paf = self.ps(es, "paf", [64, 512]); b_paf = Buf()
            AFF = self.sb(es, "AFF", [128, NT, 16]); b_AFF = Buf()
            sm = self.sb(es, "sm", [128, 4]); b_sm = Buf()
            ex = self.sb(es, "ex", [128, 16]); b_ex = Buf()
            for s, st in enumerate(sts):
                _rowbcast(self, es, scrow, b_rows, self.nsc[:, st.v, 2, :], [self.b_nsc], pbk, b_pbk, dgs, b_dgs)
                _rowbcast(self, es, shrow, b_rows, self.nsc[:, st.v, 3, :], [self.b_nsc], pbk, b_pbk, dgs, b_dgs)
                for t in range(NT):
                    k = t % 2
                    self.dma(xt[k][:], st.res[t * 128:(t + 1) * 128, :], r=[st.b_res[t]], w=[b_xt[k]])
                    self.A(lambda: nc.scalar.activation(out=junk[:], in_=xt[k][:], func=AF.Square, accum_out=ssq[k][:, 0:1]),
                           r=[b_xt[k]], w=[b_junk, b_ssq[k]])
                    self.A(lambda: nc.scalar.activation(out=ssq[k][:, 1:2], in_=ssq[k][:, 0:1], func=AF.Sqrt, scale=1.0 / D, bias=1e-6),
                           r=[b_ssq[k]], w=[b_ssq[k]])
                    self.V(lambda: nc.vector.reciprocal(ssq[k][:, 1:2], ssq[k][:, 1:2]), r=[b_ssq[k]], w=[b_ssq[k]])
                    self.V(lambda: nc.vector.scalar_tensor_tensor(xt[k][:], xt[k][:], ssq[k][:, 1:2], scrow[:], ALU.mult, ALU.mult),
                           r=[b_xt[k], b_ssq[k], b_rows], w=[b_xt[k]])
                    self.G(lambda: nc.gpsimd.tensor_tensor(xt[k][:], xt[k][:], shrow[:], ALU.add), r=[b_xt[k], b_rows], w=[b_xt[k]])
                    self.A(lambda: nc.scalar.copy(h2b[k][:], xt[k][:]), r=[b_xt[k]], w=[b_h2b[k]])
                    self.dma(st.h2[t * 128:(t + 1) * 128, :], h2b[k][:], r=[b_h2b[k]], w=[st.b_h2])
                    for half in range(2):
                        for q in range(4):
                            kc = half * 4 + q
                            self.T(lambda: nc.tensor.matmul(ptr[half][:, q, :], xt[k][:, kc * 128:(kc + 1) * 128], self.ident_f[:], start=True, stop=True),
                                   r=[b_xt[k], self.b_const], w=[b_ptr[half]])
                        if half == 0:
                            self.V(lambda: nc.vector.tensor_copy(h2T[:, 0:4, :], ptr[0][:]), r=[b_ptr[0]], w=[b_h2T])
                        else:
                            self.A(lambda: nc.scalar.copy(h2T[:, 4:8, :], ptr[1][:]), r=[b_ptr[1]], w=[b_h2T])
                    for kc in range(8):
                        self.T(lambda: nc.tensor.matmul(plg[:], h2T[:, kc, :], Wr[:, kc, :], start=(kc == 0), stop=(kc == 7)),
                               r=[b_h2T, b_Wr], w=[b_plg])
                    self.V(lambda: nc.vector.reduce_max(sm[:, 0:1], plg[:], axis=AX.X), r=[b_plg], w=[b_sm])
                    self.V(lambda: nc.vector.tensor_scalar(sm[:, 1:2], sm[:, 0:1], -1.0, None, ALU.mult), r=[b_sm], w=[b_sm])
                    self.A(lambda: nc.scalar.activation(out=ex[:], in_=plg[:], func=AF.Exp, bias=sm[:, 1:2], accum_out=sm[:, 2:3]),
                           r=[b_plg, b_sm], w=[b_ex, b_sm])
                    self.V(lambda: nc.vector.reciprocal(sm[:, 3:4], sm[:, 2:3]), r=[b_sm], w=[b_sm])
                    self.V(lambda: nc.vector.tensor_scalar(AFF[:, t, :], ex[:], sm[:, 3:4], None, ALU.mult), r=[b_ex, b_sm], w=[b_AFF])
                self.dma(st.affd.rearrange("(t p) e -> p t e", p=128), AFF[:], r=[b_AFF], w=[st.b_affd])
                TBt = min(4, NT)
                for t0 in range(0, NT, TBt):
                    for q in range(TBt):
                        self.T(lambda: nc.tensor.matmul(paf[s * 32:s * 32 + 16, q * 128:(q + 1) * 128], AFF[:, t0 + q, :], self.ident_f[:], start=True, stop=True),
                               r=[b_AFF, self.b_const], w=[b_paf])
                    self.V(lambda: nc.vector.tensor_copy(affT[s * 32:s * 32 + 16, t0 * 128:(t0 + TBt) * 128], paf[s * 32:s * 32 + 16, 0:TBt * 128]),
                           r=[b_paf], w=[b_affT])
        self.fw.barrier()
        with ExitStack() as es:
            msk = self.sb(es, "msk", [64, L]); b_msk = Buf()
            cum = self.sb(es, "cumt", [64, L]); b_cumt = Buf()
            lh = self.sb(es, "lh", [64, 8]); b_lh = Buf()
            lo, hi, mid, cnt, ge, tmp = (lh[:, j:j + 1] for j in range(6))
            self.V(lambda: nc.vector.memset(lo, 0.0), w=[b_lh])
            self.V(lambda: nc.vector.memset(hi, 1.0), w=[b_lh])
            for it in range(34):
                self.V(lambda: nc.vector.tensor_tensor(mid, lo, hi, ALU.add), r=[b_lh], w=[b_lh])
                self.V(lambda: nc.vector.tensor_scalar(mid, mid, 0.5, None, ALU.mult), r=[b_lh], w=[b_lh])
                self.V(lambda: nc.vector.tensor_scalar(msk[:], affT[:], mid, None, ALU.is_ge), r=[b_affT, b_lh], w=[b_msk])
                self.V(lambda: nc.vector.reduce_sum(cnt, msk[:], axis=AX.X), r=[b_msk], w=[b_lh])
                self.V(lambda: nc.vector.tensor_scalar(ge, cnt, float(KCAP) - 0.5, None, ALU.is_ge), r=[b_lh], w=[b_lh])
                self.V(lambda: nc.vector.tensor_tensor(tmp, mid, lo, ALU.subtract), r=[b_lh], w=[b_lh])
                self.V(lambda: nc.vector.tensor_tensor(tmp, tmp, ge, ALU.mult), r=[b_lh], w=[b_lh])
                self.V(lambda: nc.vector.tensor_tensor(lo, lo, tmp, ALU.add), r=[b_lh], w=[b_lh])
                self.V(lambda: nc.vector.tensor_tensor(tmp, hi, mid, ALU.subtract), r=[b_lh], w=[b_lh])
                self.V(lambda: nc.vector.tensor_tensor(tmp, tmp, ge, ALU.mult), r=[b_lh], w=[b_lh])
                self.V(lambda: nc.vector.tensor_tensor(hi, mid, tmp, ALU.add), r=[b_lh], w=[b_lh])
            self.V(lambda: nc.vector.tensor_scalar(msk[:], affT[:], lo, None, ALU.is_ge), r=[b_affT, b_lh], w=[b_msk])
            self.V(lambda: nc.vector.tensor_tensor_scan(cum[:], self.ones_f[0:64, 0:1].to_broadcast([64, L]), msk[:], 0.0, ALU.mult, ALU.add),
                   r=[b_msk, self.b_const], w=[b_cumt])
            for s, st in enumerate(sts):
                self.dma(st.cum, cum[s * 32:s * 32 + 16, :], r=[b_cumt], w=[st.b_cum])
        self.fw.barrier()
    with ExitStack() as es:
        wst = [self.sb(es, f"wst{k}", [128, 8, 512]) for k in range(2)]; b_wst = [Buf() for _ in range(2)]
        W1 = self.sb(es, "W1", [128, 8, 1024], BF16); b_W1 = Buf()
        W3 = self.sb(es, "W3", [128, 8, 1024], BF16); b_W3 = Buf()
        W2 = self.sb(es, "W2", [128, 8, 1024], BF16); b_W2 = Buf()
        iot = self.sb(es, "iot", [128, 4]); b_iot = Buf()
        self.dma(iot[:], self.iota_in, w=[b_iot])
        cumb = self.sb(es, "cumb", [128, L]); b_cumb = Buf()
        cmpj = self.sb(es, "cmpj", [128, L]); b_cmpj = Buf()
        idxf = self.sb(es, "idxf", [128, 4]); b_idxf = Buf()
        idxi = self.sb(es, "idxi", [128, 4], I32); b_idxi = Buf()
        xg = [self.sb(es, f"xg{k}", [128, D], BF16) for k in range(2)]; b_xg = [Buf() for _ in range(2)]
        wts = self.sb(es, "wts", [128, 4, 16]); b_wts = Buf()
        xsT = self.sb(es, "xsT", [128, 8, NCT * 128], BF16); b_xsT = Buf()
        hidT = self.sb(es, "hidT", [128, 8, NCT * 128], BF16); b_hidT = Buf()
        sgl = self.sb(es, "sgl", [128, NCT * 128]); b_sgl = Buf()
        yo = [self.sb(es, f"yoe{k}", [128, D]) for k in range(2)]; b_yo = [Buf() for _ in range(2)]
        ptx = self.ps(es, "ptx", [128, 8, 128], BF16); b_ptx = Buf()
        pe_ = [self.ps(es, f"pex{k}", [128, 512]) for k in range(6)]; b_pe = [Buf() for _ in range(6)]
        b_scat = [Buf() for _ in sts]
        for k in range(2):
            self.V(lambda: nc.vector.memset(xg[k][:], 0.0), w=[b_xg[k]])
        self.V(lambda: nc.vector.memset(wts[:], 0.0), w=[b_wts])
        CN = NCT * 128
        npe = [0]

        def nxt():
            k = npe[0] % 6; npe[0] += 1
            return pe_[k], b_pe[k]

        cnt = [0]; nxg = 0; nyo = 0
        for e in range(16):
            _load_cast(self, es, W1, b_W1, self.moe_w1[i, e], 8, 1024, wst, b_wst, cnt)
            _load_cast(self, es, W3, b_W3, self.moe_w3[i, e], 8, 1024, wst, b_wst, cnt)
            _load_cast(self, es, W2, b_W2, self.moe_w2[i, e], 8, 1024, wst, b_wst, cnt)
            for s, st in enumerate(sts):
                self.dma(cumb[:], st.cum[e:e + 1, :].partition_broadcast(128), r=[st.b_cum], w=[b_cumb])
                for ct in range(NCT):
                    self.V(lambda: nc.vector.tensor_scalar(cmpj[:], cumb[:], iot[:, ct:ct + 1], None, ALU.is_le), r=[b_cumb, b_iot], w=[b_cmpj])
                    self.V(lambda: nc.vector.reduce_sum(idxf[:, ct:ct + 1], cmpj[:], axis=AX.X), r=[b_cmpj], w=[b_idxf])
                self.V(lambda: nc.vector.tensor_copy(idxi[:, 0:NCT], idxf[:, 0:NCT]), r=[b_idxf], w=[b_idxi])
                for ct in range(NCT):
                    kx = nxg % 2; nxg += 1
                    self.fw.idma(reads=[b_idxi, st.b_h2], writes=[b_xg[kx]],
                                 out=xg[kx][:], out_offset=None, in_=st.h2[:, :],
                                 in_offset=bass.IndirectOffsetOnAxis(ap=idxi[:, ct:ct + 1], axis=0),
                                 bounds_check=self.reg_bc[L], oob_is_err=False)
                    self.fw.idma(reads=[b_idxi, st.b_affd], writes=[b_wts],
                                 out=wts[:, ct, :], out_offset=None, in_=st.affd[:, :],
                                 in_offset=bass.IndirectOffsetOnAxis(ap=idxi[:, ct:ct + 1], axis=0),
                                 bounds_check=self.reg_bc[L], oob_is_err=False)
                    for kc in range(8):
                        self.T(lambda: nc.tensor.transpose(ptx[:, kc, :], xg[kx][:, kc * 128:(kc + 1) * 128], self.ident_b[:]),
                               r=[b_xg[kx], self.b_const], w=[b_ptx])
                    self.A(lambda: nc.scalar.copy(xsT[:, :, ct * 128:(ct + 1) * 128], ptx[:]), r=[b_ptx], w=[b_xsT])
                for fch in range(8):
                    fs = slice(fch * 128, (fch + 1) * 128)
                    p1, bp1 = nxt(); p3, bp3 = nxt()
                    for kc in range(8):
                        self.T(lambda: nc.tensor.matmul(p1[:, :CN], W1[:, kc, fs], xsT[:, kc, :], start=(kc == 0), stop=(kc == 7)),
                               r=[b_W1, b_xsT], w=[bp1])
                    for kc in range(8):
                        self.T(lambda: nc.tensor.matmul(p3[:, :CN], W3[:, kc, fs], xsT[:, kc, :], start=(kc == 0), stop=(kc == 7)),
                               r=[b_W3, b_xsT], w=[bp3])
                    self.A(lambda: nc.scalar.activation(out=sgl[:], in_=p1[:, :CN], func=AF.Silu), r=[bp1], w=[b_sgl])
                    self.V(lambda: nc.vector.tensor_tensor(hidT[:, fch, :], p3[:, :CN], sgl[:], ALU.mult), r=[bp3, b_sgl], w=[b_hidT])
                g2 = self.gb[:, st.v, 1, :]
                for ct in range(NCT):
                    ky = nyo % 2; nyo += 1
                    for nb in range(2):
                        po, bpo = nxt()
                        for fch in range(8):
                            self.T(lambda: nc.tensor.matmul(po[:], hidT[:, fch, ct * 128:(ct + 1) * 128], W2[:, fch, nb * 512:(nb + 1) * 512],
                                                            start=(fch == 0), stop=(fch == 7)),
                                   r=[b_hidT, b_W2], w=[bpo])
                        self.V(lambda: nc.vector.scalar_tensor_tensor(yo[ky][:, nb * 512:(nb + 1) * 512], po[:], wts[:, ct, e:e + 1],
                                                                      g2[:, nb * 512:(nb + 1) * 512], ALU.mult, ALU.mult),
                               r=[bpo, b_wts, self.b_gb], w=[b_yo[ky]])
                    self.fw.idma(reads=[b_idxi, b_yo[ky]], writes=[b_scat[s]],
                                 out=st.res[:, :], out_offset=bass.IndirectOffsetOnAxis(ap=idxi[:, ct:ct + 1], axis=0),
                                 in_=yo[ky][:], in_offset=None, bounds_check=self.reg_bc[L], oob_is_err=False, compute_op=ALU.add)
    self.fw.barrier()


def _ph_final(self, st, out_ap, fng_in):
    nc = self.nc
    with ExitStack() as es:
        grow = self.sb(es, "fgrow", [128, D]); b_grow = Buf()
        self.dma(grow[:], fng_in.partition_broadcast(128), w=[b_grow])
        xt = [self.sb(es, f"xtf{k}", [128, D]) for k in range(3)]; b_xt = [Buf() for _ in range(3)]
        junk = self.sb(es, "junkf", [128, D]); b_junk = Buf()
        ssq = [self.sb(es, f"ssqf{k}", [128, 2]) for k in range(3)]; b_ssq = [Buf() for _ in range(3)]
        for t in range(st.NT):
            k = t % 3
            self.dma(xt[k][:], st.res[t * 128:(t + 1) * 128, :], r=[st.b_res[t]], w=[b_xt[k]])
            self.A(lambda: nc.scalar.activation(out=junk[:], in_=xt[k][:], func=AF.Square, accum_out=ssq[k][:, 0:1]), r=[b_xt[k]], w=[b_junk, b_ssq[k]])
            self.A(lambda: nc.scalar.activation(out=ssq[k][:, 1:2], in_=ssq[k][:, 0:1], func=AF.Sqrt, scale=1.0 / D, bias=1e-6), r=[b_ssq[k]], w=[b_ssq[k]])
            self.V(lambda: nc.vector.reciprocal(ssq[k][:, 1:2], ssq[k][:, 1:2]), r=[b_ssq[k]], w=[b_ssq[k]])
            self.V(lambda: nc.vector.scalar_tensor_tensor(xt[k][:], xt[k][:], ssq[k][:, 1:2], grow[:], ALU.mult, ALU.mult),
                   r=[b_xt[k], b_ssq[k], b_grow], w=[b_xt[k]])
            self.dma(out_ap[t * 128:(t + 1) * 128, :], xt[k][:], r=[b_xt[k]])
    self.fw.barrier()


KB.moe_setup = _moe_setup
KB.ph_moe = _ph_moe
KB.ph_final = _ph_final


def prep_moe(inp):
    o = {}
    wr = np.asarray(inp["moe_w_router"], np.float32)
    o["w_routerT"] = np.ascontiguousarray(wr.reshape(4, 8, 128, 16).transpose(0, 2, 1, 3))
    for k in ("moe_w1", "moe_w3", "moe_w2"):
        o[k] = np.ascontiguousarray(inp[k], np.float32)
    o["iota_c"] = np.ascontiguousarray((np.arange(128)[:, None] + 128 * np.arange(4)[None, :]).astype(np.float32))
    return o


def _layer(self, i, last=False):
    S = self.n_samp
    self.ph_mod(i)
    self.ph_s5_params(i)
    self.ph_castgates(i)
    kinds = ("c", "x")
    for kind in kinds:
        for s in range(S):
            self.ph_norm_proj(i, self.streams[(kind, s)])
    for kind in kinds:
        self.ph_lru(i, kind)
    for kind in kinds:
        self.ph_s5(i, kind)
    for s in range(S):
        for kind in kinds:
            self.ph_ssd(i, self.streams[(kind, s)])
    for kind in kinds:
        if kind == "c" and last:
            continue
        self.ph_m5(i, kind)
        self.ph_moe(i, kind)


KB.layer = _layer


def prep_all(inp):
    o = prep_common(inp)
    o.update(prep_s5(inp)); o.update(prep_ssd(inp)); o.update(prep_moe(inp))
    for k in ("s5_w_glu", "m2_w_out", "lru_w_out", "w_o"):
        o[k] = np.ascontiguousarray(inp[k], np.float32)
    o["fng"] = np.ascontiguousarray(np.asarray(inp["final_norm_g"], np.float32).reshape(1, 1024))
    return o


from concourse.bass_utils import run_bass_kernel_spmd

N_CORES = 8
N_SAMP = 2


def build_program():
    nc = bass.Bass("TRN2", target_bir_lowering=False)
    kb = KB(nc, n_samp=N_SAMP, dbg=False)
    kb.setup(); kb.s5_setup(); kb.ssd_setup(); kb.m5_setup(); kb.moe_setup()
    fng = kb.inp("fng", [1, D])
    out = nc.dram_tensor("out", [N_SAMP, 4096, D], F32, kind="ExternalOutput").ap()
    for i in range(4):
        kb.layer(i, last=(i == 3))
    for s in range(N_SAMP):
        kb.ph_final(kb.streams[("x", s)], out[s], fng)
    kb.fw.finish()
    return nc, kb


def kernel(**inputs):
    inp = {k: np.asarray(v) for k, v in inputs.items()}
    nc, kb = build_program()
    common = prep_all(inp)
    in_maps = []
    for core in range(N_CORES):
        m = dict(common)
        m.update(prep_core(inp, core, N_SAMP))
        in_maps.append({k: m[k] for k in kb.din})
    res = run_bass_kernel_spmd(nc, in_maps, core_ids=list(range(N_CORES)))
    outs = [np.asarray(r["out"], dtype=np.float32) for r in res.results]
    return np.concatenate(outs, axis=0)
```

```python
import numpy as np
from contextlib import ExitStack
import concourse.bass as bass
import concourse.mybir as mybir

F32 = mybir.dt.float32
BF16 = mybir.dt.bfloat16
I32 = mybir.dt.int32
ALU = mybir.AluOpType
AF = mybir.ActivationFunctionType
AX = mybir.AxisListType


class Buf:
    __slots__ = ("name", "w", "r")

    def __init__(self, name=""):
        self.name = name
        self.w = None
        self.r = {}


class Eng:
    def __init__(self, fw, name, handle, sem, step=1):
        self.fw = fw
        self.name = name
        self.h = handle
        self.sem = sem
        self.count = 0
        self.step = step
        self.seen = {}


class FW:
    def __init__(self, nc, n_dma_sems=24, n_gdma_sems=8):
        self.nc = nc
        self.es = ExitStack()
        mk = lambda n: self.es.enter_context(nc.semaphore(n))
        self.pe = Eng(self, "pe", nc.tensor, mk("s_pe"))
        self.dve = Eng(self, "dve", nc.vector, mk("s_dve"))
        self.act = Eng(self, "act", nc.scalar, mk("s_act"))
        self.pool = Eng(self, "pool", nc.gpsimd, mk("s_pool"))
        self.sp = Eng(self, "sp", nc.sync, mk("s_sp"))
        self.engs = [self.pe, self.dve, self.act, self.pool, self.sp]
        self.dq = [Eng(self, f"dq{i}", None, mk(f"s_dq{i}"), step=16) for i in range(n_dma_sems)]
        self.gq = [Eng(self, f"gq{i}", None, mk(f"s_gq{i}"), step=16) for i in range(n_gdma_sems)]
        self.aq = [Eng(self, f"aq{i}", None, mk(f"s_aq{i}"), step=16) for i in range(8)]
        self.dq_i = 0
        self.gq_i = 0
        self.aq_i = 0
        self.n_wait = 0
        self.n_inst = 0

    def _wait(self, eng, e2, c, raw=False):
        if e2 is eng:
            if (not raw) or eng is self.pe or eng is self.sp or eng.count - c > 2:
                return
        if eng.seen.get(e2, 0) >= c:
            return
        eng.h.wait_ge(e2.sem, c)
        eng.seen[e2] = c
        self.n_wait += 1

    def _deps(self, eng, reads, writes):
        for b in reads:
            if b.w is not None:
                self._wait(eng, b.w[0], b.w[1], raw=True)
        for b in writes:
            if b.w is not None:
                self._wait(eng, *b.w)
            for e2, c in b.r.items():
                self._wait(eng, e2, c)

    def _mark(self, tok_eng, tok_c, reads, writes):
        for b in reads:
            if b.r.get(tok_eng, 0) < tok_c:
                b.r[tok_eng] = tok_c
        for b in writes:
            b.w = (tok_eng, tok_c)
            b.r = {}

    def op(self, eng, fn, reads=(), writes=()):
        self._deps(eng, reads, writes)
        inst = fn()
        eng.count += 1
        inst.then_inc(eng.sem, 1)
        self._mark(eng, eng.count, reads, writes)
        self.n_inst += 1
        return inst

    def dma(self, out, in_, reads=(), writes=(), q="sp", **kw):
        if q == "sp":
            issuer = self.sp; pool = self.dq; i = self.dq_i; self.dq_i = (i + 1) % len(pool)
        elif q == "act":
            issuer = self.act; pool = self.aq; i = self.aq_i; self.aq_i = (i + 1) % len(pool)
        else:
            issuer = self.pool; pool = self.gq; i = self.gq_i; self.gq_i = (i + 1) % len(pool)
        tok = pool[i]
        if tok.count > 0:
            self._wait(issuer, tok, tok.count)
        self._deps(issuer, reads, writes)
        inst = issuer.h.dma_start(out=out, in_=in_, **kw)
        tok.count += 16
        inst.then_inc(tok.sem, 16)
        self._mark(tok, tok.count, reads, writes)
        self.n_inst += 1
        return inst

    def idma(self, reads=(), writes=(), **kw):
        issuer = self.pool; pool = self.gq; i = self.gq_i; self.gq_i = (i + 1) % len(pool)
        tok = pool[i]
        if tok.count > 0:
            self._wait(issuer, tok, tok.count)
        self._deps(issuer, reads, writes)
        inst = issuer.h.indirect_dma_start(**kw)
        tok.count += 16
        inst.then_inc(tok.sem, 16)
        self._mark(tok, tok.count, reads, writes)
        self.n_inst += 1
        return inst

    def barrier(self):
        allq = self.engs + self.dq + self.gq + self.aq
        for e in self.engs:
            for e2 in allq:
                if e2 is not e and e2.count > 0:
                    self._wait(e, e2, e2.count)

    def finish(self):
        self.barrier()

    def close(self):
        self.es.close()


D = 1024
NPJ = 4128
C_U, C_Z, C_XBC, C_DT, C_XL, C_GL, C_G = 0, 512, 1536, 3072, 3104, 3616, 4128
GELU_K = 1.5957691216057308


class Stream:
    pass


class KB:
    def __init__(self, nc, n_samp=2, dbg=False):
        self.nc = nc
        self.fw = FW(nc)
        self.ges = ExitStack()
        self.n_samp = n_samp
        self.din = {}
        self.dbg = dbg
        self._uid = 0

    def inp(self, name, shape, dt=F32):
        t = self.nc.dram_tensor(name, list(shape), dt, kind="ExternalInput")
        self.din[name] = (tuple(shape), dt)
        return t.ap()

    def scratch(self, name, shape, dt=F32, out=False):
        kind = "ExternalOutput" if (out or self.dbg) else "Internal"
        return self.nc.dram_tensor(name, list(shape), dt, kind=kind).ap()

    def sb(self, es, name, shape, dt=F32):
        self._uid += 1
        return es.enter_context(self.nc.sbuf_tensor(f"{name}_{self._uid}", list(shape), dt))

    def ps(self, es, name, shape, dt=F32):
        self._uid += 1
        return es.enter_context(self.nc.psum_tensor(f"{name}_{self._uid}", list(shape), dt))

    def V(self, fn, r=(), w=()):
        return self.fw.op(self.fw.dve, fn, r, w)

    def A(self, fn, r=(), w=()):
        return self.fw.op(self.fw.act, fn, r, w)

    def G(self, fn, r=(), w=()):
        return self.fw.op(self.fw.pool, fn, r, w)

    def T(self, fn, r=(), w=()):
        return self.fw.op(self.fw.pe, fn, r, w)

    def dma(self, out, in_, r=(), w=(), q="sp", **kw):
        return self.fw.dma(out, in_, r, w, q=q, **kw)

    def setup(self):
        nc = self.nc
        es = self.ges
        S = self.n_samp
        self.x_in = self.inp("x", [S, 4096, D])
        self.c_in = self.inp("ctx", [S, 256, D])
        self.cvec_in = self.inp("cvec", [128, 8, 3])
        self.w_mod = self.inp("w_mod", [4, D, 6144])
        self.b_modT = self.inp("b_modT", [4, 128, 48])
        self.n1gT = self.inp("n1gT", [4, 128, 8])
        self.n2gT = self.inp("n2gT", [4, 128, 8])
        self.w_in = self.inp("w_in", [4, D, 7200])
        self.ident_in = self.inp("ident_in", [128, 128])
        self.lru_cw = self.inp("lru_cw", [4, 128, 4, 4])
        self.lru_cb = self.inp("lru_cb", [4, 128, 4])
        self.lru_wa = self.inp("lru_wa", [4, 2, 4, 128, 128])
        self.lru_wx = self.inp("lru_wx", [4, 2, 4, 128, 128])
        self.lru_bal = self.inp("lru_bal", [4, 128, 3, 2, 4])
        self.ident_f = self.sb(es, "ident_f", [128, 128]); self.b_const = Buf("const")
        self.ident_b = self.sb(es, "ident_b", [128, 128], BF16)
        self.ones_f = self.sb(es, "ones_f", [128, 128])
        self.dma(self.ident_f[:], self.ident_in, w=[self.b_const])
        self.V(lambda: nc.vector.tensor_copy(self.ident_b[:], self.ident_f[:]), r=[self.b_const], w=[self.b_const])
        self.V(lambda: nc.vector.memset(self.ones_f[:], 1.0), w=[self.b_const])
        self.scv = self.sb(es, "scv", [128, 8, 3]); self.b_scv = Buf("scv")
        self.modc = self.sb(es, "modc", [128, 48, 3]); self.b_modc = Buf("modc")
        self.nsc = self.sb(es, "nsc", [128, 3, 4, 8]); self.b_nsc = Buf("nsc")
        self.gb = self.sb(es, "gb", [128, 3, 2, D]); self.b_gb = Buf("gb")
        self.dma(self.scv[:], self.cvec_in, w=[self.b_scv])
        self.A(lambda: nc.scalar.activation(out=self.scv[:], in_=self.scv[:], func=AF.Silu), r=[self.b_scv], w=[self.b_scv])
        self.lru_st = self.sb(es, "lru_st", [128, 4, 2, S]); self.b_lru_st = Buf("lru_st")
        self.streams = {}
        for kind, L, src in (("c", 256, self.c_in), ("x", 4096, self.x_in)):
            for s in range(S):
                st = Stream()
                st.kind, st.s, st.L = kind, s, L
                st.NT = L // 128
                st.TB = min(512, L)
                st.NB = L // st.TB
                st.v = 2 if kind == "c" else s
                st.src = src[s]
                st.res = self.scratch(f"res_{kind}{s}", [L, D])
                st.b_res = [Buf() for _ in range(st.NT)]
                st.first = True
                st.PJ = self.scratch(f"pj_{kind}{s}", [NPJ, L])
                st.b_pj = {}
                st.hTs = self.scratch(f"hTs_{kind}{s}", [128, 8, L], BF16)
                st.b_hTs = Buf()
                st.ylru = self.scratch(f"ylru_{kind}{s}", [512, L], BF16)
                st.b_ylru = [Buf() for _ in range(4)]
                st.ys5 = self.scratch(f"ys5_{kind}{s}", [512, L], BF16)
                st.b_ys5 = [Buf() for _ in range(4)]
                st.yssd = self.scratch(f"yssd_{kind}{s}", [1024, L], BF16)
                st.b_yssd = [Buf() for _ in range(8)]
                self.streams[(kind, s)] = st

    def pjbuf(self, st, r0):
        if r0 not in st.b_pj:
            st.b_pj[r0] = Buf(f"pj{r0}")
        return st.b_pj[r0]

    def ph_mod(self, i):
        nc = self.nc
        with ExitStack() as es:
            wm = [self.sb(es, f"wm{k}", [128, 8, 512]) for k in range(2)]
            b_wm = [Buf() for _ in range(2)]
            pm = [self.ps(es, f"pmod{k}", [128, 4, 4]) for k in range(2)]
            b_pm = [Buf() for _ in range(2)]
            bm = self.sb(es, "bm", [128, 48]); b_bm = Buf()
            g12 = self.sb(es, "g12", [128, 2, 8]); b_g12 = Buf()
            self.dma(bm[:], self.b_modT[i], w=[b_bm])
            self.dma(g12[:, 0, :], self.n1gT[i], w=[b_g12])
            self.dma(g12[:, 1, :], self.n2gT[i], w=[b_g12])
            for nb in range(12):
                k = nb % 2
                self.dma(wm[k][:], self.w_mod[i][:, nb * 512:(nb + 1) * 512].rearrange("(kc p) n -> p kc n", p=128), w=[b_wm[k]])
                for j in range(4):
                    for kc in range(8):
                        self.T(lambda: nc.tensor.matmul(pm[k][:, j, 0:3], wm[k][:, kc, j * 128:(j + 1) * 128], self.scv[:, kc, :],
                                                        start=(kc == 0), stop=(kc == 7)),
                               r=[b_wm[k], self.b_scv], w=[b_pm[k]])
                for j in range(4):
                    jj = nb * 4 + j
                    self.V(lambda: nc.vector.tensor_scalar(self.modc[:, jj, :], pm[k][:, j, 0:3], bm[:, jj:jj + 1], None, ALU.add),
                           r=[b_pm[k], b_bm], w=[self.b_modc])
            for v in range(3):
                for (o, seg_sc, seg_sh, gi) in ((0, 1, 0, 0), (2, 4, 3, 1)):
                    self.V(lambda: nc.vector.scalar_tensor_tensor(self.nsc[:, v, o, :], self.modc[:, seg_sc * 8:seg_sc * 8 + 8, v], 1.0,
                                                                  g12[:, gi, :], ALU.add, ALU.mult),
                           r=[self.b_modc, b_g12], w=[self.b_nsc])
                    self.V(lambda: nc.vector.tensor_copy(self.nsc[:, v, o + 1, :], self.modc[:, seg_sh * 8:seg_sh * 8 + 8, v]),
                           r=[self.b_modc], w=[self.b_nsc])
            dg = [self.sb(es, f"dg{k}", [128, 128]) for k in range(2)]; b_dg = [Buf() for _ in range(2)]
            pb = [self.ps(es, f"pb{k}", [128, 512]) for k in range(2)]; b_pb = [Buf() for _ in range(2)]
            n = 0
            for v in range(3):
                for gi, seg in ((0, 2), (1, 5)):
                    for half in range(2):
                        k = n % 2; n += 1
                        for q in range(4):
                            kc = half * 4 + q
                            d = (n * 4 + q) % 2
                            self.G(lambda: nc.gpsimd.tensor_scalar(dg[d][:], self.ident_f[:], self.modc[:, seg * 8 + kc, v:v + 1], None, ALU.mult),
                                   r=[self.b_const, self.b_modc], w=[b_dg[d]])
                            self.T(lambda: nc.tensor.matmul(pb[k][:, q * 128:(q + 1) * 128], self.ones_f[:], dg[d][:], start=True, stop=True),
                                   r=[self.b_const, b_dg[d]], w=[b_pb[k]])
                        self.A(lambda: nc.scalar.copy(self.gb[:, v, gi, half * 512:(half + 1) * 512], pb[k][:]), r=[b_pb[k]], w=[self.b_gb])
        self.fw.barrier()

    def ph_norm_proj(self, i, st):
        nc = self.nc
        L, NT, TB, NB = st.L, st.NT, st.TB, st.NB
        src = st.src if st.first else st.res
        with ExitStack() as es:
            hT = self.sb(es, "hT", [128, 8, L], BF16)
            b_hT = [Buf() for _ in range(NT)]
            with ExitStack() as es1:
                xt = [self.sb(es1, f"xt{k}", [128, D]) for k in range(2)]; b_xt = [Buf() for _ in range(2)]
                xn = [self.sb(es1, f"xn{k}", [128, D], BF16) for k in range(2)]; b_xn = [Buf() for _ in range(2)]
                junk = self.sb(es1, "junk", [128, D], BF16); b_junk = Buf()
                ssq = [self.sb(es1, f"ssq{k}", [128, 1]) for k in range(2)]; b_ssq = [Buf() for _ in range(2)]
                ptr = [self.ps(es1, f"ptr{k}", [128, 8, 128], BF16) for k in range(2)]; b_ptr = [Buf() for _ in range(2)]
                for t in range(NT):
                    k = t % 2
                    self.dma(xt[k][:], src[t * 128:(t + 1) * 128, :], r=[st.b_res[t]], w=[b_xt[k]])
                    self.A(lambda: nc.scalar.activation(out=junk[:], in_=xt[k][:], func=AF.Square, accum_out=ssq[k][:]),
                           r=[b_xt[k]], w=[b_junk, b_ssq[k]])
                    self.A(lambda: nc.scalar.activation(out=ssq[k][:], in_=ssq[k][:], func=AF.Sqrt, scale=1.0 / D, bias=1e-6),
                           r=[b_ssq[k]], w=[b_ssq[k]])
                    self.V(lambda: nc.vector.reciprocal(ssq[k][:], ssq[k][:]), r=[b_ssq[k]], w=[b_ssq[k]])
                    self.A(lambda: nc.scalar.activation(out=xn[k][:], in_=xt[k][:], func=AF.Copy, scale=ssq[k][:, 0:1]),
                           r=[b_xt[k], b_ssq[k]], w=[b_xn[k]])
                    for kc in range(8):
                        self.T(lambda: nc.tensor.transpose(ptr[k][:, kc, :], xn[k][:, kc * 128:(kc + 1) * 128], self.ident_b[:]),
                               r=[b_xn[k], self.b_const], w=[b_ptr[k]])
                    for kc in range(8):
                        o = hT[:, kc, t * 128:(t + 1) * 128]
                        sc = self.nsc[:, st.v, 0, kc:kc + 1]; sh = self.nsc[:, st.v, 1, kc:kc + 1]
                        if kc % 2 == 0:
                            self.V(lambda: nc.vector.tensor_scalar(o, ptr[k][:, kc, :], sc, sh, ALU.mult, ALU.add),
                                   r=[b_ptr[k], self.b_nsc], w=[b_hT[t]])
                        else:
                            self.A(lambda: nc.scalar.activation(out=o, in_=ptr[k][:, kc, :], func=AF.Identity, scale=sc, bias=sh),
                                   r=[b_ptr[k], self.b_nsc], w=[b_hT[t]])
                self.dma(st.hTs, hT[:], r=b_hT, w=[st.b_hTs])
            wf = self.sb(es, "wf", [128, 8, 512]); b_wf = Buf()
            wb = [self.sb(es, f"wb{k}", [128, 8, 512], BF16) for k in range(2)]; b_wb = [Buf() for _ in range(2)]
            stg = [self.sb(es, f"stg{k}", [128, L]) for k in range(2)]; b_stg = [Buf() for _ in range(2)]
            pm = [self.ps(es, f"pm{k}", [128, 512]) for k in range(4)]; b_pm = [Buf() for _ in range(4)]
            zst = [self.sb(es, f"zst{k}", [128, 512]) for k in range(2)]; b_zst = [Buf() for _ in range(2)]
            dts = self.sb(es, "dts", [128, NT, 32]); b_dts = Buf()
            groups = [(c0, 512) for c0 in range(0, 3072, 512)] + [(3072, 32)] + [(c0, 512) for c0 in (3104, 3616)]
            n_ev = 0; n_st = 0
            for gi, (c0, ncol) in enumerate(groups):
                k = gi % 2
                self.dma(wf[:, :, :ncol], self.w_in[i][:, c0:c0 + ncol].rearrange("(kc p) n -> p kc n", p=128), w=[b_wf])
                self.G(lambda: nc.gpsimd.tensor_copy(wb[k][:, :, :ncol], wf[:, :, :ncol]), r=[b_wf], w=[b_wb[k]])
                tokmajor = hasattr(st, "z_tm") and c0 in (512, 1024, 3072)
                if tokmajor:
                    for t in range(NT):
                        pk = n_ev % 4
                        for kc in range(8):
                            self.T(lambda: nc.tensor.matmul(pm[pk][:, :ncol], hT[:, kc, t * 128:(t + 1) * 128], wb[k][:, kc, :ncol],
                                                            start=(kc == 0), stop=(kc == 7)),
                                   r=[b_wb[k], b_hT[t]], w=[b_pm[pk]])
                        if c0 == 3072:
                            self.V(lambda: nc.vector.tensor_copy(dts[:, t, :], pm[pk][:, :32]), r=[b_pm[pk]], w=[b_dts])
                        else:
                            zk = n_ev % 2
                            if n_ev % 2 == 0:
                                self.A(lambda: nc.scalar.copy(zst[zk][:], pm[pk][:, :512]), r=[b_pm[pk]], w=[b_zst[zk]])
                            else:
                                self.V(lambda: nc.vector.tensor_copy(zst[zk][:], pm[pk][:, :512]), r=[b_pm[pk]], w=[b_zst[zk]])
                            self.dma(st.z_tm[t * 128:(t + 1) * 128, c0 - 512:c0], zst[zk][:], r=[b_zst[zk]], w=[st.b_z_tm[t]])
                        n_ev += 1
                    if c0 == 3072:
                        self.dma(st.dt_tm.rearrange("(t p) c -> p t c", p=128), dts[:], r=[b_dts], w=[st.b_dt_tm])
                    continue
                for r0 in range(0, ncol, 128):
                    m = min(128, ncol - r0)
                    sk = n_st % 2; n_st += 1
                    for tb in range(NB):
                        pk = n_ev % 4
                        tiles = range(tb * TB // 128, (tb + 1) * TB // 128)
                        for kc in range(8):
                            self.T(lambda: nc.tensor.matmul(pm[pk][:m, :TB], wb[k][:, kc, r0:r0 + m], hT[:, kc, tb * TB:(tb + 1) * TB],
                                                            start=(kc == 0), stop=(kc == 7)),
                                   r=[b_wb[k]] + [b_hT[t] for t in tiles], w=[b_pm[pk]])
                        if n_ev % 2 == 0:
                            self.A(lambda: nc.scalar.copy(stg[sk][:m, tb * TB:(tb + 1) * TB], pm[pk][:m, :TB]), r=[b_pm[pk]], w=[b_stg[sk]])
                        else:
                            self.V(lambda: nc.vector.tensor_copy(stg[sk][:m, tb * TB:(tb + 1) * TB], pm[pk][:m, :TB]), r=[b_pm[pk]], w=[b_stg[sk]])
                        n_ev += 1
                    self.dma(st.PJ[c0 + r0:c0 + r0 + m, :], stg[sk][:m, :], r=[b_stg[sk]], w=[self.pjbuf(st, c0 + r0)])
        self.fw.barrier()

    def ph_lru(self, i, kind):
        nc = self.nc
        sts = [self.streams[(kind, s)] for s in range(self.n_samp)]
        L = sts[0].L; TB = sts[0].TB; NB = sts[0].NB
        EPS1 = float(np.float32(1.0) - np.float32(1e-6))
        with ExitStack() as es:
            cw = self.sb(es, "cw", [128, 4, 4]); cb = self.sb(es, "cb", [128, 4]); b_cp = Buf()
            bal = self.sb(es, "bal", [128, 3, 2, 4]); b_bal = Buf()
            cc = self.sb(es, "cc", [128, 2, 2, 4]); b_cc = Buf()
            self.dma(cw[:], self.lru_cw[i], w=[b_cp]); self.dma(cb[:], self.lru_cb[i], w=[b_cp])
            self.dma(bal[:], self.lru_bal[i], w=[b_bal])
            self.A(lambda: nc.scalar.activation(out=cc[:, 0], in_=bal[:, 2], func=AF.Exp, scale=-1.0), r=[b_bal], w=[b_cc])
            self.A(lambda: nc.scalar.activation(out=cc[:, 0], in_=cc[:, 0], func=AF.Ln, bias=1.0), r=[b_cc], w=[b_cc])
            self.V(lambda: nc.vector.tensor_scalar(cc[:, 1], cc[:, 0], -16.0, None, ALU.mult), r=[b_cc], w=[b_cc])
            self.V(lambda: nc.vector.tensor_scalar(cc[:, 0], cc[:, 0], -8.0, None, ALU.mult), r=[b_cc], w=[b_cc])
            wa = self.sb(es, "wa", [128, 2, 2, 128]); b_wa = Buf()
            xlp = self.sb(es, "xlp", [128, L + 3]); b_xlp = Buf()
            gg = self.sb(es, "gg", [128, L]); b_gg = Buf()
            xc = self.sb(es, "xc", [128, L]); b_xc = Buf()
            hf = self.sb(es, "hf", [128, L]); b_hf = Buf()
            A1 = self.sb(es, "A1", [128, L]); b_A1 = Buf()
            A2 = self.sb(es, "A2", [128, L]); b_A2 = Buf()
            A3 = self.sb(es, "A3", [128, L]); b_A3 = Buf()
            yb = self.sb(es, "yb", [128, L], BF16); b_yb = Buf()
            pg = [self.ps(es, f"pg{k}", [128, 512]) for k in range(4)]; b_pg = [Buf() for _ in range(4)]
            self.V(lambda: nc.vector.memset(xlp[:, 0:2], 0.0), w=[b_xlp])
            self.V(lambda: nc.vector.memset(xlp[:, L + 2:L + 3], 0.0), w=[b_xlp])
            npg = 0
            for ch in range(4):
                for d in range(2):
                    self.dma(wa[:, d, 0, :], self.lru_wa[i, d, ch], w=[b_wa])
                    self.dma(wa[:, d, 1, :], self.lru_wx[i, d, ch], w=[b_wa])
                for st in sts:
                    r_xl = C_XL + ch * 128; r_gl = C_GL + ch * 128
                    self.dma(xlp[:, 2:L + 2], st.PJ[r_xl:r_xl + 128, :], r=[self.pjbuf(st, r_xl)], w=[b_xlp])
                    self.dma(gg[:], st.PJ[r_gl:r_gl + 128, :], r=[self.pjbuf(st, r_gl)], w=[b_gg])
                    self.V(lambda: nc.vector.tensor_scalar(xc[:], xlp[:, 0:L], cw[:, ch, 0:1], cb[:, ch:ch + 1], ALU.mult, ALU.add),
                           r=[b_xlp, b_cp], w=[b_xc])
                    for j in range(1, 4):
                        self.V(lambda: nc.vector.scalar_tensor_tensor(xc[:], xlp[:, j:j + L], cw[:, ch, j:j + 1], xc[:], ALU.mult, ALU.add),
                               r=[b_xlp, b_cp, b_xc], w=[b_xc])
                    self.G(lambda: nc.gpsimd.tensor_tensor(A1[:], gg[:], gg[:], ALU.mult), r=[b_gg], w=[b_A1])
                    self.G(lambda: nc.gpsimd.tensor_scalar(A1[:], A1[:], 0.044715, 1.0, ALU.mult, ALU.add), r=[b_A1], w=[b_A1])
                    self.G(lambda: nc.gpsimd.tensor_tensor(A1[:], A1[:], gg[:], ALU.mult), r=[b_A1, b_gg], w=[b_A1])
                    self.A(lambda: nc.scalar.activation(out=A1[:], in_=A1[:], func=AF.Sigmoid, scale=GELU_K), r=[b_A1], w=[b_A1])
                    self.G(lambda: nc.gpsimd.tensor_tensor(gg[:], gg[:], A1[:], ALU.mult), r=[b_A1, b_gg], w=[b_gg])
                    for d in range(2):
                        for tb in range(NB):
                            sl = slice(tb * TB, (tb + 1) * TB)
                            ka = npg % 4; kx = (npg + 1) % 4; npg += 2
                            self.T(lambda: nc.tensor.matmul(pg[ka][:, :TB], wa[:, d, 0, :], xc[:, sl], start=True, stop=True),
                                   r=[b_wa, b_xc], w=[b_pg[ka]])
                            self.T(lambda: nc.tensor.matmul(pg[kx][:, :TB], wa[:, d, 1, :], xc[:, sl], start=True, stop=True),
                                   r=[b_wa, b_xc], w=[b_pg[kx]])
                            self.A(lambda: nc.scalar.activation(out=A1[:, sl], in_=pg[ka][:, :TB], func=AF.Sigmoid, bias=bal[:, 0, d, ch:ch + 1]),
                                   r=[b_pg[ka], b_bal], w=[b_A1])
                            self.A(lambda: nc.scalar.activation(out=A3[:, sl], in_=pg[kx][:, :TB], func=AF.Sigmoid, bias=bal[:, 1, d, ch:ch + 1]),
                                   r=[b_pg[kx], b_bal], w=[b_A3])
                        self.A(lambda: nc.scalar.activation(out=A2[:], in_=A1[:], func=AF.Exp, scale=cc[:, 1, d, ch:ch + 1]), r=[b_A1, b_cc], w=[b_A2])
                        self.A(lambda: nc.scalar.activation(out=A1[:], in_=A1[:], func=AF.Exp, scale=cc[:, 0, d, ch:ch + 1]), r=[b_A1, b_cc], w=[b_A1])
                        self.V(lambda: nc.vector.tensor_scalar(A2[:], A2[:], EPS1, -1.0, ALU.min, ALU.mult), r=[b_A2], w=[b_A2])
                        self.A(lambda: nc.scalar.activation(out=A2[:], in_=A2[:], func=AF.Sqrt, bias=1.0), r=[b_A2], w=[b_A2])
                        self.G(lambda: nc.gpsimd.tensor_tensor(A3[:], A3[:], xc[:], ALU.mult), r=[b_A3, b_xc], w=[b_A3])
                        self.G(lambda: nc.gpsimd.tensor_tensor(A3[:], A3[:], A2[:], ALU.mult), r=[b_A3, b_A2], w=[b_A3])
                        if kind == "c":
                            init = 0.0; rd = []
                        else:
                            init = self.lru_st[:, ch, d, st.s:st.s + 1]; rd = [self.b_lru_st]
                        if d == 0:
                            self.V(lambda: nc.vector.tensor_tensor_scan(hf[:], A1[:], A3[:], init, ALU.mult, ALU.add),
                                   r=[b_A1, b_A3] + rd, w=[b_hf])
                            if kind == "c":
                                self.G(lambda: nc.gpsimd.tensor_copy(self.lru_st[:, ch, 0, st.s:st.s + 1], hf[:, L - 1:L]), r=[b_hf], w=[self.b_lru_st])
                        else:
                            rev = lambda t: bass.AP(t, L - 1, [[t[:].ap[0][0], 128], [-1, L]])
                            self.V(lambda: nc.vector.tensor_tensor_scan(rev(A2), rev(A1), rev(A3), init, ALU.mult, ALU.add),
                                   r=[b_A1, b_A3] + rd, w=[b_A2])
                            if kind == "c":
                                self.G(lambda: nc.gpsimd.tensor_copy(self.lru_st[:, ch, 1, st.s:st.s + 1], A2[:, 0:1]), r=[b_A2], w=[self.b_lru_st])
                    self.V(lambda: nc.vector.tensor_tensor(A2[:], A2[:], hf[:], ALU.add), r=[b_A2, b_hf], w=[b_A2])
                    self.V(lambda: nc.vector.tensor_tensor(yb[:], A2[:], gg[:], ALU.mult), r=[b_A2, b_gg], w=[b_yb])
                    self.dma(st.ylru[ch * 128:(ch + 1) * 128, :], yb[:], r=[b_yb], w=[st.b_ylru[ch]])
        self.fw.barrier()


def colform(v, nchunk):
    return np.ascontiguousarray(np.asarray(v, np.float32).reshape(nchunk, 128).T)


def prep_common(inp):
    o = {}
    o["w_mod"] = np.ascontiguousarray(inp["w_mod"], np.float32)
    o["b_modT"] = np.stack([colform(inp["b_mod"][i], 48) for i in range(4)])
    o["n1gT"] = np.stack([colform(inp["norm1_g"][i], 8) for i in range(4)])
    o["n2gT"] = np.stack([colform(inp["norm2_g"][i], 8) for i in range(4)])
    o["w_in"] = np.ascontiguousarray(inp["w_in"], np.float32)
    o["ident_in"] = np.eye(128, dtype=np.float32)
    cw = np.asarray(inp["lru_conv_w"], np.float32)
    o["lru_cw"] = np.ascontiguousarray(cw.reshape(4, 4, 4, 128).transpose(0, 3, 2, 1))
    o["lru_cb"] = np.ascontiguousarray(np.asarray(inp["lru_conv_b"], np.float32).reshape(4, 4, 128).transpose(0, 2, 1))
    for nm, key in (("lru_wa", "lru_w_a"), ("lru_wx", "lru_w_x")):
        w = np.asarray(inp[key], np.float32)
        bd = np.zeros((4, 2, 4, 128, 128), np.float32)
        for ch in range(4):
            bd[:, :, ch, 0:64, 0:64] = w[:, :, 2 * ch]
            bd[:, :, ch, 64:128, 64:128] = w[:, :, 2 * ch + 1]
        o[nm] = bd
    bal = np.stack([np.asarray(inp[k], np.float32) for k in ("lru_b_a", "lru_b_x", "lru_lam")], axis=1)
    o["lru_bal"] = np.ascontiguousarray(bal.reshape(4, 3, 2, 4, 128).transpose(0, 4, 1, 2, 3))
    return o


def prep_core(inp, core, n_samp=2):
    o = {}
    sl = slice(core * n_samp, (core + 1) * n_samp)
    o["x"] = np.ascontiguousarray(inp["x"][sl], np.float32)
    o["ctx"] = np.ascontiguousarray(inp["ctx"][sl], np.float32)
    cs = [np.asarray(inp["c"][core * n_samp + s], np.float32) for s in range(n_samp)]
    while len(cs) < 2:
        cs.append(cs[0])
    cs.append(np.asarray(inp["c_ctx"], np.float32))
    o["cvec"] = np.ascontiguousarray(np.stack([colform(c, 8) for c in cs], axis=-1))
    return o


TWO_PI = 6.283185307179586


def _s5_setup(self):
    es = self.ges
    S = self.n_samp
    self.s5_lam = self.inp("s5_lam", [4, 128, 3, 32])
    self.s5_B = self.inp("s5_B", [4, 2, 16, 128, 2, 128])
    self.s5_C = self.inp("s5_C", [4, 2, 16, 128, 2, 128])
    self.s5_dT = self.inp("s5_dT", [4, 128, 4])
    self.s5_st = self.sb(es, "s5_st", [128, 2, 32, S]); self.b_s5_st = Buf("s5_st")
    self.s5_par = self.sb(es, "s5_par", [128, 8, 32]); self.b_s5_par = Buf("s5_par")
    self.s5_W = self.sb(es, "s5_W", [128, 2, 32, 12]); self.b_s5_W = Buf("s5_W")


def _sincos(self, es, out_sin, out_cos, ang, shape, bufs_r, buf_w, tagn):
    nc = self.nc
    kf = self.sb(es, f"kf{tagn}", shape); ki = self.sb(es, f"ki{tagn}", shape, I32); rr = self.sb(es, f"rr{tagn}", shape)
    b = Buf()
    for out, shift in ((out_sin, 0.0), (out_cos, np.pi / 2)):
        self.V(lambda: nc.vector.tensor_scalar(kf[:], ang, shift, 1.0 / TWO_PI, ALU.add, ALU.mult), r=bufs_r, w=[b])
        self.V(lambda: nc.vector.tensor_copy(ki[:], kf[:]), r=[b], w=[b])
        self.V(lambda: nc.vector.tensor_copy(kf[:], ki[:]), r=[b], w=[b])
        self.V(lambda: nc.vector.tensor_scalar(rr[:], ang, shift, None, ALU.add), r=bufs_r + [b], w=[b])
        self.V(lambda: nc.vector.scalar_tensor_tensor(rr[:], kf[:], -TWO_PI, rr[:], ALU.mult, ALU.add), r=[b], w=[b])
        self.V(lambda: nc.vector.tensor_scalar(rr[:], rr[:], -np.pi, np.pi, ALU.max, ALU.min), r=[b], w=[b])
        self.A(lambda: nc.scalar.activation(out=out, in_=rr[:], func=AF.Sin), r=[b], w=[buf_w, b])


def _ph_s5_params(self, i):
    nc = self.nc
    with ExitStack() as es:
        lam = self.sb(es, "lam", [128, 3, 32]); b_lam = Buf()
        t = [self.sb(es, f"s5t{k}", [128, 32]) for k in range(8)]; bt = Buf()
        self.dma(lam[:], self.s5_lam[i], w=[b_lam])
        P = self.s5_par; bP = self.b_s5_par
        step, ar, th, cth, sth, den, nr = t[0], t[1], t[2], t[3], t[4], t[5], t[6]
        self.A(lambda: nc.scalar.activation(out=step[:], in_=lam[:, 2, :], func=AF.Exp), r=[b_lam], w=[bt])
        self.V(lambda: nc.vector.tensor_tensor(ar[:], lam[:, 0, :], step[:], ALU.mult), r=[b_lam, bt], w=[bt])
        self.V(lambda: nc.vector.tensor_tensor(th[:], lam[:, 1, :], step[:], ALU.mult), r=[b_lam, bt], w=[bt])
        self.A(lambda: nc.scalar.activation(out=P[:, 0, :], in_=ar[:], func=AF.Exp), r=[bt], w=[bP])
        _sincos(self, es, P[:, 2, :], P[:, 1, :], th[:], [128, 32], [bt], bP, "a")
        self.V(lambda: nc.vector.tensor_tensor(cth[:], P[:, 0, :], P[:, 1, :], ALU.mult), r=[bP], w=[bt])
        self.V(lambda: nc.vector.tensor_scalar(cth[:], cth[:], -1.0, None, ALU.add), r=[bt], w=[bt])
        self.V(lambda: nc.vector.tensor_tensor(sth[:], P[:, 0, :], P[:, 2, :], ALU.mult), r=[bP], w=[bt])
        self.V(lambda: nc.vector.tensor_tensor(den[:], lam[:, 0, :], lam[:, 0, :], ALU.mult), r=[b_lam], w=[bt])
        self.V(lambda: nc.vector.tensor_tensor(nr[:], lam[:, 1, :], lam[:, 1, :], ALU.mult), r=[b_lam], w=[bt])
        self.V(lambda: nc.vector.tensor_tensor(den[:], den[:], nr[:], ALU.add), r=[bt], w=[bt])
        self.V(lambda: nc.vector.reciprocal(den[:], den[:]), r=[bt], w=[bt])
        a, b2 = t[6], t[7]
        self.V(lambda: nc.vector.tensor_tensor(a[:], cth[:], lam[:, 0, :], ALU.mult), r=[bt, b_lam], w=[bt])
        self.V(lambda: nc.vector.tensor_tensor(b2[:], sth[:], lam[:, 1, :], ALU.mult), r=[bt, b_lam], w=[bt])
        self.V(lambda: nc.vector.tensor_tensor(a[:], a[:], b2[:], ALU.add), r=[bt], w=[bt])
        self.V(lambda: nc.vector.tensor_tensor(P[:, 3, :], a[:], den[:], ALU.mult), r=[bt], w=[bP])
        self.V(lambda: nc.vector.tensor_tensor(a[:], sth[:], lam[:, 0, :], ALU.mult), r=[bt, b_lam], w=[bt])
        self.V(lambda: nc.vector.tensor_tensor(b2[:], cth[:], lam[:, 1, :], ALU.mult), r=[bt, b_lam], w=[bt])
        self.V(lambda: nc.vector.tensor_tensor(a[:], a[:], b2[:], ALU.subtract), r=[bt], w=[bt])
        self.V(lambda: nc.vector.tensor_tensor(P[:, 4, :], a[:], den[:], ALU.mult), r=[bt], w=[bP])
        self.V(lambda: nc.vector.tensor_scalar(P[:, 5, :], P[:, 4, :], -1.0, None, ALU.mult), r=[bP], w=[bP])
        ang = self.sb(es, "angW", [128, 32, 12]); b_ang = Buf()
        for k in range(12):
            self.V(lambda: nc.vector.tensor_scalar(ang[:, :, k], th[:], float(2 ** k), None, ALU.mult), r=[bt], w=[b_ang])
        _sincos(self, es, self.s5_W[:, 1], self.s5_W[:, 0], ang[:], [128, 32, 12], [b_ang], self.b_s5_W, "w")
    self.fw.barrier()


def _ph_s5(self, i, kind):
    nc = self.nc
    sts = [self.streams[(kind, s)] for s in range(self.n_samp)]
    L = sts[0].L
    TBS = min(1024, L); NTS = L // TBS
    TB = min(512, L)
    NLV = int(np.log2(L))
    P = self.s5_par; bP = self.b_s5_par
    perm = (kind == "x")
    with ExitStack() as es:
        TC = self.sb(es, "TC", [128, L]); TS = self.sb(es, "TS", [128, L]); b_T = Buf()
        tmpT = self.sb(es, "tmpT", [128, 2, L // 2]); b_tmpT = Buf(); b_tmpT2 = Buf()
        u = [self.sb(es, f"u{s}", [128, L]) for s in range(len(sts))]; b_u = [Buf() for _ in sts]
        yacc = [self.sb(es, f"yacc{s}", [128, L]) for s in range(len(sts))]; b_yacc = [Buf() for _ in sts]
        Bt = self.sb(es, "Bt", [128, 2, 2, 128]); b_Bt = Buf()
        Cf = self.sb(es, "Cf", [128, 2, 128]); b_Cf = Buf()
        Cb = self.sb(es, "Cb", [128, 2, 128], BF16); b_Cb = Buf()
        ctmp = self.sb(es, "ctmp", [128, 128]); b_ctmp = Buf()
        W4s = [[self.sb(es, f"W4_{s_}_{k}", [128, TBS]) for k in range(4)] for s_ in range(len(sts))]
        b_W4s = [[Buf() for _ in range(4)] for _ in sts]
        sb16s = [self.sb(es, f"sb16_{s_}", [128, 2, TBS], BF16) for s_ in range(len(sts))]; b_sb16s = [Buf() for _ in sts]
        cars = [self.sb(es, f"car{s_}", [128, 4]) for s_ in range(len(sts))]; b_cars = [Buf() for _ in sts]
        inis = [self.sb(es, f"ini{s_}", [128, 4]) for s_ in range(len(sts))]; b_inis = [Buf() for _ in sts]
        pcnt = {"npv": 0, "npy": 0}
        dcol = self.sb(es, "dcol", [128, 4]); b_dcol = Buf()
        ybf = self.sb(es, "ybf", [128, L], BF16); b_ybf = Buf()
        pv = [self.ps(es, f"pv{k}", [128, 512]) for k in range(4)]; b_pv = [Buf() for _ in range(4)]
        py = [self.ps(es, f"py{k}", [128, 512]) for k in range(2)]; b_py = [Buf() for _ in range(2)]
        self.dma(dcol[:], self.s5_dT[i], w=[b_dcol])
        for chunk in range(4):
            for s, st in enumerate(sts):
                self.dma(u[s][:], st.PJ[C_U + chunk * 128:C_U + (chunk + 1) * 128, :], r=[self.pjbuf(st, C_U + chunk * 128)], w=[b_u[s]])
            for gl4 in range(4):
                gp = chunk * 4 + gl4
                pr = slice(gl4 * 32, gl4 * 32 + 32)
                for d in range(2):
                    self.dma(Bt[:, d], self.s5_B[i, d, gp], w=[b_Bt])
                for d in range(2):
                    dg = d * 16 + gp
                    self.dma(Cf[:], self.s5_C[i, d, gp], w=[b_Cf])
                    cre, cim, ncim = P[:, 3, dg:dg + 1], P[:, 4, dg:dg + 1], P[:, 5, dg:dg + 1]
                    self.V(lambda: nc.vector.tensor_scalar(ctmp[:], Cf[:, 1, :], cim, None, ALU.mult), r=[b_Cf, bP], w=[b_ctmp])
                    self.V(lambda: nc.vector.scalar_tensor_tensor(Cb[:, 0, :], Cf[:, 0, :], cre, ctmp[:], ALU.mult, ALU.subtract),
                           r=[b_Cf, bP, b_ctmp], w=[b_Cb])
                    self.V(lambda: nc.vector.tensor_scalar(ctmp[:], Cf[:, 1, :], cre, None, ALU.mult), r=[b_Cf, bP], w=[b_ctmp])
                    self.V(lambda: nc.vector.scalar_tensor_tensor(Cb[:, 1, :], Cf[:, 0, :], ncim, ctmp[:], ALU.mult, ALU.subtract),
                           r=[b_Cf, bP, b_ctmp], w=[b_Cb])
                    self.V(lambda: nc.vector.memset(TC[:, 0:1], 1.0), w=[b_T])
                    self.V(lambda: nc.vector.memset(TS[:, 0:1], 0.0), w=[b_T])
                    for k in range(NLV):
                        n = 2 ** k
                        wr = self.s5_W[:, 0, dg, k:k + 1]; wi = self.s5_W[:, 1, dg, k:k + 1]
                        rW = [b_T, self.b_s5_W]
                        self.A(lambda: nc.scalar.activation(out=tmpT[:, 0, 0:n], in_=TS[:, 0:n], func=AF.Copy, scale=wi), r=rW, w=[b_tmpT])
                        self.A(lambda: nc.scalar.activation(out=tmpT[:, 1, 0:n], in_=TC[:, 0:n], func=AF.Copy, scale=wi), r=rW, w=[b_tmpT2])
                        self.V(lambda: nc.vector.scalar_tensor_tensor(TC[:, n:2 * n], TC[:, 0:n], wr, tmpT[:, 0, 0:n], ALU.mult, ALU.subtract),
                               r=rW + [b_tmpT], w=[b_T])
                        self.V(lambda: nc.vector.scalar_tensor_tensor(TS[:, n:2 * n], TS[:, 0:n], wr, tmpT[:, 1, 0:n], ALU.mult, ALU.add),
                               r=rW + [b_tmpT2], w=[b_T])
                    rho_b = P[:, 0, dg:dg + 1].to_broadcast([128, TBS])
                    def chain(s, st, d=d, dg=dg, gp=gp, pr=pr, rho_b=rho_b):
                        W4 = W4s[s]; b_W4 = b_W4s[s]; sb16 = sb16s[s]; b_sb16 = b_sb16s[s]
                        car = cars[s]; b_car = b_cars[s]; ini = inis[s]; b_ini = b_inis[s]
                        if kind == "x":
                            hre = self.s5_st[:, 0, dg, s:s + 1]; him = self.s5_st[:, 1, dg, s:s + 1]
                            c1 = P[:, 1, dg:dg + 1]; s1 = P[:, 2, dg:dg + 1]
                            self.V(lambda: nc.vector.tensor_tensor(ini[:, 2:3], him, s1, ALU.mult), r=[self.b_s5_st, bP], w=[b_ini])
                            self.V(lambda: nc.vector.scalar_tensor_tensor(ini[:, 0:1], hre, c1, ini[:, 2:3], ALU.mult, ALU.subtract),
                                   r=[self.b_s5_st, bP, b_ini], w=[b_ini])
                            self.V(lambda: nc.vector.tensor_tensor(ini[:, 2:3], hre, s1, ALU.mult), r=[self.b_s5_st, bP], w=[b_ini])
                            self.V(lambda: nc.vector.scalar_tensor_tensor(ini[:, 1:2], him, c1, ini[:, 2:3], ALU.mult, ALU.add),
                                   r=[self.b_s5_st, bP, b_ini], w=[b_ini])
                        tbs_order = range(NTS) if d == 0 else range(NTS - 1, -1, -1)
                        for ti, tb in enumerate(tbs_order):
                            n0 = tb * TBS
                            if d == 0:
                                tc = TC[:, n0:n0 + TBS]; ts = TS[:, n0:n0 + TBS]
                            else:
                                o = L - 1 - n0
                                tc = bass.AP(TC, o, [[TC[:].ap[0][0], 128], [-1, TBS]])
                                ts = bass.AP(TS, o, [[TS[:].ap[0][0], 128], [-1, TBS]])
                            Vre, Vim, Abuf, Bbuf = W4
                            bVre, bVim, bA, bB = b_W4
                            for sb_ in range(TBS // TB):
                                c0 = n0 + sb_ * TB
                                if perm:
                                    w0 = c0 // 64
                                    rhs = bass.AP(u[s], w0, [[u[s][:].ap[0][0], 128], [1, TB // 64], [64, 64]])
                                else:
                                    rhs = u[s][:, c0:c0 + TB]
                                for ri, (dst, bdst) in enumerate(((Vre, bVre), (Vim, bVim))):
                                    k = pcnt['npv'] % 4; pcnt['npv'] += 1
                                    self.T(lambda: nc.tensor.matmul(pv[k][:, :TB], Bt[:, d, ri, :], rhs, start=True, stop=True),
                                           r=[b_Bt, b_u[s]], w=[b_pv[k]])
                                    self.A(lambda: nc.scalar.copy(dst[:, sb_ * TB:(sb_ + 1) * TB], pv[k][:, :TB]), r=[b_pv[k]], w=[bdst])
                            yield
                            self.G(lambda: nc.gpsimd.tensor_tensor(Bbuf[:], Vim[:], ts, ALU.mult), r=[bVim, b_T], w=[bB])
                            self.V(lambda: nc.vector.tensor_tensor(Abuf[:], Vre[:], tc, ALU.mult), r=[bVre, b_T], w=[bA])
                            self.G(lambda: nc.gpsimd.tensor_tensor(Vre[:], Vre[:], ts, ALU.mult), r=[bVre, b_T], w=[bVre])
                            self.V(lambda: nc.vector.tensor_tensor(Abuf[:], Abuf[:], Bbuf[:], ALU.add), r=[bA, bB], w=[bA])
                            self.V(lambda: nc.vector.tensor_tensor(Bbuf[:], Vim[:], tc, ALU.mult), r=[bVim, b_T], w=[bB])
                            self.V(lambda: nc.vector.tensor_tensor(Bbuf[:], Bbuf[:], Vre[:], ALU.subtract), r=[bB, bVre], w=[bB])
                            yield
                            if ti == 0:
                                if kind == "x":
                                    i_re, i_im, rd = ini[:, 0:1], ini[:, 1:2], [b_ini]
                                else:
                                    i_re, i_im, rd = 0.0, 0.0, []
                            else:
                                i_re, i_im, rd = car[:, 0:1], car[:, 1:2], [b_car]
                            if d == 0:
                                rv = lambda t_: t_[:]
                                last = lambda t_: t_[:, TBS - 1:TBS]
                            else:
                                rv = lambda t_: bass.AP(t_, TBS - 1, [[t_[:].ap[0][0], 128], [-1, TBS]])
                                last = lambda t_: t_[:, 0:1]
                            self.V(lambda: nc.vector.tensor_tensor_scan(rv(Vim), rho_b, rv(Abuf), i_re, ALU.mult, ALU.add), r=[bA, bP] + rd, w=[bVim])
                            self.V(lambda: nc.vector.tensor_tensor_scan(rv(Vre), rho_b, rv(Bbuf), i_im, ALU.mult, ALU.add), r=[bB, bP] + rd, w=[bVre])
                            self.A(lambda: nc.scalar.copy(car[:, 0:1], last(Vim)), r=[bVim], w=[b_car])
                            self.A(lambda: nc.scalar.copy(car[:, 1:2], last(Vre)), r=[bVre], w=[b_car])
                            qre, qim, bqre, bqim = Vim, Vre, bVim, bVre
                            yield
                            self.G(lambda: nc.gpsimd.tensor_tensor(Abuf[:], qim[:], ts, ALU.mult), r=[bqim, b_T], w=[bA])
                            self.V(lambda: nc.vector.tensor_tensor(Bbuf[:], qre[:], tc, ALU.mult), r=[bqre, b_T], w=[bB])
                            self.V(lambda: nc.vector.tensor_tensor(sb16[:, 0, :], Bbuf[:], Abuf[:], ALU.subtract), r=[bA, bB], w=[b_sb16])
                            self.G(lambda: nc.gpsimd.tensor_tensor(Bbuf[:], qim[:], tc, ALU.mult), r=[bqim, b_T], w=[bB])
                            self.V(lambda: nc.vector.tensor_tensor(Abuf[:], qre[:], ts, ALU.mult), r=[bqre, b_T], w=[bA])
                            self.V(lambda: nc.vector.tensor_tensor(sb16[:, 1, :], Abuf[:], Bbuf[:], ALU.add), r=[bA, bB], w=[b_sb16])
                            if kind == "c" and ti == NTS - 1:
                                e = (TBS - 1) if d == 0 else 0
                                tce = TC[:, L - 1:L]; tse = TS[:, L - 1:L]
                                qr_e = qre[:, e:e + 1]; qi_e = qim[:, e:e + 1]
                                self.V(lambda: nc.vector.tensor_tensor(car[:, 2:3], qi_e, tse, ALU.mult), r=[bqim, b_T], w=[b_car])
                                self.V(lambda: nc.vector.scalar_tensor_tensor(self.s5_st[:, 0, dg, s:s + 1], qr_e, tce, car[:, 2:3], ALU.mult, ALU.subtract),
                                       r=[bqre, b_T, b_car], w=[self.b_s5_st])
                                self.V(lambda: nc.vector.tensor_tensor(car[:, 2:3], qr_e, tse, ALU.mult), r=[bqre, b_T], w=[b_car])
                                self.V(lambda: nc.vector.scalar_tensor_tensor(self.s5_st[:, 1, dg, s:s + 1], qi_e, tce, car[:, 2:3], ALU.mult, ALU.add),
                                       r=[bqim, b_T, b_car], w=[self.b_s5_st])
                            yield
                            for sb_ in range(TBS // TB):
                                c0 = n0 + sb_ * TB
                                k = pcnt['npy'] % 2; pcnt['npy'] += 1
                                self.T(lambda: nc.tensor.matmul(py[k][:, :TB], Cb[:, 0, :], sb16[:, 0, sb_ * TB:(sb_ + 1) * TB], start=True, stop=False),
                                       r=[b_Cb, b_sb16], w=[b_py[k]])
                                self.T(lambda: nc.tensor.matmul(py[k][:, :TB], Cb[:, 1, :], sb16[:, 1, sb_ * TB:(sb_ + 1) * TB], start=False, stop=True),
                                       r=[b_Cb, b_sb16], w=[b_py[k]])
                                if d == 0:
                                    self.A(lambda: nc.scalar.copy(yacc[s][pr, c0:c0 + TB], py[k][pr, :TB]), r=[b_py[k]], w=[b_yacc[s]])
                                else:
                                    self.V(lambda: nc.vector.tensor_tensor(yacc[s][pr, c0:c0 + TB], yacc[s][pr, c0:c0 + TB], py[k][pr, :TB], ALU.add),
                                           r=[b_py[k], b_yacc[s]], w=[b_yacc[s]])
                    gens = [chain(s_, st_) for s_, st_ in enumerate(sts)]
                    while gens:
                        for g_ in list(gens):
                            try:
                                next(g_)
                            except StopIteration:
                                gens.remove(g_)
            for s, st in enumerate(sts):
                ya = yacc[s]; uu = u[s]
                if perm:
                    ps_ = uu[:].ap[0][0]
                    nat = lambda t_: bass.AP(t_, 0, [[ps_, 128], [1, 64], [64, 64]])
                    seq = lambda t_: bass.AP(t_, 0, [[ps_, 128], [64, 64], [1, 64]])
                    self.V(lambda: nc.vector.scalar_tensor_tensor(seq(uu), seq(uu), dcol[:, chunk:chunk + 1], nat(ya), ALU.mult, ALU.add),
                           r=[b_u[s], b_yacc[s], b_dcol], w=[b_u[s]])
                else:
                    self.V(lambda: nc.vector.scalar_tensor_tensor(uu[:], uu[:], dcol[:, chunk:chunk + 1], ya[:], ALU.mult, ALU.add),
                           r=[b_u[s], b_yacc[s], b_dcol], w=[b_u[s]])
                self.G(lambda: nc.gpsimd.tensor_tensor(ya[:], uu[:], uu[:], ALU.mult), r=[b_u[s]], w=[b_yacc[s]])
                self.G(lambda: nc.gpsimd.tensor_scalar(ya[:], ya[:], 0.044715, 1.0, ALU.mult, ALU.add), r=[b_yacc[s]], w=[b_yacc[s]])
                self.G(lambda: nc.gpsimd.tensor_tensor(ya[:], ya[:], uu[:], ALU.mult), r=[b_yacc[s], b_u[s]], w=[b_yacc[s]])
                self.A(lambda: nc.scalar.activation(out=ya[:], in_=ya[:], func=AF.Sigmoid, scale=GELU_K), r=[b_yacc[s]], w=[b_yacc[s]])
                self.V(lambda: nc.vector.tensor_tensor(ybf[:], uu[:], ya[:], ALU.mult), r=[b_yacc[s], b_u[s]], w=[b_ybf])
                self.dma(st.ys5[chunk * 128:(chunk + 1) * 128, :], ybf[:], r=[b_ybf], w=[st.b_ys5[chunk]])
    self.fw.barrier()


KB.s5_setup = _s5_setup
KB.ph_s5_params = _ph_s5_params
KB.ph_s5 = _ph_s5


def prep_s5(inp):
    o = {}
    lam = np.zeros((4, 128, 3, 32), np.float32)
    Bm = np.zeros((4, 2, 16, 128, 2, 128), np.float32)
    Cm = np.zeros((4, 2, 16, 128, 2, 128), np.float32)
    lre, lim, lst = (np.asarray(inp[k], np.float32) for k in ("s5_lam_re", "s5_lam_im", "s5_log_step"))
    bre, bim, cre, cim = (np.asarray(inp[k], np.float32) for k in ("s5_b_re", "s5_b_im", "s5_c_re", "s5_c_im"))
    for d in range(2):
        for gp in range(16):
            dg = d * 16 + gp
            chunk, gl4 = gp // 4, gp % 4
            for gl in range(2):
                g = 2 * gp + gl
                rows = slice(gl * 64, gl * 64 + 64)
                lam[:, rows, 0, dg] = lre[:, d, g, :]
                lam[:, rows, 1, dg] = lim[:, d, g, :]
                lam[:, rows, 2, dg] = lst[:, d, g][:, None]
                r0 = gl4 * 32 + gl * 16
                Bm[:, d, gp, r0:r0 + 16, 0, rows] = bre[:, d, g].transpose(0, 2, 1)
                Bm[:, d, gp, r0:r0 + 16, 1, rows] = bim[:, d, g].transpose(0, 2, 1)
                Cm[:, d, gp, rows, 0, r0:r0 + 16] = cre[:, d, g].transpose(0, 2, 1)
                Cm[:, d, gp, rows, 1, r0:r0 + 16] = cim[:, d, g].transpose(0, 2, 1)
    o["s5_lam"] = lam; o["s5_B"] = Bm; o["s5_C"] = Cm
    o["s5_dT"] = np.stack([colform(inp["s5_d"][i], 4) for i in range(4)])
    return o


def _ssd_setup(self):
    es = self.ges
    S = self.n_samp
    self.m2_cw = self.inp("m2_cw", [4, 128, 12, 4])
    self.m2_cb = self.inp("m2_cb", [4, 128, 12])
    self.m2_row = self.inp("m2_row", [4, 1, 64])
    self.m2_drow = self.inp("m2_drow", [4, 1, 2048])
    self.ssd_const = self.inp("ssd_const", [128, 4, 128])
    self.ssd_c = self.sb(es, "ssd_c", [128, 4, 128]); self.b_ssd_c = Buf("ssd_c")
    self.dma(self.ssd_c[:], self.ssd_const, w=[self.b_ssd_c])
    for st in self.streams.values():
        L = st.L
        st.xbc_tm = self.scratch(f"xbctm_{st.kind}{st.s}", [L, 1280])
        st.b_xbc_tm = [Buf() for _ in range(10)]
        st.bc_fm = self.scratch(f"bcfm_{st.kind}{st.s}", [512, L])
        st.b_bc_fm = [Buf() for _ in range(4)]
        st.z_tm = self.scratch(f"ztm_{st.kind}{st.s}", [L, 1024])
        st.b_z_tm = [Buf() for _ in range(st.NT)]
        st.dt_tm = self.scratch(f"dttm_{st.kind}{st.s}", [L, 32])
        st.b_dt_tm = Buf()
        st.yf = self.scratch(f"yf_{st.kind}{st.s}", [L, 1024])
        st.b_yf = [Buf() for _ in range(st.NT)]


def _ph_ssd(self, i, st):
    nc = self.nc
    L, NT = st.L, st.NT
    s = st.s
    C = self.ssd_c
    triU, triL, mbF, mbB = C[:, 0, :], C[:, 1, :], C[:, 2, :], C[:, 3, :]
    with ExitStack() as es:
        cw = self.sb(es, "m2cw", [128, 12, 4]); cb = self.sb(es, "m2cb", [128, 12]); b_cp = Buf()
        self.dma(cw[:], self.m2_cw[i], w=[b_cp]); self.dma(cb[:], self.m2_cb[i], w=[b_cp])
        xp = [self.sb(es, f"xp{k}", [128, L + 3]) for k in range(2)]; b_xp = [Buf() for _ in range(2)]
        acc = self.sb(es, "cacc", [128, L]); b_acc = Buf()
        act = [self.sb(es, f"cact{k}", [128, L]) for k in range(2)]; b_act = [Buf() for _ in range(2)]
        tms = self.sb(es, "tms", [128, NT, 128]); b_tms = Buf()
        pt = [self.ps(es, f"ptt{k}", [128, 4, 128]) for k in range(2)]; b_pt = [Buf() for _ in range(2)]
        for k in range(2):
            self.V(lambda: nc.vector.memset(xp[k][:, 0:2], 0.0), w=[b_xp[k]])
            self.V(lambda: nc.vector.memset(xp[k][:, L + 2:L + 3], 0.0), w=[b_xp[k]])
        npt = 0
        for ch in range(12):
            k = ch % 2
            r0 = C_XBC + ch * 128
            self.dma(xp[k][:, 2:L + 2], st.PJ[r0:r0 + 128, :], r=[self.pjbuf(st, r0)], w=[b_xp[k]])
            self.V(lambda: nc.vector.tensor_scalar(acc[:], xp[k][:, 0:L], cw[:, ch, 0:1], cb[:, ch:ch + 1], ALU.mult, ALU.add),
                   r=[b_xp[k], b_cp], w=[b_acc])
            for j in range(1, 4):
                self.V(lambda: nc.vector.scalar_tensor_tensor(acc[:], xp[k][:, j:j + L], cw[:, ch, j:j + 1], acc[:], ALU.mult, ALU.add),
                       r=[b_xp[k], b_cp, b_acc], w=[b_acc])
            self.A(lambda: nc.scalar.activation(out=act[k][:], in_=acc[:], func=AF.Silu), r=[b_acc], w=[b_act[k]])
            if ch >= 8:
                self.dma(st.bc_fm[(ch - 8) * 128:(ch - 7) * 128, :], act[k][:], r=[b_act[k]], w=[st.b_bc_fm[ch - 8]])
            if ch < 10:
                for t0 in range(0, NT, 4):
                    pk = npt % 2; npt += 1
                    nt4 = min(4, NT - t0)
                    for q in range(nt4):
                        t = t0 + q
                        self.T(lambda: nc.tensor.matmul(pt[pk][:, q, :], act[k][:, t * 128:(t + 1) * 128], self.ident_f[:], start=True, stop=True),
                               r=[b_act[k], self.b_const], w=[b_pt[pk]])
                    if npt % 2 == 0:
                        self.A(lambda: nc.scalar.copy(tms[:, t0:t0 + nt4, :], pt[pk][:, 0:nt4, :]), r=[b_pt[pk]], w=[b_tms])
                    else:
                        self.G(lambda: nc.gpsimd.tensor_copy(tms[:, t0:t0 + nt4, :], pt[pk][:, 0:nt4, :]), r=[b_pt[pk]], w=[b_tms]) if False else \
                            self.V(lambda: nc.vector.tensor_copy(tms[:, t0:t0 + nt4, :], pt[pk][:, 0:nt4, :]), r=[b_pt[pk]], w=[b_tms])
                self.dma(st.xbc_tm[:, ch * 128:(ch + 1) * 128].rearrange("(t p) c -> p t c", p=128), tms[:], r=[b_tms], w=[st.b_xbc_tm[ch]])
    self.fw.barrier()
    with ExitStack() as es:
        rowp = self.sb(es, "rowp", [128, 64]); b_rowp = Buf()
        drow = self.sb(es, "drow", [128, 2048]); b_drow = Buf()
        self.dma(rowp[:], self.m2_row[i].partition_broadcast(128), w=[b_rowp])
        self.dma(drow[:], self.m2_drow[i].partition_broadcast(128), w=[b_drow])
        self.A(lambda: nc.scalar.activation(out=rowp[:, 32:64], in_=rowp[:, 32:64], func=AF.Exp), r=[b_rowp], w=[b_rowp])
        self.V(lambda: nc.vector.tensor_scalar(rowp[:, 32:64], rowp[:, 32:64], -1.0, None, ALU.mult), r=[b_rowp], w=[b_rowp])
        DT = self.sb(es, "DT", [128, NT, 32]); b_DT = Buf()
        LA = self.sb(es, "LA", [128, NT, 32]); b_LA = Buf()
        self.dma(DT[:], st.dt_tm.rearrange("(t p) c -> p t c", p=128), r=[st.b_dt_tm], w=[b_DT])
        self.V(lambda: nc.vector.tensor_tensor(DT[:], DT[:], rowp[:, 0:32].unsqueeze(1).to_broadcast([128, NT, 32]), ALU.add), r=[b_DT, b_rowp], w=[b_DT])
        self.A(lambda: nc.scalar.activation(out=DT[:], in_=DT[:], func=AF.Exp), r=[b_DT], w=[b_DT])
        self.A(lambda: nc.scalar.activation(out=DT[:], in_=DT[:], func=AF.Ln, bias=1.0), r=[b_DT], w=[b_DT])
        self.V(lambda: nc.vector.tensor_tensor(LA[:], DT[:], rowp[:, 32:64].unsqueeze(1).to_broadcast([128, NT, 32]), ALU.mult), r=[b_DT, b_rowp], w=[b_LA])
        XT = [self.sb(es, f"XT{k}", [128, 1280]) for k in range(2)]; b_XT = [Buf() for _ in range(2)]
        BCf = [self.sb(es, f"BCf{k}", [64, 8, 128]) for k in range(2)]; b_BCf = [Buf() for _ in range(2)]
        acs = self.sb(es, "acs", [128, 48]); b_acs = Buf()
        din = self.sb(es, "din", [128, 16]); b_din = Buf()
        dch = self.sb(es, "dch", [64, 16]); b_dch = Buf()
        CBs = self.sb(es, "CBs", [128, 4, 128]); b_CBs = Buf()
        arg = self.sb(es, "arg", [128, 8, 128]); b_arg = Buf()
        MT = self.sb(es, "MT", [128, 8, 128], BF16); b_MT = Buf()
        Bd = self.sb(es, "Bd", [128, 16, 64], BF16); b_Bd = Buf()
        xs = self.sb(es, "xs", [128, 1024], BF16); b_xs = Buf()
        yt = self.sb(es, "yt", [128, 1024]); b_yt = Buf()
        yfl = self.sb(es, "yfl", [128, 1024]); b_yfl = Buf()
        zt = self.sb(es, "zt", [128, 1024]); b_zt = Buf()
        ybf = self.sb(es, "ybf16", [128, 1024], BF16); b_ybf = Buf()
        ssq = self.sb(es, "ssqm", [128, 2]); b_ssq = Buf()
        ystg = self.sb(es, "ystg", [128, 8, 512], BF16); b_ystg = Buf()
        etmp = self.sb(es, "etmp", [64, 512]); b_etmp = Buf()
        pc = self.ps(es, "pc", [128, 32]); b_pc = Buf()
        cbp = self.ps(es, "cbp", [128, 4, 128]); b_cbp = Buf()
        abc = self.ps(es, "abc", [128, 8, 128]); b_abc = Buf()
        yd = self.ps(es, "yd", [128, 512]); b_yd = Buf()
        yo = self.ps(es, "yo", [128, 512]); b_yo = Buf()
        stp = self.ps(es, "stp", [64, 512]); b_stp = Buf()
        ptr = self.ps(es, "ptr2", [128, 8, 128], BF16); b_ptr = Buf()
        grp = min(4, NT)
        for d in range(2):
            ent = self.ssd_st[:, d, s, :]
            b_ent = self.b_ssd_st[d][s]
            if st.kind == "c":
                self.V(lambda: nc.vector.memset(ent, 0.0), w=[b_ent])
            tri = triU if d == 0 else triL
            mb = mbF if d == 0 else mbB
            order = range(NT) if d == 0 else range(NT - 1, -1, -1)
            for ci, c in enumerate(order):
                k = ci % 2
                tok = slice(c * 128, (c + 1) * 128)
                self.dma(XT[k][:], st.xbc_tm[tok, :], r=st.b_xbc_tm, w=[b_XT[k]])
                self.dma(BCf[k][:], st.bc_fm[:, tok].rearrange("(k n) t -> n k t", n=64), r=st.b_bc_fm, w=[b_BCf[k]])
                la = LA[:, c, d * 16:(d + 1) * 16]
                dtv = DT[:, c, d * 16:(d + 1) * 16]
                self.T(lambda: nc.tensor.matmul(pc[:, 0:16], tri, la, start=True, stop=True), r=[self.b_ssd_c, b_LA], w=[b_pc])
                self.T(lambda: nc.tensor.matmul(pc[:, 16:32], self.ones_f[:], la, start=True, stop=True), r=[self.b_const, b_LA], w=[b_pc])
                for g in range(4):
                    self.T(lambda: nc.tensor.matmul(cbp[:, g, :], BCf[k][:, g, :], BCf[k][:, 4 + g, :], start=True, stop=True),
                           r=[b_BCf[k]], w=[b_cbp])
                self.V(lambda: nc.vector.tensor_copy(acs[:, 0:32], pc[:, 0:32]), r=[b_pc], w=[b_acs])
                self.A(lambda: nc.scalar.copy(CBs[:], cbp[:]), r=[b_cbp], w=[b_CBs])
                self.V(lambda: nc.vector.tensor_tensor(din[:], acs[:, 16:32], acs[:, 0:16], ALU.subtract), r=[b_acs], w=[b_din])
                self.A(lambda: nc.scalar.activation(out=din[:], in_=din[:], func=AF.Exp), r=[b_din], w=[b_din])
                self.A(lambda: nc.scalar.activation(out=acs[:, 32:48], in_=acs[:, 0:16], func=AF.Exp), r=[b_acs], w=[b_acs])
                self.A(lambda: nc.scalar.activation(out=dch[:], in_=acs[0:64, 16:32], func=AF.Exp), r=[b_acs], w=[b_dch])
                Btm = XT[k][:, 1024:1280].rearrange("p (g n) -> p g n", g=4).unsqueeze(2).to_broadcast([128, 4, 4, 64])
                self.V(lambda: nc.vector.tensor_tensor(Bd[:].rearrange("p (g j) n -> p g j n", g=4), Btm,
                                                       din[:].rearrange("p (g j) -> p g j", g=4).unsqueeze(3).to_broadcast([128, 4, 4, 64]), ALU.mult),
                       r=[b_XT[k], b_din], w=[b_Bd])
                self.G(lambda: nc.gpsimd.tensor_tensor(xs[:].rearrange("p (h c) -> p h c", h=16), XT[k][:, 0:1024].rearrange("p (h c) -> p h c", h=16),
                                                       dtv.unsqueeze(2).to_broadcast([128, 16, 64]), ALU.mult),
                       r=[b_XT[k], b_DT], w=[b_xs])
                for hh in range(2):
                    hs = slice(hh * 8, hh * 8 + 8)
                    for hq in range(8):
                        h = hh * 8 + hq
                        self.T(lambda: nc.tensor.matmul(abc[:, hq, :], LA[:, c, d * 16 + h:d * 16 + h + 1].to_broadcast([128, 128]), tri, start=True, stop=False),
                               r=[b_LA, self.b_ssd_c], w=[b_abc])
                        self.T(lambda: nc.tensor.matmul(abc[:, hq, :], self.ident_f[:], mb, start=False, stop=True),
                               r=[self.b_const, self.b_ssd_c], w=[b_abc])
                    self.V(lambda: nc.vector.tensor_tensor(arg[:], abc[:], acs[:, hh * 8:hh * 8 + 8].unsqueeze(2).to_broadcast([128, 8, 128]), ALU.subtract),
                           r=[b_abc, b_acs], w=[b_arg])
                    self.A(lambda: nc.scalar.activation(out=arg[:], in_=arg[:], func=AF.Exp), r=[b_arg], w=[b_arg])
                    self.G(lambda: nc.gpsimd.tensor_tensor(MT[:].rearrange("p (g j) q -> p g j q", g=2), arg[:].rearrange("p (g j) q -> p g j q", g=2),
                                                           CBs[:, hh * 2:hh * 2 + 2, :].unsqueeze(2).to_broadcast([128, 2, 4, 128]), ALU.mult),
                           r=[b_arg, b_CBs], w=[b_MT])
                    for hq in range(8):
                        h = hh * 8 + hq
                        self.T(lambda: nc.tensor.matmul(yd[:, hq * 64:(hq + 1) * 64], MT[:, hq, :], xs[:, h * 64:(h + 1) * 64], start=True, stop=True),
                               r=[b_MT, b_xs], w=[b_yd])
                    for gq in range(2):
                        g = hh * 2 + gq
                        self.T(lambda: nc.tensor.matmul(yo[:, gq * 256:(gq + 1) * 256], BCf[k][:, 4 + g, :], ent[:, g * 256:(g + 1) * 256], start=True, stop=True),
                               r=[b_BCf[k], b_ent], w=[b_yo])
                    for hq in range(8):
                        h = hh * 8 + hq
                        self.T(lambda: nc.tensor.matmul(stp[:, hq * 64:(hq + 1) * 64], Bd[:, h, :], xs[:, h * 64:(h + 1) * 64], start=True, stop=True),
                               r=[b_Bd, b_xs], w=[b_stp])
                    ysl = yt[:, hh * 512:(hh + 1) * 512]
                    self.V(lambda: nc.vector.tensor_tensor(ysl.rearrange("p (h c) -> p h c", h=8), yo[:].rearrange("p (h c) -> p h c", h=8),
                                                           acs[:, 32 + hh * 8:32 + hh * 8 + 8].unsqueeze(2).to_broadcast([128, 8, 64]), ALU.mult),
                           r=[b_yo, b_acs], w=[b_yt])
                    self.V(lambda: nc.vector.tensor_tensor(ysl, ysl, yd[:], ALU.add), r=[b_yt, b_yd], w=[b_yt])
                    esl = ent[:, hh * 512:(hh + 1) * 512]
                    self.V(lambda: nc.vector.tensor_tensor(etmp[:].rearrange("p (h c) -> p h c", h=8), esl.rearrange("p (h c) -> p h c", h=8),
                                                           dch[:, hs].unsqueeze(2).to_broadcast([64, 8, 64]), ALU.mult),
                           r=[b_ent, b_dch], w=[b_etmp])
                    self.V(lambda: nc.vector.tensor_tensor(esl, etmp[:], stp[:], ALU.add), r=[b_etmp, b_stp], w=[b_ent])
                if d == 0:
                    self.dma(st.yf[tok, :], yt[:], r=[b_yt], w=[st.b_yf[c]])
                else:
                    self.dma(yfl[:], st.yf[tok, :], r=[st.b_yf[c]], w=[b_yfl])
                    self.dma(zt[:], st.z_tm[tok, :], r=[st.b_z_tm[c]], w=[b_zt])
                    self.G(lambda: nc.gpsimd.tensor_tensor(yfl[:], yfl[:], yt[:], ALU.add), r=[b_yfl, b_yt], w=[b_yfl])
                    self.G(lambda: nc.gpsimd.tensor_tensor(yt[:], XT[k][:, 0:1024], drow[:, 0:1024], ALU.mult), r=[b_XT[k], b_drow], w=[b_yt])
                    self.G(lambda: nc.gpsimd.tensor_tensor(yfl[:], yfl[:], yt[:], ALU.add), r=[b_yfl, b_yt], w=[b_yfl])
                    self.A(lambda: nc.scalar.activation(out=zt[:], in_=zt[:], func=AF.Silu), r=[b_zt], w=[b_zt])
                    self.G(lambda: nc.gpsimd.tensor_tensor(yfl[:], yfl[:], zt[:], ALU.mult), r=[b_yfl, b_zt], w=[b_yfl])
                    self.A(lambda: nc.scalar.activation(out=zt[:], in_=yfl[:], func=AF.Square, accum_out=ssq[:, 0:1]), r=[b_yfl], w=[b_zt, b_ssq])
                    self.A(lambda: nc.scalar.activation(out=ssq[:, 1:2], in_=ssq[:, 0:1], func=AF.Sqrt, scale=1.0 / 1024, bias=1e-6), r=[b_ssq], w=[b_ssq])
                    self.V(lambda: nc.vector.reciprocal(ssq[:, 1:2], ssq[:, 1:2]), r=[b_ssq], w=[b_ssq])
                    self.V(lambda: nc.vector.scalar_tensor_tensor(ybf[:], yfl[:], ssq[:, 1:2], drow[:, 1024:2048], ALU.mult, ALU.mult),
                           r=[b_yfl, b_ssq, b_drow], w=[b_ybf])
                    for kc in range(8):
                        self.T(lambda: nc.tensor.transpose(ptr[:, kc, :], ybf[:, kc * 128:(kc + 1) * 128], self.ident_b[:]),
                               r=[b_ybf, self.b_const], w=[b_ptr])
                    q = c % grp
                    self.A(lambda: nc.scalar.copy(ystg[:, :, q * 128:(q + 1) * 128], ptr[:]), r=[b_ptr], w=[b_ystg])
                    if q == 0:
                        c0 = c * 128
                        self.dma(st.yssd[:, c0:c0 + grp * 128].rearrange("(kc p) t -> p kc t", p=128), ystg[:, :, 0:grp * 128],
                                 r=[b_ystg], w=st.b_yssd)
    self.fw.barrier()


KB.ssd_setup = _ssd_setup
KB.ph_ssd = _ph_ssd


def prep_ssd(inp):
    o = {}
    cw = np.asarray(inp["m2_conv_w"], np.float32)
    o["m2_cw"] = np.ascontiguousarray(cw.reshape(4, 4, 12, 128).transpose(0, 3, 2, 1))
    o["m2_cb"] = np.ascontiguousarray(np.asarray(inp["m2_conv_b"], np.float32).reshape(4, 12, 128).transpose(0, 2, 1))
    o["m2_row"] = np.ascontiguousarray(np.concatenate([np.asarray(inp["m2_dt_bias"], np.float32).reshape(4, 1, 32),
                                                       np.asarray(inp["m2_a_log"], np.float32).reshape(4, 1, 32)], axis=2))
    drep = np.repeat(np.asarray(inp["m2_d"], np.float32), 64, axis=1)
    o["m2_drow"] = np.ascontiguousarray(np.concatenate([drep, np.asarray(inp["m2_norm_g"], np.float32)], axis=1).reshape(4, 1, 2048))
    q = np.arange(128)
    triU = (q[:, None] <= q[None, :]).astype(np.float32)
    triL = (q[:, None] >= q[None, :]).astype(np.float32)
    mbF = np.where(q[None, :] >= q[:, None], 0.0, -30000.0).astype(np.float32)
    mbB = np.where(q[None, :] <= q[:, None], 0.0, -30000.0).astype(np.float32)
    o["ssd_const"] = np.ascontiguousarray(np.stack([triU, triL, mbF, mbB], axis=1))
    return o


def _m5_setup(self):
    self.s5_wglu = self.inp("s5_w_glu", [4, 512, 2048])
    self.m2_wout = self.inp("m2_w_out", [4, 1024, 1024])
    self.lru_wout = self.inp("lru_w_out", [4, 512, 1024])
    self.w_o = self.inp("w_o", [4, 1024, 1024])
    self.wg_bf = self.scratch("wg_bf", [128, 8, 3072], BF16)
    self.b_wg_bf = Buf("wg_bf")


def _ph_castgates(self, i):
    nc = self.nc
    with ExitStack() as es:
        wf = [self.sb(es, f"cgf{k}", [128, 8, 512]) for k in range(2)]; b_wf = [Buf() for _ in range(2)]
        wb = [self.sb(es, f"cgb{k}", [128, 8, 512], BF16) for k in range(2)]; b_wb = [Buf() for _ in range(2)]
        for n in range(6):
            k = n % 2
            c0 = C_G + n * 512
            self.dma(wf[k][:], self.w_in[i][:, c0:c0 + 512].rearrange("(kc p) n -> p kc n", p=128), w=[b_wf[k]])
            if k == 0:
                self.G(lambda: nc.gpsimd.tensor_copy(wb[k][:], wf[k][:]), r=[b_wf[k]], w=[b_wb[k]])
            else:
                self.A(lambda: nc.scalar.copy(wb[k][:], wf[k][:]), r=[b_wf[k]], w=[b_wb[k]])
            self.dma(self.wg_bf[:, :, n * 512:(n + 1) * 512], wb[k][:], r=[b_wb[k]], w=[self.b_wg_bf])
    self.fw.barrier()


def _load_cast(self, es, dst, b_dst, src_ap, nk, ncols, stg, b_stg, cnt):
    nc = self.nc
    for c0 in range(0, ncols, 512):
        k = cnt[0] % 2; cnt[0] += 1
        self.dma(stg[k][:, :nk, :], src_ap[:, c0:c0 + 512].rearrange("(kc p) n -> p kc n", p=128), w=[b_stg[k]])
        if k == 0:
            self.G(lambda: nc.gpsimd.tensor_copy(dst[:, :nk, c0:c0 + 512], stg[k][:, :nk, :]), r=[b_stg[k]], w=[b_dst])
        else:
            self.A(lambda: nc.scalar.copy(dst[:, :nk, c0:c0 + 512], stg[k][:, :nk, :]), r=[b_stg[k]], w=[b_dst])


def _ph_m5(self, i, kind):
    nc = self.nc
    sts = [self.streams[(kind, s)] for s in range(self.n_samp)]
    L = sts[0].L; TB = sts[0].TB; NB = sts[0].NB
    with ExitStack() as es:
        Wglu = self.sb(es, "Wglu", [128, 4, 2048], BF16); b_Wglu = Buf()
        Wm2 = self.sb(es, "Wm2", [128, 8, 1024], BF16); b_Wm2 = Buf()
        Wlru = self.sb(es, "Wlru", [128, 4, 1024], BF16); b_Wlru = Buf()
        Wo = self.sb(es, "Wo", [128, 8, 1024], BF16); b_Wo = Buf()
        with ExitStack() as es_w:
            stg = [self.sb(es_w, f"m5stg{k}", [128, 8, 512]) for k in range(2)]; b_stg = [Buf() for _ in range(2)]
            cnt = [0]
            _load_cast(self, es_w, Wglu, b_Wglu, self.s5_wglu[i], 4, 2048, stg, b_stg, cnt)
            _load_cast(self, es_w, Wm2, b_Wm2, self.m2_wout[i], 8, 1024, stg, b_stg, cnt)
            _load_cast(self, es_w, Wlru, b_Wlru, self.lru_wout[i], 4, 1024, stg, b_stg, cnt)
            _load_cast(self, es_w, Wo, b_Wo, self.w_o[i], 8, 1024, stg, b_stg, cnt)
            self.fw.barrier()
        Wg = [self.sb(es, f"Wg{k}", [128, 8, 3, 128], BF16) for k in range(2)]; b_Wg = [Buf() for _ in range(2)]
        hTb = self.sb(es, "hTb", [128, 8, TB], BF16); b_hTb = Buf()
        y5b = self.sb(es, "y5b", [128, 4, TB], BF16); b_y5b = Buf()
        ymb = self.sb(es, "ymb", [128, 8, TB], BF16); b_ymb = Buf()
        ylb = self.sb(es, "ylb", [128, 4, TB], BF16); b_ylb = Buf()
        gt = [self.sb(es, f"gt{k}", [128, TB]) for k in range(3)]; b_gt = [Buf() for _ in range(3)]
        sg = self.sb(es, "sgm5", [128, TB]); b_sg = Buf()
        mt = self.sb(es, "mt", [128, TB]); b_mt = Buf()
        tt = self.sb(es, "ttm5", [128, TB]); b_tt = Buf()
        mrg = self.sb(es, "mrg", [128, 8, TB], BF16); b_mrg = Buf()
        xt = [self.sb(es, f"xtm5{k}", [128, D]) for k in range(2)]; b_xt = [Buf() for _ in range(2)]
        tmp = self.sb(es, "tmpm5", [128, 512]); b_tmp = Buf()
        pp = [self.ps(es, f"pp{k}", [128, 512]) for k in range(8)]; b_pp = [Buf() for _ in range(8)]
        npp = [0]

        def nxt():
            k = npp[0] % 8; npp[0] += 1
            return pp[k], b_pp[k]

        ng = 0; nx = 0
        for st in sts:
            src = st.src if st.first else st.res
            gate_row = self.gb[:, st.v, 0, :]
            for tb in range(NB):
                ts_ = slice(tb * TB, (tb + 1) * TB)
                self.dma(hTb[:], st.hTs[:, :, ts_], r=[st.b_hTs], w=[b_hTb])
                self.dma(y5b[:], st.ys5[:, ts_].rearrange("(kc p) t -> p kc t", p=128), r=st.b_ys5, w=[b_y5b])
                self.dma(ymb[:], st.yssd[:, ts_].rearrange("(kc p) t -> p kc t", p=128), r=st.b_yssd, w=[b_ymb])
                self.dma(ylb[:], st.ylru[:, ts_].rearrange("(kc p) t -> p kc t", p=128), r=st.b_ylru, w=[b_ylb])
                for nch in range(8):
                    kg = ng % 2; ng += 1
                    ncs = slice(nch * 128, (nch + 1) * 128)
                    self.dma(Wg[kg][:], self.wg_bf.rearrange("p k (j n) -> p k j n", j=3)[:, :, :, ncs], r=[self.b_wg_bf], w=[b_Wg[kg]])
                    for j in range(3):
                        p_, bp_ = nxt()
                        for kc in range(8):
                            self.T(lambda: nc.tensor.matmul(p_[:, :TB], Wg[kg][:, kc, j, :], hTb[:, kc, :], start=(kc == 0), stop=(kc == 7)),
                                   r=[b_Wg[kg], b_hTb], w=[bp_])
                        self.A(lambda: nc.scalar.activation(out=gt[j][:], in_=p_[:, :TB], func=AF.Sigmoid), r=[bp_], w=[b_gt[j]])
                    pv_, bpv = nxt(); pg_, bpg = nxt()
                    for kc in range(4):
                        self.T(lambda: nc.tensor.matmul(pv_[:, :TB], Wglu[:, kc, ncs], y5b[:, kc, :], start=(kc == 0), stop=(kc == 3)),
                               r=[b_Wglu, b_y5b], w=[bpv])
                    for kc in range(4):
                        self.T(lambda: nc.tensor.matmul(pg_[:, :TB], Wglu[:, kc, 1024 + nch * 128:1024 + (nch + 1) * 128], y5b[:, kc, :],
                                                        start=(kc == 0), stop=(kc == 3)),
                               r=[b_Wglu, b_y5b], w=[bpg])
                    self.A(lambda: nc.scalar.activation(out=sg[:], in_=pg_[:, :TB], func=AF.Sigmoid), r=[bpg], w=[b_sg])
                    self.V(lambda: nc.vector.tensor_tensor(mt[:], pv_[:, :TB], sg[:], ALU.mult), r=[bpv, b_sg], w=[b_mt])
                    self.G(lambda: nc.gpsimd.tensor_tensor(mt[:], mt[:], gt[0][:], ALU.mult), r=[b_mt, b_gt[0]], w=[b_mt])
                    pb_, bpb = nxt()
                    for kc in range(8):
                        self.T(lambda: nc.tensor.matmul(pb_[:, :TB], Wm2[:, kc, ncs], ymb[:, kc, :], start=(kc == 0), stop=(kc == 7)),
                               r=[b_Wm2, b_ymb], w=[bpb])
                    self.V(lambda: nc.vector.tensor_tensor(tt[:], pb_[:, :TB], gt[1][:], ALU.mult), r=[bpb, b_gt[1]], w=[b_tt])
                    self.G(lambda: nc.gpsimd.tensor_tensor(mt[:], mt[:], tt[:], ALU.add), r=[b_mt, b_tt], w=[b_mt])
                    pc_, bpc = nxt()
                    for kc in range(4):
                        self.T(lambda: nc.tensor.matmul(pc_[:, :TB], Wlru[:, kc, ncs], ylb[:, kc, :], start=(kc == 0), stop=(kc == 3)),
                               r=[b_Wlru, b_ylb], w=[bpc])
                    self.V(lambda: nc.vector.tensor_tensor(tt[:], pc_[:, :TB], gt[2][:], ALU.mult), r=[bpc, b_gt[2]], w=[b_tt])
                    self.G(lambda: nc.gpsimd.tensor_tensor(mrg[:, nch, :], mt[:], tt[:], ALU.add), r=[b_mt, b_tt], w=[b_mrg])
                for q in range(TB // 128):
                    t = tb * (TB // 128) + q
                    kx = nx % 2; nx += 1
                    self.dma(xt[kx][:], src[t * 128:(t + 1) * 128, :], r=[st.b_res[t]], w=[b_xt[kx]])
                    for nb in range(2):
                        po_, bpo = nxt()
                        for kc in range(8):
                            self.T(lambda: nc.tensor.matmul(po_[:], mrg[:, kc, q * 128:(q + 1) * 128], Wo[:, kc, nb * 512:(nb + 1) * 512],
                                                            start=(kc == 0), stop=(kc == 7)),
                                   r=[b_mrg, b_Wo], w=[bpo])
                        self.V(lambda: nc.vector.tensor_tensor(tmp[:], po_[:], gate_row[:, nb * 512:(nb + 1) * 512], ALU.mult), r=[bpo, self.b_gb], w=[b_tmp])
                        self.G(lambda: nc.gpsimd.tensor_tensor(xt[kx][:, nb * 512:(nb + 1) * 512], xt[kx][:, nb * 512:(nb + 1) * 512], tmp[:], ALU.add),
                               r=[b_xt[kx], b_tmp], w=[b_xt[kx]])
                    self.dma(st.res[t * 128:(t + 1) * 128, :], xt[kx][:], r=[b_xt[kx]], w=[st.b_res[t]])
            st.first = False
    self.fw.barrier()


KB.m5_setup = _m5_setup
KB.ph_castgates = _ph_castgates
KB.ph_m5 = _ph_m5


def _moe_setup(self):
    self.w_router = self.inp("w_routerT", [4, 128, 8, 16])
    self.moe_w1 = self.inp("moe_w1", [4, 16, 1024, 1024])
    self.moe_w3 = self.inp("moe_w3", [4, 16, 1024, 1024])
    self.moe_w2 = self.inp("moe_w2", [4, 16, 1024, 1024])
    self.iota_in = self.inp("iota_c", [128, 4])
    self.reg_bc = {256: self.nc.gpsimd.to_reg(255), 4096: self.nc.gpsimd.to_reg(4095)}
    for st in self.streams.values():
        L = st.L
        st.h2 = self.scratch(f"h2_{st.kind}{st.s}", [L, 1024], BF16)
        st.b_h2 = Buf()
        st.affd = self.scratch(f"affd_{st.kind}{st.s}", [L, 16])
        st.b_affd = Buf()
        st.cum = self.scratch(f"cum_{st.kind}{st.s}", [16, L])
        st.b_cum = Buf()


def _rowbcast(self, es, dst, b_dst, col8, rbufs, pbank, b_pbank, dgs, b_dgs):
    nc = self.nc
    for half in range(2):
        for q in range(4):
            kc = half * 4 + q
            d = q % 2
            self.G(lambda: nc.gpsimd.tensor_scalar(dgs[d][:], self.ident_f[:], col8[:, kc:kc + 1], None, ALU.mult),
                   r=[self.b_const] + rbufs, w=[b_dgs[d]])
            self.T(lambda: nc.tensor.matmul(pbank[:, q * 128:(q + 1) * 128], self.ones_f[:], dgs[d][:], start=True, stop=True),
                   r=[self.b_const, b_dgs[d]], w=[b_pbank])
        self.A(lambda: nc.scalar.copy(dst[:, half * 512:(half + 1) * 512], pbank[:]), r=[b_pbank], w=[b_dst])


def _ph_moe(self, i, kind):
    nc = self.nc
    sts = [self.streams[(kind, s)] for s in range(self.n_samp)]
    L = sts[0].L; NT = sts[0].NT
    KCAP = L // 8
    NCT = max(1, KCAP // 128)
    with ExitStack() as es0:
        affT = self.sb(es0, "affT", [64, L]); b_affT = Buf()
        self.V(lambda: nc.vector.memset(affT[:], 0.0), w=[b_affT])
        with ExitStack() as es:
            Wr = self.sb(es, "Wr", [128, 8, 16]); b_Wr = Buf()
            self.dma(Wr[:], self.w_router[i], w=[b_Wr])
            scrow = self.sb(es, "scrow", [128, D]); shrow = self.sb(es, "shrow", [128, D]); b_rows = Buf()
            dgs = [self.sb(es, f"dgs{k}", [128, 128]) for k in range(2)]; b_dgs = [Buf() for _ in range(2)]
            pbk = self.ps(es, "pbk", [128, 512]); b_pbk = Buf()
            xt = [self.sb(es, f"xte{k}", [128, D]) for k in range(2)]; b_xt = [Buf() for _ in range(2)]
            h2b = [self.sb(es, f"h2b{k}", [128, D], BF16) for k in range(2)]; b_h2b = [Buf() for _ in range(2)]
            junk = self.sb(es, "junke", [128, D]); b_junk = Buf()
            ssq = [self.sb(es, f"ssqe{k}", [128, 2]) for k in range(2)]; b_ssq = [Buf() for _ in range(2)]
            h2T = self.sb(es, "h2T", [128, 8, 128]); b_h2T = Buf()
            ptr = [self.ps(es, f"ptre{k}", [128, 4, 128]) for k in range(2)]; b_ptr = [Buf() for _ in range(2)]
            plg = self.ps(es, "plg", [128, 16]); b_plg = Buf()
            paf = self.ps(es, "paf", [64, 512]); b_paf = Buf()
            AFF = self.sb(es, "AFF", [128, NT, 16]); b_AFF = Buf()
            sm = self.sb(es, "sm", [128, 4]); b_sm = Buf()
            ex = self.sb(es, "ex", [128, 16]); b_ex = Buf()
            for s, st in enumerate(sts):
                _rowbcast(self, es, scrow, b_rows, self.nsc[:, st.v, 2, :], [self.b_nsc], pbk, b_pbk, dgs, b_dgs)
                _rowbcast(self, es, shrow, b_rows, self.nsc[:, st.v, 3, :], [self.b_nsc], pbk, b_pbk, dgs, b_dgs)
                for t in range(NT):
                    k = t % 2
                    self.dma(xt[k][:], st.res[t * 128:(t + 1) * 128, :], r=[st.b_res[t]], w=[b_xt[k]])
                    self.A(lambda: nc.scalar.activation(out=junk[:], in_=xt[k][:], func=AF.Square, accum_out=ssq[k][:, 0:1]),
                           r=[b_xt[k]], w=[b_junk, b_ssq[k]])
                    self.A(lambda: nc.scalar.activation(out=ssq[k][:, 1:2], in_=ssq[k][:, 0:1], func=AF.Sqrt, scale=1.0 / D, bias=1e-6),
                           r=[b_ssq[k]], w=[b_ssq[k]])
                    self.V(lambda: nc.vector.reciprocal(ssq[k][:, 1:2], ssq[k][:, 1:2]), r=[b_ssq[k]], w=[b_ssq[k]])
                    self.V(lambda: nc.vector.scalar_tensor_tensor(xt[k][:], xt[k][:], ssq[k][:, 1:2], scrow[:], ALU.mult, ALU.mult),
                           r=[b_xt[k], b_ssq[k], b_rows], w=[b_xt[k]])
                    self.G(lambda: nc.gpsimd.tensor_tensor(xt[k][:], xt[k][:], shrow[:], ALU.add), r=[b_xt[k], b_rows], w=[b_xt[k]])
                    self.A(lambda: nc.scalar.copy(h2b[k][:], xt[k][:]), r=[b_xt[k]], w=[b_h2b[k]])
                    self.dma(st.h2[t * 128:(t + 1) * 128, :], h2b[k][:], r=[b_h2b[k]], w=[st.b_h2])
                    for half in range(2):
                        for q in range(4):
                            kc = half * 4 + q
                            self.T(lambda: nc.tensor.matmul(ptr[half][:, q, :], xt[k][:, kc * 128:(kc + 1) * 128], self.ident_f[:], start=True, stop=True),
                                   r=[b_xt[k], self.b_const], w=[b_ptr[half]])
                        if half == 0:
                            self.V(lambda: nc.vector.tensor_copy(h2T[:, 0:4, :], ptr[0][:]), r=[b_ptr[0]], w=[b_h2T])
                        else:
                            self.A(lambda: nc.scalar.copy(h2T[:, 4:8, :], ptr[1][:]), r=[b_ptr[1]], w=[b_h2T])
                    for kc in range(8):
                        self.T(lambda: nc.tensor.matmul(plg[:], h2T[:, kc, :], Wr[:, kc, :], start=(kc == 0), stop=(kc == 7)),
                               r=[b_h2T, b_Wr], w=[b_plg])
                    self.V(lambda: nc.vector.reduce_max(sm[:, 0:1], plg[:], axis=AX.X), r=[b_plg], w=[b_sm])
                    self.V(lambda: nc.vector.tensor_scalar(sm[:, 1:2], sm[:, 0:1], -1.0, None, ALU.mult), r=[b_sm], w=[b_sm])
                    self.A(lambda: nc.scalar.activation(out=ex[:], in_=plg[:], func=AF.Exp, bias=sm[:, 1:2], accum_out=sm[:, 2:3]),
                           r=[b_plg, b_sm], w=[b_ex, b_sm])
                    self.V(lambda: nc.vector.reciprocal(sm[:, 3:4], sm[:, 2:3]), r=[b_sm], w=[b_sm])
                    self.V(lambda: nc.vector.tensor_scalar(AFF[:, t, :], ex[:], sm[:, 3:4], None, ALU.mult), r=[b_ex, b_sm], w=[b_AFF])
                self.dma(st.affd.rearrange("(t p) e -> p t e", p=128), AFF[:], r=[b_AFF], w=[st.b_affd])
                TBt = min(4, NT)
                for t0 in range(0, NT, TBt):
                    for q in range(TBt):
                        self.T(lambda: nc.tensor.matmul(paf[s * 32:s * 32 + 16, q * 128:(q + 1) * 128], AFF[:, t0 + q, :], self.ident_f[:], start=True, stop=True),
                               r=[b_AFF, self.b_const], w=[b_paf])
                    self.V(lambda: nc.vector.tensor_copy(affT[s * 32:s * 32 + 16, t0 * 128:(t0 + TBt) * 128], paf[s * 32:s * 32 + 16, 0:TBt * 128]),
                           r=[b_paf], w=[b_affT])
        self.fw.barrier()
        with ExitStack() as es:
            msk = self.sb(es, "msk", [64, L]); b_msk = Buf()
            cum = self.sb(es, "cumt", [64, L]); b_cumt = Buf()
            lh = self.sb(es, "lh", [64, 8]); b_lh = Buf()
            lo, hi, mid, cnt, ge, tmp = (lh[:, j:j + 1] for j in range(6))
            self.V(lambda: nc.vector.memset(lo, 0.0), w=[b_lh])
            self.V(lambda: nc.vector.memset(hi, 1.0), w=[b_lh])
            for it in range(34):
                self.V(lambda: nc.vector.tensor_tensor(mid, lo, hi, ALU.add), r=[b_lh], w=[b_lh])
                self.V(lambda: nc.vector.tensor_scalar(mid, mid, 0.5, None, ALU.mult), r=[b_lh], w=[b_lh])
                self.V(lambda: nc.vector.tensor_scalar(msk[:], affT[:], mid, None, ALU.is_ge), r=[b_affT, b_lh], w=[b_msk])
                self.V(lambda: nc.vector.reduce_sum(cnt, msk[:], axis=AX.X), r=[b_msk], w=[b_lh])
                self.V(lambda: nc.vector.tensor_scalar(ge, cnt, float(KCAP) - 0.5, None, ALU.is_ge), r=[b_lh], w=[b_lh])
                self.V(lambda: nc.vector.tensor_tensor(tmp, mid, lo, ALU.subtract), r=[b_lh], w=[b_lh])
                self.V(lambda: nc.vector.tensor_tensor(tmp, tmp, ge, ALU.mult), r=[b_lh], w=[b_lh])
                self.V(lambda: nc.vector.tensor_tensor(lo, lo, tmp, ALU.add), r=[b_lh], w=[b_lh])
                self.V(lambda: nc.vector.tensor_tensor(tmp, hi, mid, ALU.subtract), r=[b_lh], w=[b_lh])
                self.V(lambda: nc.vector.tensor_tensor(tmp, tmp, ge, ALU.mult), r=[b_lh], w=[b_lh])
                self.V(lambda: nc.vector.tensor_tensor(hi, mid, tmp, ALU.add), r=[b_lh], w=[b_lh])
            self.V(lambda: nc.vector.tensor_scalar(msk[:], affT[:], lo, None, ALU.is_ge), r=[b_affT, b_lh], w=[b_msk])
            self.V(lambda: nc.vector.tensor_tensor_scan(cum[:], self.ones_f[0:64, 0:1].to_broadcast([64, L]), msk[:], 0.0, ALU.mult, ALU.add),
                   r=[b_msk, self.b_const], w=[b_cumt])
            for s, st in enumerate(sts):
                self.dma(st.cum, cum[s * 32:s * 32 + 16, :], r=[b_cumt], w=[st.b_cum])
        self.fw.barrier()
    with ExitStack() as es:
        wst = [self.sb(es, f"wst{k}", [128, 8, 512]) for k in range(2)]; b_wst = [Buf() for _ in range(2)]
        W1 = self.sb(es, "W1", [128, 8, 1024], BF16); b_W1 = Buf()
        W3 = self.sb(es, "W3", [128, 8, 1024], BF16); b_W3 = Buf()
        W2 = self.sb(es, "W2", [128, 8, 1024], BF16); b_W2 = Buf()
        iot = self.sb(es, "iot", [128, 4]); b_iot = Buf()
        self.dma(iot[:], self.iota_in, w=[b_iot])
        cumb = self.sb(es, "cumb", [128, L]); b_cumb = Buf()
        cmpj = self.sb(es, "cmpj", [128, L]); b_cmpj = Buf()
        idxf = self.sb(es, "idxf", [128, 4]); b_idxf = Buf()
        idxi = self.sb(es, "idxi", [128, 4], I32); b_idxi = Buf()
        xg = [self.sb(es, f"xg{k}", [128, D], BF16) for k in range(2)]; b_xg = [Buf() for _ in range(2)]
        wts = self.sb(es, "wts", [128, 4, 16]); b_wts = Buf()
        xsT = self.sb(es, "xsT", [128, 8, NCT * 128], BF16); b_xsT = Buf()
        hidT = self.sb(es, "hidT", [128, 8, NCT * 128], BF16); b_hidT = Buf()
        sgl = self.sb(es, "sgl", [128, NCT * 128]); b_sgl = Buf()
        yo = [self.sb(es, f"yoe{k}", [128, D]) for k in range(2)]; b_yo = [Buf() for _ in range(2)]
        ptx = self.ps(es, "ptx", [128, 8, 128], BF16); b_ptx = Buf()
        pe_ = [self.ps(es, f"pex{k}", [128, 512]) for k in range(6)]; b_pe = [Buf() for _ in range(6)]
        b_scat = [Buf() for _ in sts]
        for k in range(2):
            self.V(lambda: nc.vector.memset(xg[k][:], 0.0), w=[b_xg[k]])
        self.V(lambda: nc.vector.memset(wts[:], 0.0), w=[b_wts])
        CN = NCT * 128
        npe = [0]

        def nxt():
            k = npe[0] % 6; npe[0] += 1
            return pe_[k], b_pe[k]

        cnt = [0]; nxg = 0; nyo = 0
        for e in range(16):
            _load_cast(self, es, W1, b_W1, self.moe_w1[i, e], 8, 1024, wst, b_wst, cnt)
            _load_cast(self, es, W3, b_W3, self.moe_w3[i, e], 8, 1024, wst, b_wst, cnt)
            _load_cast(self, es, W2, b_W2, self.moe_w2[i, e], 8, 1024, wst, b_wst, cnt)
            for s, st in enumerate(sts):
                self.dma(cumb[:], st.cum[e:e + 1, :].partition_broadcast(128), r=[st.b_cum], w=[b_cumb])
                for ct in range(NCT):
                    self.V(lambda: nc.vector.tensor_scalar(cmpj[:], cumb[:], iot[:, ct:ct + 1], None, ALU.is_le), r=[b_cumb, b_iot], w=[b_cmpj])
                    self.V(lambda: nc.vector.reduce_sum(idxf[:, ct:ct + 1], cmpj[:], axis=AX.X), r=[b_cmpj], w=[b_idxf])
                self.V(lambda: nc.vector.tensor_copy(idxi[:, 0:NCT], idxf[:, 0:NCT]), r=[b_idxf], w=[b_idxi])
                for ct in range(NCT):
                    kx = nxg % 2; nxg += 1
                    self.fw.idma(reads=[b_idxi, st.b_h2], writes=[b_xg[kx]],
                                 out=xg[kx][:], out_offset=None, in_=st.h2[:, :],
                                 in_offset=bass.IndirectOffsetOnAxis(ap=idxi[:, ct:ct + 1], axis=0),
                                 bounds_check=self.reg_bc[L], oob_is_err=False)
                    self.fw.idma(reads=[b_idxi, st.b_affd], writes=[b_wts],
                                 out=wts[:, ct, :], out_offset=None, in_=st.affd[:, :],
                                 in_offset=bass.IndirectOffsetOnAxis(ap=idxi[:, ct:ct + 1], axis=0),
                                 bounds_check=self.reg_bc[L], oob_is_err=False)
                    for kc in range(8):
                        self.T(lambda: nc.tensor.transpose(ptx[:, kc, :], xg[kx][:, kc * 128:(kc + 1) * 128], self.ident_b[:]),
                               r=[b_xg[kx], self.b_const], w=[b_ptx])
                    self.A(lambda: nc.scalar.copy(xsT[:, :, ct * 128:(ct + 1) * 128], ptx[:]), r=[b_ptx], w=[b_xsT])
                for fch in range(8):
                    fs = slice(fch * 128, (fch + 1) * 128)
                    p1, bp1 = nxt(); p3, bp3 = nxt()
                    for kc in range(8):
                        self.T(lambda: nc.tensor.matmul(p1[:, :CN], W1[:, kc, fs], xsT[:, kc, :], start=(kc == 0), stop=(kc == 7)),
                               r=[b_W1, b_xsT], w=[bp1])
                    for kc in range(8):
                        self.T(lambda: nc.tensor.matmul(p3[:, :CN], W3[:, kc, fs], xsT[:, kc, :], start=(kc == 0), stop=(kc == 7)),
                               r=[b_W3, b_xsT], w=[bp3])
                    self.A(lambda: nc.scalar.activation(out=sgl[:], in_=p1[:, :CN], func=AF.Silu), r=[bp1], w=[b_sgl])
                    self.V(lambda: nc.vector.tensor_tensor(hidT[:, fch, :], p3[:, :CN], sgl[:], ALU.mult), r=[bp3, b_sgl], w=[b_hidT])
                g2 = self.gb[:, st.v, 1, :]
                for ct in range(NCT):
                    ky = nyo % 2; nyo += 1
                    for nb in range(2):
                        po, bpo = nxt()
                        for fch in range(8):
                            self.T(lambda: nc.tensor.matmul(po[:], hidT[:, fch, ct * 128:(ct + 1) * 128], W2[:, fch, nb * 512:(nb + 1) * 512],
                                                            start=(fch == 0), stop=(fch == 7)),
                                   r=[b_hidT, b_W2], w=[bpo])
                        self.V(lambda: nc.vector.scalar_tensor_tensor(yo[ky][:, nb * 512:(nb + 1) * 512], po[:], wts[:, ct, e:e + 1],
                                                                      g2[:, nb * 512:(nb + 1) * 512], ALU.mult, ALU.mult),
                               r=[bpo, b_wts, self.b_gb], w=[b_yo[ky]])
                    self.fw.idma(reads=[b_idxi, b_yo[ky]], writes=[b_scat[s]],
                                 out=st.res[:, :], out_offset=bass.IndirectOffsetOnAxis(ap=idxi[:, ct:ct + 1], axis=0),
                                 in_=yo[ky][:], in_offset=None, bounds_check=self.reg_bc[L], oob_is_err=False, compute_op=ALU.add)
    self.fw.barrier()


def _ph_final(self, st, out_ap, fng_in):
    nc = self.nc
    with ExitStack() as es:
        grow = self.sb(es, "fgrow", [128, D]); b_grow = Buf()
        self.dma(grow[:], fng_in.partition_broadcast(128), w=[b_grow])
        xt = [self.sb(es, f"xtf{k}", [128, D]) for k in range(3)]; b_xt = [Buf() for _ in range(3)]
        junk = self.sb(es, "junkf", [128, D]); b_junk = Buf()
        ssq = [self.sb(es, f"ssqf{k}", [128, 2]) for k in range(3)]; b_ssq = [Buf() for _ in range(3)]
        for t in range(st.NT):
            k = t % 3
            self.dma(xt[k][:], st.res[t * 128:(t + 1) * 128, :], r=[st.b_res[t]], w=[b_xt[k]])
            self.A(lambda: nc.scalar.activation(out=junk[:], in_=xt[k][:], func=AF.Square, accum_out=ssq[k][:, 0:1]), r=[b_xt[k]], w=[b_junk, b_ssq[k]])
            self.A(lambda: nc.scalar.activation(out=ssq[k][:, 1:2], in_=ssq[k][:, 0:1], func=AF.Sqrt, scale=1.0 / D, bias=1e-6), r=[b_ssq[k]], w=[b_ssq[k]])
            self.V(lambda: nc.vector.reciprocal(ssq[k][:, 1:2], ssq[k][:, 1:2]), r=[b_ssq[k]], w=[b_ssq[k]])
            self.V(lambda: nc.vector.scalar_tensor_tensor(xt[k][:], xt[k][:], ssq[k][:, 1:2], grow[:], ALU.mult, ALU.mult),
                   r=[b_xt[k], b_ssq[k], b_grow], w=[b_xt[k]])
            self.dma(out_ap[t * 128:(t + 1) * 128, :], xt[k][:], r=[b_xt[k]])
    self.fw.barrier()


KB.moe_setup = _moe_setup
KB.ph_moe = _ph_moe
KB.ph_final = _ph_final


def prep_moe(inp):
    o = {}
    wr = np.asarray(inp["moe_w_router"], np.float32)
    o["w_routerT"] = np.ascontiguousarray(wr.reshape(4, 8, 128, 16).transpose(0, 2, 1, 3))
    for k in ("moe_w1", "moe_w3", "moe_w2"):
        o[k] = np.ascontiguousarray(inp[k], np.float32)
    o["iota_c"] = np.ascontiguousarray((np.arange(128)[:, None] + 128 * np.arange(4)[None, :]).astype(np.float32))
    return o


def _layer(self, i, last=False):
    S = self.n_samp
    self.ph_mod(i)
    self.ph_s5_params(i)
    self.ph_castgates(i)
    kinds = ("c", "x")
    for kind in kinds:
        for s in range(S):
            self.ph_norm_proj(i, self.streams[(kind, s)])
    for kind in kinds:
        self.ph_lru(i, kind)
    for kind in kinds:
        self.ph_s5(i, kind)
    with ExitStack() as es_s:
        self.ssd_st = self.sb(es_s, "ssd_st", [64, 2, S, 1024]); self.b_ssd_st = [[Buf() for _ in range(S)] for _ in range(2)]
        for s in range(S):
            for kind in kinds:
                self.ph_ssd(i, self.streams[(kind, s)])
        self.fw.barrier()
    for kind in kinds:
        if kind == "c" and last:
            continue
        self.ph_m5(i, kind)
        self.ph_moe(i, kind)


KB.layer = _layer


def prep_all(inp):
    o = prep_common(inp)
    o.update(prep_s5(inp)); o.update(prep_ssd(inp)); o.update(prep_moe(inp))
    for k in ("s5_w_glu", "m2_w_out", "lru_w_out", "w_o"):
        o[k] = np.ascontiguousarray(inp[k], np.float32)
    o["fng"] = np.ascontiguousarray(np.asarray(inp["final_norm_g"], np.float32).reshape(1, 1024))
    return o


from concourse.bass_utils import run_bass_kernel_spmd

N_CORES = 8
N_SAMP = 2


def build_program():
    nc = bass.Bass("TRN2", target_bir_lowering=False)
    kb = KB(nc, n_samp=N_SAMP, dbg=False)
    kb.setup(); kb.s5_setup(); kb.ssd_setup(); kb.m5_setup(); kb.moe_setup()
    fng = kb.inp("fng", [1, D])
    out = nc.dram_tensor("out", [N_SAMP, 4096, D], F32, kind="ExternalOutput").ap()
    for i in range(4):
        kb.layer(i, last=(i == 3))
    for s in range(N_SAMP):
        kb.ph_final(kb.streams[("x", s)], out[s], fng)
    kb.fw.finish()
    return nc, kb


def kernel(**inputs):
    inp = {k: np.asarray(v) for k, v in inputs.items()}
    nc, kb = build_program()
    common = prep_all(inp)
    in_maps = []
    for core in range(N_CORES):
        m = dict(common)
        m.update(prep_core(inp, core, N_SAMP))
        in_maps.append({k: m[k] for k in kb.din})
    res = run_bass_kernel_spmd(nc, in_maps, core_ids=list(range(N_CORES)))
    outs = [np.asarray(r["out"], dtype=np.float32) for r in res.results]
    return np.concatenate(outs, axis=0)
```

```python
import numpy as np
from contextlib import ExitStack
import concourse.bass as bass
import concourse.mybir as mybir

F32 = mybir.dt.float32
BF16 = mybir.dt.bfloat16
I32 = mybir.dt.int32
ALU = mybir.AluOpType
AF = mybir.ActivationFunctionType
AX = mybir.AxisListType


class Buf:
    __slots__ = ("name", "w", "r")

    def __init__(self, name=""):
        self.name = name
        self.w = None
        self.r = {}


class Eng:
    def __init__(self, fw, name, handle, sem, step=1):
        self.fw = fw
        self.name = name
        self.h = handle
        self.sem = sem
        self.count = 0
        self.step = step
        self.seen = {}


class FW:
    def __init__(self, nc, n_dma_sems=24, n_gdma_sems=8):
        self.nc = nc
        self.es = ExitStack()
        mk = lambda n: self.es.enter_context(nc.semaphore(n))
        self.pe = Eng(self, "pe", nc.tensor, mk("s_pe"))
        self.dve = Eng(self, "dve", nc.vector, mk("s_dve"))
        self.act = Eng(self, "act", nc.scalar, mk("s_act"))
        self.pool = Eng(self, "pool", nc.gpsimd, mk("s_pool"))
        self.sp = Eng(self, "sp", nc.sync, mk("s_sp"))
        self.engs = [self.pe, self.dve, self.act, self.pool, self.sp]
        self.dq = [Eng(self, f"dq{i}", None, mk(f"s_dq{i}"), step=16) for i in range(n_dma_sems)]
        self.gq = [Eng(self, f"gq{i}", None, mk(f"s_gq{i}"), step=16) for i in range(n_gdma_sems)]
        self.aq = [Eng(self, f"aq{i}", None, mk(f"s_aq{i}"), step=16) for i in range(8)]
        self.dq_i = 0
        self.gq_i = 0
        self.aq_i = 0
        self.n_wait = 0
        self.n_inst = 0

    def _wait(self, eng, e2, c, raw=False):
        if e2 is eng:
            if (not raw) or eng is self.pe or eng is self.sp or eng.count - c > 2:
                return
        if eng.seen.get(e2, 0) >= c:
            return
        eng.h.wait_ge(e2.sem, c)
        eng.seen[e2] = c
        self.n_wait += 1

    def _deps(self, eng, reads, writes):
        for b in reads:
            if b.w is not None:
                self._wait(eng, b.w[0], b.w[1], raw=True)
        for b in writes:
            if b.w is not None:
                self._wait(eng, *b.w)
            for e2, c in b.r.items():
                self._wait(eng, e2, c)

    def _mark(self, tok_eng, tok_c, reads, writes):
        for b in reads:
            if b.r.get(tok_eng, 0) < tok_c:
                b.r[tok_eng] = tok_c
        for b in writes:
            b.w = (tok_eng, tok_c)
            b.r = {}

    def op(self, eng, fn, reads=(), writes=()):
        self._deps(eng, reads, writes)
        inst = fn()
        eng.count += 1
        inst.then_inc(eng.sem, 1)
        self._mark(eng, eng.count, reads, writes)
        self.n_inst += 1
        return inst

    def dma(self, out, in_, reads=(), writes=(), q="sp", **kw):
        if q == "sp":
            issuer = self.sp; pool = self.dq; i = self.dq_i; self.dq_i = (i + 1) % len(pool)
        elif q == "act":
            issuer = self.act; pool = self.aq; i = self.aq_i; self.aq_i = (i + 1) % len(pool)
        else:
            issuer = self.pool; pool = self.gq; i = self.gq_i; self.gq_i = (i + 1) % len(pool)
        tok = pool[i]
        if tok.count > 0:
            self._wait(issuer, tok, tok.count)
        self._deps(issuer, reads, writes)
        inst = issuer.h.dma_start(out=out, in_=in_, **kw)
        tok.count += 16
        inst.then_inc(tok.sem, 16)
        self._mark(tok, tok.count, reads, writes)
        self.n_inst += 1
        return inst

    def idma(self, reads=(), writes=(), **kw):
        issuer = self.pool; pool = self.gq; i = self.gq_i; self.gq_i = (i + 1) % len(pool)
        tok = pool[i]
        if tok.count > 0:
            self._wait(issuer, tok, tok.count)
        self._deps(issuer, reads, writes)
        inst = issuer.h.indirect_dma_start(**kw)
        tok.count += 16
        inst.then_inc(tok.sem, 16)
        self._mark(tok, tok.count, reads, writes)
        self.n_inst += 1
        return inst

    def barrier(self):
        allq = self.engs + self.dq + self.gq + self.aq
        for e in self.engs:
            for e2 in allq:
                if e2 is not e and e2.count > 0:
                    self._wait(e, e2, e2.count)

    def finish(self):
        self.barrier()

    def close(self):
        self.es.close()


D = 1024
NPJ = 4128
C_U, C_Z, C_XBC, C_DT, C_XL, C_GL, C_G = 0, 512, 1536, 3072, 3104, 3616, 4128
GELU_K = 1.5957691216057308


class Stream:
    pass


class KB:
    def __init__(self, nc, n_samp=2, dbg=False):
        self.nc = nc
        self.fw = FW(nc)
        self.ges = ExitStack()
        self.n_samp = n_samp
        self.din = {}
        self.dbg = dbg
        self._uid = 0

    def inp(self, name, shape, dt=F32):
        t = self.nc.dram_tensor(name, list(shape), dt, kind="ExternalInput")
        self.din[name] = (tuple(shape), dt)
        return t.ap()

    def scratch(self, name, shape, dt=F32, out=False):
        kind = "ExternalOutput" if (out or self.dbg) else "Internal"
        return self.nc.dram_tensor(name, list(shape), dt, kind=kind).ap()

    def sb(self, es, name, shape, dt=F32):
        self._uid += 1
        return es.enter_context(self.nc.sbuf_tensor(f"{name}_{self._uid}", list(shape), dt))

    def ps(self, es, name, shape, dt=F32):
        self._uid += 1
        return es.enter_context(self.nc.psum_tensor(f"{name}_{self._uid}", list(shape), dt))

    def V(self, fn, r=(), w=()):
        return self.fw.op(self.fw.dve, fn, r, w)

    def A(self, fn, r=(), w=()):
        return self.fw.op(self.fw.act, fn, r, w)

    def G(self, fn, r=(), w=()):
        return self.fw.op(self.fw.pool, fn, r, w)

    def T(self, fn, r=(), w=()):
        return self.fw.op(self.fw.pe, fn, r, w)

    def dma(self, out, in_, r=(), w=(), q="sp", **kw):
        return self.fw.dma(out, in_, r, w, q=q, **kw)

    def setup(self):
        nc = self.nc
        es = self.ges
        S = self.n_samp
        self.x_in = self.inp("x", [S, 4096, D])
        self.c_in = self.inp("ctx", [S, 256, D])
        self.cvec_in = self.inp("cvec", [128, 8, 3])
        self.w_mod = self.inp("w_mod", [4, D, 6144])
        self.b_modT = self.inp("b_modT", [4, 128, 48])
        self.n1gT = self.inp("n1gT", [4, 128, 8])
        self.n2gT = self.inp("n2gT", [4, 128, 8])
        self.w_in = self.inp("w_in", [4, D, 7200])
        self.ident_in = self.inp("ident_in", [128, 128])
        self.lru_cw = self.inp("lru_cw", [4, 128, 4, 4])
        self.lru_cb = self.inp("lru_cb", [4, 128, 4])
        self.lru_wa = self.inp("lru_wa", [4, 2, 4, 128, 128])
        self.lru_wx = self.inp("lru_wx", [4, 2, 4, 128, 128])
        self.lru_bal = self.inp("lru_bal", [4, 128, 3, 2, 4])
        self.ident_f = self.sb(es, "ident_f", [128, 128]); self.b_const = Buf("const")
        self.ident_b = self.sb(es, "ident_b", [128, 128], BF16)
        self.ones_f = self.sb(es, "ones_f", [128, 128])
        self.dma(self.ident_f[:], self.ident_in, w=[self.b_const])
        self.V(lambda: nc.vector.tensor_copy(self.ident_b[:], self.ident_f[:]), r=[self.b_const], w=[self.b_const])
        self.V(lambda: nc.vector.memset(self.ones_f[:], 1.0), w=[self.b_const])
        self.scv = self.sb(es, "scv", [128, 8, 3]); self.b_scv = Buf("scv")
        self.modc = self.sb(es, "modc", [128, 48, 3]); self.b_modc = Buf("modc")
        self.nsc = self.sb(es, "nsc", [128, 3, 4, 8]); self.b_nsc = Buf("nsc")
        self.gb = self.sb(es, "gb", [128, 3, 2, D]); self.b_gb = Buf("gb")
        self.dma(self.scv[:], self.cvec_in, w=[self.b_scv])
        self.A(lambda: nc.scalar.activation(out=self.scv[:], in_=self.scv[:], func=AF.Silu), r=[self.b_scv], w=[self.b_scv])
        self.lru_st = self.sb(es, "lru_st", [128, 4, 2, S]); self.b_lru_st = Buf("lru_st")
        self.streams = {}
        for kind, L, src in (("c", 256, self.c_in), ("x", 4096, self.x_in)):
            for s in range(S):
                st = Stream()
                st.kind, st.s, st.L = kind, s, L
                st.NT = L // 128
                st.TB = min(512, L)
                st.NB = L // st.TB
                st.v = 2 if kind == "c" else s
                st.src = src[s]
                st.res = self.scratch(f"res_{kind}{s}", [L, D])
                st.b_res = [Buf() for _ in range(st.NT)]
                st.first = True
                st.PJ = self.scratch(f"pj_{kind}{s}", [NPJ, L])
                st.b_pj = {}
                st.hTs = self.scratch(f"hTs_{kind}{s}", [128, 8, L], BF16)
                st.b_hTs = Buf()
                st.ylru = self.scratch(f"ylru_{kind}{s}", [512, L], BF16)
                st.b_ylru = [Buf() for _ in range(4)]
                st.ys5 = self.scratch(f"ys5_{kind}{s}", [512, L], BF16)
                st.b_ys5 = [Buf() for _ in range(4)]
                st.yssd = self.scratch(f"yssd_{kind}{s}", [1024, L], BF16)
                st.b_yssd = [Buf() for _ in range(8)]
                self.streams[(kind, s)] = st

    def pjbuf(self, st, r0):
        if r0 not in st.b_pj:
            st.b_pj[r0] = Buf(f"pj{r0}")
        return st.b_pj[r0]

    def ph_mod(self, i):
        nc = self.nc
        with ExitStack() as es:
            wm = [self.sb(es, f"wm{k}", [128, 8, 512]) for k in range(2)]
            b_wm = [Buf() for _ in range(2)]
            pm = [self.ps(es, f"pmod{k}", [128, 4, 4]) for k in range(2)]
            b_pm = [Buf() for _ in range(2)]
            bm = self.sb(es, "bm", [128, 48]); b_bm = Buf()
            g12 = self.sb(es, "g12", [128, 2, 8]); b_g12 = Buf()
            self.dma(bm[:], self.b_modT[i], w=[b_bm])
            self.dma(g12[:, 0, :], self.n1gT[i], w=[b_g12])
            self.dma(g12[:, 1, :], self.n2gT[i], w=[b_g12])
            for nb in range(12):
                k = nb % 2
                self.dma(wm[k][:], self.w_mod[i][:, nb * 512:(nb + 1) * 512].rearrange("(kc p) n -> p kc n", p=128), w=[b_wm[k]])
                for j in range(4):
                    for kc in range(8):
                        self.T(lambda: nc.tensor.matmul(pm[k][:, j, 0:3], wm[k][:, kc, j * 128:(j + 1) * 128], self.scv[:, kc, :],
                                                        start=(kc == 0), stop=(kc == 7)),
                               r=[b_wm[k], self.b_scv], w=[b_pm[k]])
                for j in range(4):
                    jj = nb * 4 + j
                    self.V(lambda: nc.vector.tensor_scalar(self.modc[:, jj, :], pm[k][:, j, 0:3], bm[:, jj:jj + 1], None, ALU.add),
                           r=[b_pm[k], b_bm], w=[self.b_modc])
            for v in range(3):
                for (o, seg_sc, seg_sh, gi) in ((0, 1, 0, 0), (2, 4, 3, 1)):
                    self.V(lambda: nc.vector.scalar_tensor_tensor(self.nsc[:, v, o, :], self.modc[:, seg_sc * 8:seg_sc * 8 + 8, v], 1.0,
                                                                  g12[:, gi, :], ALU.add, ALU.mult),
                           r=[self.b_modc, b_g12], w=[self.b_nsc])
                    self.V(lambda: nc.vector.tensor_copy(self.nsc[:, v, o + 1, :], self.modc[:, seg_sh * 8:seg_sh * 8 + 8, v]),
                           r=[self.b_modc], w=[self.b_nsc])
            dg = [self.sb(es, f"dg{k}", [128, 128]) for k in range(2)]; b_dg = [Buf() for _ in range(2)]
            pb = [self.ps(es, f"pb{k}", [128, 512]) for k in range(2)]; b_pb = [Buf() for _ in range(2)]
            n = 0
            for v in range(3):
                for gi, seg in ((0, 2), (1, 5)):
                    for half in range(2):
                        k = n % 2; n += 1
                        for q in range(4):
                            kc = half * 4 + q
                            d = (n * 4 + q) % 2
                            self.G(lambda: nc.gpsimd.tensor_scalar(dg[d][:], self.ident_f[:], self.modc[:, seg * 8 + kc, v:v + 1], None, ALU.mult),
                                   r=[self.b_const, self.b_modc], w=[b_dg[d]])
                            self.T(lambda: nc.tensor.matmul(pb[k][:, q * 128:(q + 1) * 128], self.ones_f[:], dg[d][:], start=True, stop=True),
                                   r=[self.b_const, b_dg[d]], w=[b_pb[k]])
                        self.A(lambda: nc.scalar.copy(self.gb[:, v, gi, half * 512:(half + 1) * 512], pb[k][:]), r=[b_pb[k]], w=[self.b_gb])
        self.fw.barrier()

    def ph_norm_proj(self, i, st):
        nc = self.nc
        L, NT, TB, NB = st.L, st.NT, st.TB, st.NB
        src = st.src if st.first else st.res
        with ExitStack() as es:
            hT = self.sb(es, "hT", [128, 8, L], BF16)
            b_hT = [Buf() for _ in range(NT)]
            with ExitStack() as es1:
                xt = [self.sb(es1, f"xt{k}", [128, D]) for k in range(2)]; b_xt = [Buf() for _ in range(2)]
                xn = [self.sb(es1, f"xn{k}", [128, D], BF16) for k in range(2)]; b_xn = [Buf() for _ in range(2)]
                junk = self.sb(es1, "junk", [128, D], BF16); b_junk = Buf()
                ssq = [self.sb(es1, f"ssq{k}", [128, 1]) for k in range(2)]; b_ssq = [Buf() for _ in range(2)]
                ptr = [self.ps(es1, f"ptr{k}", [128, 8, 128], BF16) for k in range(2)]; b_ptr = [Buf() for _ in range(2)]
                for t in range(NT):
                    k = t % 2
                    self.dma(xt[k][:], src[t * 128:(t + 1) * 128, :], r=[st.b_res[t]], w=[b_xt[k]])
                    self.A(lambda: nc.scalar.activation(out=junk[:], in_=xt[k][:], func=AF.Square, accum_out=ssq[k][:]),
                           r=[b_xt[k]], w=[b_junk, b_ssq[k]])
                    self.A(lambda: nc.scalar.activation(out=ssq[k][:], in_=ssq[k][:], func=AF.Sqrt, scale=1.0 / D, bias=1e-6),
                           r=[b_ssq[k]], w=[b_ssq[k]])
                    self.V(lambda: nc.vector.reciprocal(ssq[k][:], ssq[k][:]), r=[b_ssq[k]], w=[b_ssq[k]])
                    self.A(lambda: nc.scalar.activation(out=xn[k][:], in_=xt[k][:], func=AF.Copy, scale=ssq[k][:, 0:1]),
                           r=[b_xt[k], b_ssq[k]], w=[b_xn[k]])
                    for kc in range(8):
                        self.T(lambda: nc.tensor.transpose(ptr[k][:, kc, :], xn[k][:, kc * 128:(kc + 1) * 128], self.ident_b[:]),
                               r=[b_xn[k], self.b_const], w=[b_ptr[k]])
                    for kc in range(8):
                        o = hT[:, kc, t * 128:(t + 1) * 128]
                        sc = self.nsc[:, st.v, 0, kc:kc + 1]; sh = self.nsc[:, st.v, 1, kc:kc + 1]
                        if kc % 2 == 0:
                            self.V(lambda: nc.vector.tensor_scalar(o, ptr[k][:, kc, :], sc, sh, ALU.mult, ALU.add),
                                   r=[b_ptr[k], self.b_nsc], w=[b_hT[t]])
                        else:
                            self.A(lambda: nc.scalar.activation(out=o, in_=ptr[k][:, kc, :], func=AF.Identity, scale=sc, bias=sh),
                                   r=[b_ptr[k], self.b_nsc], w=[b_hT[t]])
                self.dma(st.hTs, hT[:], r=b_hT, w=[st.b_hTs])
            wf = self.sb(es, "wf", [128, 8, 512]); b_wf = Buf()
            wb = [self.sb(es, f"wb{k}", [128, 8, 512], BF16) for k in range(2)]; b_wb = [Buf() for _ in range(2)]
            stg = [self.sb(es, f"stg{k}", [128, L]) for k in range(2)]; b_stg = [Buf() for _ in range(2)]
            pm = [self.ps(es, f"pm{k}", [128, 512]) for k in range(4)]; b_pm = [Buf() for _ in range(4)]
            zst = [self.sb(es, f"zst{k}", [128, 512]) for k in range(2)]; b_zst = [Buf() for _ in range(2)]
            dts = self.sb(es, "dts", [128, NT, 32]); b_dts = Buf()
            groups = [(c0, 512) for c0 in range(0, 3072, 512)] + [(3072, 32)] + [(c0, 512) for c0 in (3104, 3616)]
            n_ev = 0; n_st = 0
            for gi, (c0, ncol) in enumerate(groups):
                k = gi % 2
                self.dma(wf[:, :, :ncol], self.w_in[i][:, c0:c0 + ncol].rearrange("(kc p) n -> p kc n", p=128), w=[b_wf])
                self.G(lambda: nc.gpsimd.tensor_copy(wb[k][:, :, :ncol], wf[:, :, :ncol]), r=[b_wf], w=[b_wb[k]])
                tokmajor = hasattr(st, "z_tm") and c0 in (512, 1024, 3072)
                if tokmajor:
                    for t in range(NT):
                        pk = n_ev % 4
                        for kc in range(8):
                            self.T(lambda: nc.tensor.matmul(pm[pk][:, :ncol], hT[:, kc, t * 128:(t + 1) * 128], wb[k][:, kc, :ncol],
                                                            start=(kc == 0), stop=(kc == 7)),
                                   r=[b_wb[k], b_hT[t]], w=[b_pm[pk]])
                        if c0 == 3072:
                            self.V(lambda: nc.vector.tensor_copy(dts[:, t, :], pm[pk][:, :32]), r=[b_pm[pk]], w=[b_dts])
                        else:
                            zk = n_ev % 2
                            if n_ev % 2 == 0:
                                self.A(lambda: nc.scalar.copy(zst[zk][:], pm[pk][:, :512]), r=[b_pm[pk]], w=[b_zst[zk]])
                            else:
                                self.V(lambda: nc.vector.tensor_copy(zst[zk][:], pm[pk][:, :512]), r=[b_pm[pk]], w=[b_zst[zk]])
                            self.dma(st.z_tm[t * 128:(t + 1) * 128, c0 - 512:c0], zst[zk][:], r=[b_zst[zk]], w=[st.b_z_tm[t]])
                        n_ev += 1
                    if c0 == 3072:
                        self.dma(st.dt_tm.rearrange("(t p) c -> p t c", p=128), dts[:], r=[b_dts], w=[st.b_dt_tm])
                    continue
                for r0 in range(0, ncol, 128):
                    m = min(128, ncol - r0)
                    sk = n_st % 2; n_st += 1
                    for tb in range(NB):
                        pk = n_ev % 4
                        tiles = range(tb * TB // 128, (tb + 1) * TB // 128)
                        for kc in range(8):
                            self.T(lambda: nc.tensor.matmul(pm[pk][:m, :TB], wb[k][:, kc, r0:r0 + m], hT[:, kc, tb * TB:(tb + 1) * TB],
                                                            start=(kc == 0), stop=(kc == 7)),
                                   r=[b_wb[k]] + [b_hT[t] for t in tiles], w=[b_pm[pk]])
                        if n_ev % 2 == 0:
                            self.A(lambda: nc.scalar.copy(stg[sk][:m, tb * TB:(tb + 1) * TB], pm[pk][:m, :TB]), r=[b_pm[pk]], w=[b_stg[sk]])
                        else:
                            self.V(lambda: nc.vector.tensor_copy(stg[sk][:m, tb * TB:(tb + 1) * TB], pm[pk][:m, :TB]), r=[b_pm[pk]], w=[b_stg[sk]])
                        n_ev += 1
                    self.dma(st.PJ[c0 + r0:c0 + r0 + m, :], stg[sk][:m, :], r=[b_stg[sk]], w=[self.pjbuf(st, c0 + r0)])
        self.fw.barrier()

    def ph_lru(self, i, kind):
        nc = self.nc
        sts = [self.streams[(kind, s)] for s in range(self.n_samp)]
        L = sts[0].L; TB = sts[0].TB; NB = sts[0].NB
        EPS1 = float(np.float32(1.0) - np.float32(1e-6))
        with ExitStack() as es:
            cw = self.sb(es, "cw", [128, 4, 4]); cb = self.sb(es, "cb", [128, 4]); b_cp = Buf()
            bal = self.sb(es, "bal", [128, 3, 2, 4]); b_bal = Buf()
            cc = self.sb(es, "cc", [128, 2, 2, 4]); b_cc = Buf()
            self.dma(cw[:], self.lru_cw[i], w=[b_cp]); self.dma(cb[:], self.lru_cb[i], w=[b_cp])
            self.dma(bal[:], self.lru_bal[i], w=[b_bal])
            self.A(lambda: nc.scalar.activation(out=cc[:, 0], in_=bal[:, 2], func=AF.Exp, scale=-1.0), r=[b_bal], w=[b_cc])
            self.A(lambda: nc.scalar.activation(out=cc[:, 0], in_=cc[:, 0], func=AF.Ln, bias=1.0), r=[b_cc], w=[b_cc])
            self.V(lambda: nc.vector.tensor_scalar(cc[:, 1], cc[:, 0], -16.0, None, ALU.mult), r=[b_cc], w=[b_cc])
            self.V(lambda: nc.vector.tensor_scalar(cc[:, 0], cc[:, 0], -8.0, None, ALU.mult), r=[b_cc], w=[b_cc])
            wa = self.sb(es, "wa", [128, 2, 2, 128]); b_wa = Buf()
            xlp = self.sb(es, "xlp", [128, L + 3]); b_xlp = Buf()
            gg = self.sb(es, "gg", [128, L]); b_gg = Buf()
            xc = self.sb(es, "xc", [128, L]); b_xc = Buf()
            hf = self.sb(es, "hf", [128, L]); b_hf = Buf()
            A1 = self.sb(es, "A1", [128, L]); b_A1 = Buf()
            A2 = self.sb(es, "A2", [128, L]); b_A2 = Buf()
            A3 = self.sb(es, "A3", [128, L]); b_A3 = Buf()
            yb = self.sb(es, "yb", [128, L], BF16); b_yb = Buf()
            pg = [self.ps(es, f"pg{k}", [128, 512]) for k in range(4)]; b_pg = [Buf() for _ in range(4)]
            self.V(lambda: nc.vector.memset(xlp[:, 0:2], 0.0), w=[b_xlp])
            self.V(lambda: nc.vector.memset(xlp[:, L + 2:L + 3], 0.0), w=[b_xlp])
            npg = 0
            for ch in range(4):
                for d in range(2):
                    self.dma(wa[:, d, 0, :], self.lru_wa[i, d, ch], w=[b_wa])
                    self.dma(wa[:, d, 1, :], self.lru_wx[i, d, ch], w=[b_wa])
                for st in sts:
                    r_xl = C_XL + ch * 128; r_gl = C_GL + ch * 128
                    self.dma(xlp[:, 2:L + 2], st.PJ[r_xl:r_xl + 128, :], r=[self.pjbuf(st, r_xl)], w=[b_xlp])
                    self.dma(gg[:], st.PJ[r_gl:r_gl + 128, :], r=[self.pjbuf(st, r_gl)], w=[b_gg])
                    self.V(lambda: nc.vector.tensor_scalar(xc[:], xlp[:, 0:L], cw[:, ch, 0:1], cb[:, ch:ch + 1], ALU.mult, ALU.add),
                           r=[b_xlp, b_cp], w=[b_xc])
                    for j in range(1, 4):
                        self.V(lambda: nc.vector.scalar_tensor_tensor(xc[:], xlp[:, j:j + L], cw[:, ch, j:j + 1], xc[:], ALU.mult, ALU.add),
                               r=[b_xlp, b_cp, b_xc], w=[b_xc])
                    self.G(lambda: nc.gpsimd.tensor_tensor(A1[:], gg[:], gg[:], ALU.mult), r=[b_gg], w=[b_A1])
                    self.G(lambda: nc.gpsimd.tensor_scalar(A1[:], A1[:], 0.044715, 1.0, ALU.mult, ALU.add), r=[b_A1], w=[b_A1])
                    self.G(lambda: nc.gpsimd.tensor_tensor(A1[:], A1[:], gg[:], ALU.mult), r=[b_A1, b_gg], w=[b_A1])
                    self.A(lambda: nc.scalar.activation(out=A1[:], in_=A1[:], func=AF.Sigmoid, scale=GELU_K), r=[b_A1], w=[b_A1])
                    self.G(lambda: nc.gpsimd.tensor_tensor(gg[:], gg[:], A1[:], ALU.mult), r=[b_A1, b_gg], w=[b_gg])
                    for d in range(2):
                        for tb in range(NB):
                            sl = slice(tb * TB, (tb + 1) * TB)
                            ka = npg % 4; kx = (npg + 1) % 4; npg += 2
                            self.T(lambda: nc.tensor.matmul(pg[ka][:, :TB], wa[:, d, 0, :], xc[:, sl], start=True, stop=True),
                                   r=[b_wa, b_xc], w=[b_pg[ka]])
                            self.T(lambda: nc.tensor.matmul(pg[kx][:, :TB], wa[:, d, 1, :], xc[:, sl], start=True, stop=True),
                                   r=[b_wa, b_xc], w=[b_pg[kx]])
                            self.A(lambda: nc.scalar.activation(out=A1[:, sl], in_=pg[ka][:, :TB], func=AF.Sigmoid, bias=bal[:, 0, d, ch:ch + 1]),
                                   r=[b_pg[ka], b_bal], w=[b_A1])
                            self.A(lambda: nc.scalar.activation(out=A3[:, sl], in_=pg[kx][:, :TB], func=AF.Sigmoid, bias=bal[:, 1, d, ch:ch + 1]),
                                   r=[b_pg[kx], b_bal], w=[b_A3])
                        self.A(lambda: nc.scalar.activation(out=A2[:], in_=A1[:], func=AF.Exp, scale=cc[:, 1, d, ch:ch + 1]), r=[b_A1, b_cc], w=[b_A2])
                        self.A(lambda: nc.scalar.activation(out=A1[:], in_=A1[:], func=AF.Exp, scale=cc[:, 0, d, ch:ch + 1]), r=[b_A1, b_cc], w=[b_A1])
                        self.V(lambda: nc.vector.tensor_scalar(A2[:], A2[:], EPS1, -1.0, ALU.min, ALU.mult), r=[b_A2], w=[b_A2])
                        self.A(lambda: nc.scalar.activation(out=A2[:], in_=A2[:], func=AF.Sqrt, bias=1.0), r=[b_A2], w=[b_A2])
                        self.G(lambda: nc.gpsimd.tensor_tensor(A3[:], A3[:], xc[:], ALU.mult), r=[b_A3, b_xc], w=[b_A3])
                        self.G(lambda: nc.gpsimd.tensor_tensor(A3[:], A3[:], A2[:], ALU.mult), r=[b_A3, b_A2], w=[b_A3])
                        if kind == "c":
                            init = 0.0; rd = []
                        else:
                            init = self.lru_st[:, ch, d, st.s:st.s + 1]; rd = [self.b_lru_st]
                        if d == 0:
                            self.V(lambda: nc.vector.tensor_tensor_scan(hf[:], A1[:], A3[:], init, ALU.mult, ALU.add),
                                   r=[b_A1, b_A3] + rd, w=[b_hf])
                            if kind == "c":
                                self.G(lambda: nc.gpsimd.tensor_copy(self.lru_st[:, ch, 0, st.s:st.s + 1], hf[:, L - 1:L]), r=[b_hf], w=[self.b_lru_st])
                        else:
                            rev = lambda t: bass.AP(t, L - 1, [[t[:].ap[0][0], 128], [-1, L]])
                            self.V(lambda: nc.vector.tensor_tensor_scan(rev(A2), rev(A1), rev(A3), init, ALU.mult, ALU.add),
                                   r=[b_A1, b_A3] + rd, w=[b_A2])
                            if kind == "c":
                                self.G(lambda: nc.gpsimd.tensor_copy(self.lru_st[:, ch, 1, st.s:st.s + 1], A2[:, 0:1]), r=[b_A2], w=[self.b_lru_st])
                    self.V(lambda: nc.vector.tensor_tensor(A2[:], A2[:], hf[:], ALU.add), r=[b_A2, b_hf], w=[b_A2])
                    self.V(lambda: nc.vector.tensor_tensor(yb[:], A2[:], gg[:], ALU.mult), r=[b_A2, b_gg], w=[b_yb])
                    self.dma(st.ylru[ch * 128:(ch + 1) * 128, :], yb[:], r=[b_yb], w=[st.b_ylru[ch]])
        self.fw.barrier()


def colform(v, nchunk):
    return np.ascontiguousarray(np.asarray(v, np.float32).reshape(nchunk, 128).T)


def prep_common(inp):
    o = {}
    o["w_mod"] = np.ascontiguousarray(inp["w_mod"], np.float32)
    o["b_modT"] = np.stack([colform(inp["b_mod"][i], 48) for i in range(4)])
    o["n1gT"] = np.stack([colform(inp["norm1_g"][i], 8) for i in range(4)])
    o["n2gT"] = np.stack([colform(inp["norm2_g"][i], 8) for i in range(4)])
    o["w_in"] = np.ascontiguousarray(inp["w_in"], np.float32)
    o["ident_in"] = np.eye(128, dtype=np.float32)
    cw = np.asarray(inp["lru_conv_w"], np.float32)
    o["lru_cw"] = np.ascontiguousarray(cw.reshape(4, 4, 4, 128).transpose(0, 3, 2, 1))
    o["lru_cb"] = np.ascontiguousarray(np.asarray(inp["lru_conv_b"], np.float32).reshape(4, 4, 128).transpose(0, 2, 1))
    for nm, key in (("lru_wa", "lru_w_a"), ("lru_wx", "lru_w_x")):
        w = np.asarray(inp[key], np.float32)
        bd = np.zeros((4, 2, 4, 128, 128), np.float32)
        for ch in range(4):
            bd[:, :, ch, 0:64, 0:64] = w[:, :, 2 * ch]
            bd[:, :, ch, 64:128, 64:128] = w[:, :, 2 * ch + 1]
        o[nm] = bd
    bal = np.stack([np.asarray(inp[k], np.float32) for k in ("lru_b_a", "lru_b_x", "lru_lam")], axis=1)
    o["lru_bal"] = np.ascontiguousarray(bal.reshape(4, 3, 2, 4, 128).transpose(0, 4, 1, 2, 3))
    return o


def prep_core(inp, core, n_samp=2):
    o = {}
    sl = slice(core * n_samp, (core + 1) * n_samp)
    o["x"] = np.ascontiguousarray(inp["x"][sl], np.float32)
    o["ctx"] = np.ascontiguousarray(inp["ctx"][sl], np.float32)
    cs = [np.asarray(inp["c"][core * n_samp + s], np.float32) for s in range(n_samp)]
    while len(cs) < 2:
        cs.append(cs[0])
    cs.append(np.asarray(inp["c_ctx"], np.float32))
    o["cvec"] = np.ascontiguousarray(np.stack([colform(c, 8) for c in cs], axis=-1))
    return o


TWO_PI = 6.283185307179586


def _s5_setup(self):
    es = self.ges
    S = self.n_samp
    self.s5_lam = self.inp("s5_lam", [4, 128, 3, 32])
    self.s5_B = self.inp("s5_B", [4, 2, 16, 128, 2, 128])
    self.s5_C = self.inp("s5_C", [4, 2, 16, 128, 2, 128])
    self.s5_dT = self.inp("s5_dT", [4, 128, 4])
    self.s5_st = self.sb(es, "s5_st", [128, 2, 32, S]); self.b_s5_st = Buf("s5_st")
    self.s5_par = self.sb(es, "s5_par", [128, 8, 32]); self.b_s5_par = Buf("s5_par")
    self.s5_W = self.sb(es, "s5_W", [128, 2, 32, 12]); self.b_s5_W = Buf("s5_W")


def _sincos(self, es, out_sin, out_cos, ang, shape, bufs_r, buf_w, tagn):
    nc = self.nc
    kf = self.sb(es, f"kf{tagn}", shape); ki = self.sb(es, f"ki{tagn}", shape, I32); rr = self.sb(es, f"rr{tagn}", shape)
    b = Buf()
    for out, shift in ((out_sin, 0.0), (out_cos, np.pi / 2)):
        self.V(lambda: nc.vector.tensor_scalar(kf[:], ang, shift, 1.0 / TWO_PI, ALU.add, ALU.mult), r=bufs_r, w=[b])
        self.V(lambda: nc.vector.tensor_copy(ki[:], kf[:]), r=[b], w=[b])
        self.V(lambda: nc.vector.tensor_copy(kf[:], ki[:]), r=[b], w=[b])
        self.V(lambda: nc.vector.tensor_scalar(rr[:], ang, shift, None, ALU.add), r=bufs_r + [b], w=[b])
        self.V(lambda: nc.vector.scalar_tensor_tensor(rr[:], kf[:], -TWO_PI, rr[:], ALU.mult, ALU.add), r=[b], w=[b])
        self.V(lambda: nc.vector.tensor_scalar(rr[:], rr[:], -np.pi, np.pi, ALU.max, ALU.min), r=[b], w=[b])
        self.A(lambda: nc.scalar.activation(out=out, in_=rr[:], func=AF.Sin), r=[b], w=[buf_w, b])


def _ph_s5_params(self, i):
    nc = self.nc
    with ExitStack() as es:
        lam = self.sb(es, "lam", [128, 3, 32]); b_lam = Buf()
        t = [self.sb(es, f"s5t{k}", [128, 32]) for k in range(8)]; bt = Buf()
        self.dma(lam[:], self.s5_lam[i], w=[b_lam])
        P = self.s5_par; bP = self.b_s5_par
        step, ar, th, cth, sth, den, nr = t[0], t[1], t[2], t[3], t[4], t[5], t[6]
        self.A(lambda: nc.scalar.activation(out=step[:], in_=lam[:, 2, :], func=AF.Exp), r=[b_lam], w=[bt])
        self.V(lambda: nc.vector.tensor_tensor(ar[:], lam[:, 0, :], step[:], ALU.mult), r=[b_lam, bt], w=[bt])
        self.V(lambda: nc.vector.tensor_tensor(th[:], lam[:, 1, :], step[:], ALU.mult), r=[b_lam, bt], w=[bt])
        self.A(lambda: nc.scalar.activation(out=P[:, 0, :], in_=ar[:], func=AF.Exp), r=[bt], w=[bP])
        _sincos(self, es, P[:, 2, :], P[:, 1, :], th[:], [128, 32], [bt], bP, "a")
        self.V(lambda: nc.vector.tensor_tensor(cth[:], P[:, 0, :], P[:, 1, :], ALU.mult), r=[bP], w=[bt])
        self.V(lambda: nc.vector.tensor_scalar(cth[:], cth[:], -1.0, None, ALU.add), r=[bt], w=[bt])
        self.V(lambda: nc.vector.tensor_tensor(sth[:], P[:, 0, :], P[:, 2, :], ALU.mult), r=[bP], w=[bt])
        self.V(lambda: nc.vector.tensor_tensor(den[:], lam[:, 0, :], lam[:, 0, :], ALU.mult), r=[b_lam], w=[bt])
        self.V(lambda: nc.vector.tensor_tensor(nr[:], lam[:, 1, :], lam[:, 1, :], ALU.mult), r=[b_lam], w=[bt])
        self.V(lambda: nc.vector.tensor_tensor(den[:], den[:], nr[:], ALU.add), r=[bt], w=[bt])
        self.V(lambda: nc.vector.reciprocal(den[:], den[:]), r=[bt], w=[bt])
        a, b2 = t[6], t[7]
        self.V(lambda: nc.vector.tensor_tensor(a[:], cth[:], lam[:, 0, :], ALU.mult), r=[bt, b_lam], w=[bt])
        self.V(lambda: nc.vector.tensor_tensor(b2[:], sth[:], lam[:, 1, :], ALU.mult), r=[bt, b_lam], w=[bt])
        self.V(lambda: nc.vector.tensor_tensor(a[:], a[:], b2[:], ALU.add), r=[bt], w=[bt])
        self.V(lambda: nc.vector.tensor_tensor(P[:, 3, :], a[:], den[:], ALU.mult), r=[bt], w=[bP])
        self.V(lambda: nc.vector.tensor_tensor(a[:], sth[:], lam[:, 0, :], ALU.mult), r=[bt, b_lam], w=[bt])
        self.V(lambda: nc.vector.tensor_tensor(b2[:], cth[:], lam[:, 1, :], ALU.mult), r=[bt, b_lam], w=[bt])
        self.V(lambda: nc.vector.tensor_tensor(a[:], a[:], b2[:], ALU.subtract), r=[bt], w=[bt])
        self.V(lambda: nc.vector.tensor_tensor(P[:, 4, :], a[:], den[:], ALU.mult), r=[bt], w=[bP])
        self.V(lambda: nc.vector.tensor_scalar(P[:, 5, :], P[:, 4, :], -1.0, None, ALU.mult), r=[bP], w=[bP])
        ang = self.sb(es, "angW", [128, 32, 12]); b_ang = Buf()
        for k in range(12):
            self.V(lambda: nc.vector.tensor_scalar(ang[:, :, k], th[:], float(2 ** k), None, ALU.mult), r=[bt], w=[b_ang])
        _sincos(self, es, self.s5_W[:, 1], self.s5_W[:, 0], ang[:], [128, 32, 12], [b_ang], self.b_s5_W, "w")
    self.fw.barrier()


def _ph_s5(self, i, kind):
    nc = self.nc
    sts = [self.streams[(kind, s)] for s in range(self.n_samp)]
    L = sts[0].L
    TBS = min(1024, L); NTS = L // TBS
    TB = min(512, L)
    NLV = int(np.log2(L))
    P = self.s5_par; bP = self.b_s5_par
    perm = (kind == "x")
    with ExitStack() as es:
        TC = self.sb(es, "TC", [128, L]); TS = self.sb(es, "TS", [128, L]); b_T = Buf()
        tmpT = self.sb(es, "tmpT", [128, 2, L // 2]); b_tmpT = Buf(); b_tmpT2 = Buf()
        u = [self.sb(es, f"u{s}", [128, L]) for s in range(len(sts))]; b_u = [Buf() for _ in sts]
        yacc = [self.sb(es, f"yacc{s}", [128, L]) for s in range(len(sts))]; b_yacc = [Buf() for _ in sts]
        Bt = self.sb(es, "Bt", [128, 2, 2, 128]); b_Bt = Buf()
        Cf = self.sb(es, "Cf", [128, 2, 128]); b_Cf = Buf()
        Cb = self.sb(es, "Cb", [128, 2, 128], BF16); b_Cb = Buf()
        ctmp = self.sb(es, "ctmp", [128, 128]); b_ctmp = Buf()
        W4s = [[self.sb(es, f"W4_{s_}_{k}", [128, TBS]) for k in range(4)] for s_ in range(len(sts))]
        b_W4s = [[Buf() for _ in range(4)] for _ in sts]
        sb16s = [self.sb(es, f"sb16_{s_}", [128, 2, TBS], BF16) for s_ in range(len(sts))]; b_sb16s = [Buf() for _ in sts]
        cars = [self.sb(es, f"car{s_}", [128, 4]) for s_ in range(len(sts))]; b_cars = [Buf() for _ in sts]
        inis = [self.sb(es, f"ini{s_}", [128, 4]) for s_ in range(len(sts))]; b_inis = [Buf() for _ in sts]
        pcnt = {"npv": 0, "npy": 0}
        dcol = self.sb(es, "dcol", [128, 4]); b_dcol = Buf()
        ybf = self.sb(es, "ybf", [128, L], BF16); b_ybf = Buf()
        pv = [self.ps(es, f"pv{k}", [128, 512]) for k in range(4)]; b_pv = [Buf() for _ in range(4)]
        py = [self.ps(es, f"py{k}", [128, 512]) for k in range(2)]; b_py = [Buf() for _ in range(2)]
        self.dma(dcol[:], self.s5_dT[i], w=[b_dcol])
        for chunk in range(4):
            for s, st in enumerate(sts):
                self.dma(u[s][:], st.PJ[C_U + chunk * 128:C_U + (chunk + 1) * 128, :], r=[self.pjbuf(st, C_U + chunk * 128)], w=[b_u[s]])
            for gl4 in range(4):
                gp = chunk * 4 + gl4
                pr = slice(gl4 * 32, gl4 * 32 + 32)
                for d in range(2):
                    self.dma(Bt[:, d], self.s5_B[i, d, gp], w=[b_Bt])
                for d in range(2):
                    dg = d * 16 + gp
                    self.dma(Cf[:], self.s5_C[i, d, gp], w=[b_Cf])
                    cre, cim, ncim = P[:, 3, dg:dg + 1], P[:, 4, dg:dg + 1], P[:, 5, dg:dg + 1]
                    self.V(lambda: nc.vector.tensor_scalar(ctmp[:], Cf[:, 1, :], cim, None, ALU.mult), r=[b_Cf, bP], w=[b_ctmp])
                    self.V(lambda: nc.vector.scalar_tensor_tensor(Cb[:, 0, :], Cf[:, 0, :], cre, ctmp[:], ALU.mult, ALU.subtract),
                           r=[b_Cf, bP, b_ctmp], w=[b_Cb])
                    self.V(lambda: nc.vector.tensor_scalar(ctmp[:], Cf[:, 1, :], cre, None, ALU.mult), r=[b_Cf, bP], w=[b_ctmp])
                    self.V(lambda: nc.vector.scalar_tensor_tensor(Cb[:, 1, :], Cf[:, 0, :], ncim, ctmp[:], ALU.mult, ALU.subtract),
                           r=[b_Cf, bP, b_ctmp], w=[b_Cb])
                    self.V(lambda: nc.vector.memset(TC[:, 0:1], 1.0), w=[b_T])
                    self.V(lambda: nc.vector.memset(TS[:, 0:1], 0.0), w=[b_T])
                    for k in range(NLV):
                        n = 2 ** k
                        wr = self.s5_W[:, 0, dg, k:k + 1]; wi = self.s5_W[:, 1, dg, k:k + 1]
                        rW = [b_T, self.b_s5_W]
                        self.A(lambda: nc.scalar.activation(out=tmpT[:, 0, 0:n], in_=TS[:, 0:n], func=AF.Copy, scale=wi), r=rW, w=[b_tmpT])
                        self.A(lambda: nc.scalar.activation(out=tmpT[:, 1, 0:n], in_=TC[:, 0:n], func=AF.Copy, scale=wi), r=rW, w=[b_tmpT2])
                        self.V(lambda: nc.vector.scalar_tensor_tensor(TC[:, n:2 * n], TC[:, 0:n], wr, tmpT[:, 0, 0:n], ALU.mult, ALU.subtract),
                               r=rW + [b_tmpT], w=[b_T])
                        self.V(lambda: nc.vector.scalar_tensor_tensor(TS[:, n:2 * n], TS[:, 0:n], wr, tmpT[:, 1, 0:n], ALU.mult, ALU.add),
                               r=rW + [b_tmpT2], w=[b_T])
                    rho_b = P[:, 0, dg:dg + 1].to_broadcast([128, TBS])
                    def chain(s, st, d=d, dg=dg, gp=gp, pr=pr, rho_b=rho_b):
                        W4 = W4s[s]; b_W4 = b_W4s[s]; sb16 = sb16s[s]; b_sb16 = b_sb16s[s]
                        car = cars[s]; b_car = b_cars[s]; ini = inis[s]; b_ini = b_inis[s]
                        if kind == "x":
                            hre = self.s5_st[:, 0, dg, s:s + 1]; him = self.s5_st[:, 1, dg, s:s + 1]
                            c1 = P[:, 1, dg:dg + 1]; s1 = P[:, 2, dg:dg + 1]
                            self.V(lambda: nc.vector.tensor_tensor(ini[:, 2:3], him, s1, ALU.mult), r=[self.b_s5_st, bP], w=[b_ini])
                            self.V(lambda: nc.vector.scalar_tensor_tensor(ini[:, 0:1], hre, c1, ini[:, 2:3], ALU.mult, ALU.subtract),
                                   r=[self.b_s5_st, bP, b_ini], w=[b_ini])
                            self.V(lambda: nc.vector.tensor_tensor(ini[:, 2:3], hre, s1, ALU.mult), r=[self.b_s5_st, bP], w=[b_ini])
                            self.V(lambda: nc.vector.scalar_tensor_tensor(ini[:, 1:2], him, c1, ini[:, 2:3], ALU.mult, ALU.add),
                                   r=[self.b_s5_st, bP, b_ini], w=[b_ini])
                        tbs_order = range(NTS) if d == 0 else range(NTS - 1, -1, -1)
                        for ti, tb in enumerate(tbs_order):
                            n0 = tb * TBS
                            if d == 0:
                                tc = TC[:, n0:n0 + TBS]; ts = TS[:, n0:n0 + TBS]
                            else:
                                o = L - 1 - n0
                                tc = bass.AP(TC, o, [[TC[:].ap[0][0], 128], [-1, TBS]])
                                ts = bass.AP(TS, o, [[TS[:].ap[0][0], 128], [-1, TBS]])
                            Vre, Vim, Abuf, Bbuf = W4
                            bVre, bVim, bA, bB = b_W4
                            for sb_ in range(TBS // TB):
                                c0 = n0 + sb_ * TB
                                if perm:
                                    w0 = c0 // 64
                                    rhs = bass.AP(u[s], w0, [[u[s][:].ap[0][0], 128], [1, TB // 64], [64, 64]])
                                else:
                                    rhs = u[s][:, c0:c0 + TB]
                                for ri, (dst, bdst) in enumerate(((Vre, bVre), (Vim, bVim))):
                                    k = pcnt['npv'] % 4; pcnt['npv'] += 1
                                    self.T(lambda: nc.tensor.matmul(pv[k][:, :TB], Bt[:, d, ri, :], rhs, start=True, stop=True),
                                           r=[b_Bt, b_u[s]], w=[b_pv[k]])
                                    self.A(lambda: nc.scalar.copy(dst[:, sb_ * TB:(sb_ + 1) * TB], pv[k][:, :TB]), r=[b_pv[k]], w=[bdst])
                            yield
                            self.G(lambda: nc.gpsimd.tensor_tensor(Bbuf[:], Vim[:], ts, ALU.mult), r=[bVim, b_T], w=[bB])
                            self.V(lambda: nc.vector.tensor_tensor(Abuf[:], Vre[:], tc, ALU.mult), r=[bVre, b_T], w=[bA])
                            self.G(lambda: nc.gpsimd.tensor_tensor(Vre[:], Vre[:], ts, ALU.mult), r=[bVre, b_T], w=[bVre])
                            self.V(lambda: nc.vector.tensor_tensor(Abuf[:], Abuf[:], Bbuf[:], ALU.add), r=[bA, bB], w=[bA])
                            self.V(lambda: nc.vector.tensor_tensor(Bbuf[:], Vim[:], tc, ALU.mult), r=[bVim, b_T], w=[bB])
                            self.V(lambda: nc.vector.tensor_tensor(Bbuf[:], Bbuf[:], Vre[:], ALU.subtract), r=[bB, bVre], w=[bB])
                            yield
                            if ti == 0:
                                if kind == "x":
                                    i_re, i_im, rd = ini[:, 0:1], ini[:, 1:2], [b_ini]
                                else:
                                    i_re, i_im, rd = 0.0, 0.0, []
                            else:
                                i_re, i_im, rd = car[:, 0:1], car[:, 1:2], [b_car]
                            if d == 0:
                                rv = lambda t_: t_[:]
                                last = lambda t_: t_[:, TBS - 1:TBS]
                            else:
                                rv = lambda t_: bass.AP(t_, TBS - 1, [[t_[:].ap[0][0], 128], [-1, TBS]])
                                last = lambda t_: t_[:, 0:1]
                            self.V(lambda: nc.vector.tensor_tensor_scan(rv(Vim), rho_b, rv(Abuf), i_re, ALU.mult, ALU.add), r=[bA, bP] + rd, w=[bVim])
                            self.V(lambda: nc.vector.tensor_tensor_scan(rv(Vre), rho_b, rv(Bbuf), i_im, ALU.mult, ALU.add), r=[bB, bP] + rd, w=[bVre])
                            self.A(lambda: nc.scalar.copy(car[:, 0:1], last(Vim)), r=[bVim], w=[b_car])
                            self.A(lambda: nc.scalar.copy(car[:, 1:2], last(Vre)), r=[bVre], w=[b_car])
                            qre, qim, bqre, bqim = Vim, Vre, bVim, bVre
                            yield
                            self.G(lambda: nc.gpsimd.tensor_tensor(Abuf[:], qim[:], ts, ALU.mult), r=[bqim, b_T], w=[bA])
                            self.V(lambda: nc.vector.tensor_tensor(Bbuf[:], qre[:], tc, ALU.mult), r=[bqre, b_T], w=[bB])
                            self.V(lambda: nc.vector.tensor_tensor(sb16[:, 0, :], Bbuf[:], Abuf[:], ALU.subtract), r=[bA, bB], w=[b_sb16])
                            self.G(lambda: nc.gpsimd.tensor_tensor(Bbuf[:], qim[:], tc, ALU.mult), r=[bqim, b_T], w=[bB])
                            self.V(lambda: nc.vector.tensor_tensor(Abuf[:], qre[:], ts, ALU.mult), r=[bqre, b_T], w=[bA])
                            self.V(lambda: nc.vector.tensor_tensor(sb16[:, 1, :], Abuf[:], Bbuf[:], ALU.add), r=[bA, bB], w=[b_sb16])
                            if kind == "c" and ti == NTS - 1:
                                e = (TBS - 1) if d == 0 else 0
                                tce = TC[:, L - 1:L]; tse = TS[:, L - 1:L]
                                qr_e = qre[:, e:e + 1]; qi_e = qim[:, e:e + 1]
                                self.V(lambda: nc.vector.tensor_tensor(car[:, 2:3], qi_e, tse, ALU.mult), r=[bqim, b_T], w=[b_car])
                                self.V(lambda: nc.vector.scalar_tensor_tensor(self.s5_st[:, 0, dg, s:s + 1], qr_e, tce, car[:, 2:3], ALU.mult, ALU.subtract),
                                       r=[bqre, b_T, b_car], w=[self.b_s5_st])
                                self.V(lambda: nc.vector.tensor_tensor(car[:, 2:3], qr_e, tse, ALU.mult), r=[bqre, b_T], w=[b_car])
                                self.V(lambda: nc.vector.scalar_tensor_tensor(self.s5_st[:, 1, dg, s:s + 1], qi_e, tce, car[:, 2:3], ALU.mult, ALU.add),
                                       r=[bqim, b_T, b_car], w=[self.b_s5_st])
                            yield
                            for sb_ in range(TBS // TB):
                                c0 = n0 + sb_ * TB
                                k = pcnt['npy'] % 2; pcnt['npy'] += 1
                                self.T(lambda: nc.tensor.matmul(py[k][:, :TB], Cb[:, 0, :], sb16[:, 0, sb_ * TB:(sb_ + 1) * TB], start=True, stop=False),
                                       r=[b_Cb, b_sb16], w=[b_py[k]])
                                self.T(lambda: nc.tensor.matmul(py[k][:, :TB], Cb[:, 1, :], sb16[:, 1, sb_ * TB:(sb_ + 1) * TB], start=False, stop=True),
                                       r=[b_Cb, b_sb16], w=[b_py[k]])
                                if d == 0:
                                    self.A(lambda: nc.scalar.copy(yacc[s][pr, c0:c0 + TB], py[k][pr, :TB]), r=[b_py[k]], w=[b_yacc[s]])
                                else:
                                    self.V(lambda: nc.vector.tensor_tensor(yacc[s][pr, c0:c0 + TB], yacc[s][pr, c0:c0 + TB], py[k][pr, :TB], ALU.add),
                                           r=[b_py[k], b_yacc[s]], w=[b_yacc[s]])
                    gens = [chain(s_, st_) for s_, st_ in enumerate(sts)]
                    while gens:
                        for g_ in list(gens):
                            try:
                                next(g_)
                            except StopIteration:
                                gens.remove(g_)
            for s, st in enumerate(sts):
                ya = yacc[s]; uu = u[s]
                if perm:
                    ps_ = uu[:].ap[0][0]
                    nat = lambda t_: bass.AP(t_, 0, [[ps_, 128], [1, 64], [64, 64]])
                    seq = lambda t_: bass.AP(t_, 0, [[ps_, 128], [64, 64], [1, 64]])
                    self.V(lambda: nc.vector.scalar_tensor_tensor(seq(uu), seq(uu), dcol[:, chunk:chunk + 1], nat(ya), ALU.mult, ALU.add),
                           r=[b_u[s], b_yacc[s], b_dcol], w=[b_u[s]])
                else:
                    self.V(lambda: nc.vector.scalar_tensor_tensor(uu[:], uu[:], dcol[:, chunk:chunk + 1], ya[:], ALU.mult, ALU.add),
                           r=[b_u[s], b_yacc[s], b_dcol], w=[b_u[s]])
                self.G(lambda: nc.gpsimd.tensor_tensor(ya[:], uu[:], uu[:], ALU.mult), r=[b_u[s]], w=[b_yacc[s]])
                self.G(lambda: nc.gpsimd.tensor_scalar(ya[:], ya[:], 0.044715, 1.0, ALU.mult, ALU.add), r=[b_yacc[s]], w=[b_yacc[s]])
                self.G(lambda: nc.gpsimd.tensor_tensor(ya[:], ya[:], uu[:], ALU.mult), r=[b_yacc[s], b_u[s]], w=[b_yacc[s]])
                self.A(lambda: nc.scalar.activation(out=ya[:], in_=ya[:], func=AF.Sigmoid, scale=GELU_K), r=[b_yacc[s]], w=[b_yacc[s]])
                self.V(lambda: nc.vector.tensor_tensor(ybf[:], uu[:], ya[:], ALU.mult), r=[b_yacc[s], b_u[s]], w=[b_ybf])
                self.dma(st.ys5[chunk * 128:(chunk + 1) * 128, :], ybf[:], r=[b_ybf], w=[st.b_ys5[chunk]])
    self.fw.barrier()


KB.s5_setup = _s5_setup
KB.ph_s5_params = _ph_s5_params
KB.ph_s5 = _ph_s5


def prep_s5(inp):
    o = {}
    lam = np.zeros((4, 128, 3, 32), np.float32)
    Bm = np.zeros((4, 2, 16, 128, 2, 128), np.float32)
    Cm = np.zeros((4, 2, 16, 128, 2, 128), np.float32)
    lre, lim, lst = (np.asarray(inp[k], np.float32) for k in ("s5_lam_re", "s5_lam_im", "s5_log_step"))
    bre, bim, cre, cim = (np.asarray(inp[k], np.float32) for k in ("s5_b_re", "s5_b_im", "s5_c_re", "s5_c_im"))
    for d in range(2):
        for gp in range(16):
            dg = d * 16 + gp
            chunk, gl4 = gp // 4, gp % 4
            for gl in range(2):
                g = 2 * gp + gl
                rows = slice(gl * 64, gl * 64 + 64)
                lam[:, rows, 0, dg] = lre[:, d, g, :]
                lam[:, rows, 1, dg] = lim[:, d, g, :]
                lam[:, rows, 2, dg] = lst[:, d, g][:, None]
                r0 = gl4 * 32 + gl * 16
                Bm[:, d, gp, r0:r0 + 16, 0, rows] = bre[:, d, g].transpose(0, 2, 1)
                Bm[:, d, gp, r0:r0 + 16, 1, rows] = bim[:, d, g].transpose(0, 2, 1)
                Cm[:, d, gp, rows, 0, r0:r0 + 16] = cre[:, d, g].transpose(0, 2, 1)
                Cm[:, d, gp, rows, 1, r0:r0 + 16] = cim[:, d, g].transpose(0, 2, 1)
    o["s5_lam"] = lam; o["s5_B"] = Bm; o["s5_C"] = Cm
    o["s5_dT"] = np.stack([colform(inp["s5_d"][i], 4) for i in range(4)])
    return o


def _ssd_setup(self):
    es = self.ges
    S = self.n_samp
    self.m2_cw = self.inp("m2_cw", [4, 128, 12, 4])
    self.m2_cb = self.inp("m2_cb", [4, 128, 12])
    self.m2_row = self.inp("m2_row", [4, 1, 64])
    self.m2_drow = self.inp("m2_drow", [4, 1, 2048])
    self.ssd_const = self.inp("ssd_const", [128, 4, 128])
    self.ssd_c = self.sb(es, "ssd_c", [128, 4, 128]); self.b_ssd_c = Buf("ssd_c")
    self.dma(self.ssd_c[:], self.ssd_const, w=[self.b_ssd_c])
    for st in self.streams.values():
        L = st.L
        st.xbc_tm = self.scratch(f"xbctm_{st.kind}{st.s}", [L, 1280])
        st.b_xbc_tm = [Buf() for _ in range(10)]
        st.bc_fm = self.scratch(f"bcfm_{st.kind}{st.s}", [512, L])
        st.b_bc_fm = [Buf() for _ in range(4)]
        st.z_tm = self.scratch(f"ztm_{st.kind}{st.s}", [L, 1024])
        st.b_z_tm = [Buf() for _ in range(st.NT)]
        st.dt_tm = self.scratch(f"dttm_{st.kind}{st.s}", [L, 32])
        st.b_dt_tm = Buf()
        st.yf = self.scratch(f"yf_{st.kind}{st.s}", [L, 1024])
        st.b_yf = [Buf() for _ in range(st.NT)]


def _ph_ssd(self, i, st):
    nc = self.nc
    L, NT = st.L, st.NT
    s = st.s
    C = self.ssd_c
    triU, triL, mbF, mbB = C[:, 0, :], C[:, 1, :], C[:, 2, :], C[:, 3, :]
    with ExitStack() as es:
        cw = self.sb(es, "m2cw", [128, 12, 4]); cb = self.sb(es, "m2cb", [128, 12]); b_cp = Buf()
        self.dma(cw[:], self.m2_cw[i], w=[b_cp]); self.dma(cb[:], self.m2_cb[i], w=[b_cp])
        xp = [self.sb(es, f"xp{k}", [128, L + 3]) for k in range(2)]; b_xp = [Buf() for _ in range(2)]
        acc = self.sb(es, "cacc", [128, L]); b_acc = Buf()
        act = [self.sb(es, f"cact{k}", [128, L]) for k in range(2)]; b_act = [Buf() for _ in range(2)]
        tms = self.sb(es, "tms", [128, NT, 128]); b_tms = Buf()
        pt = [self.ps(es, f"ptt{k}", [128, 4, 128]) for k in range(2)]; b_pt = [Buf() for _ in range(2)]
        for k in range(2):
            self.V(lambda: nc.vector.memset(xp[k][:, 0:2], 0.0), w=[b_xp[k]])
            self.V(lambda: nc.vector.memset(xp[k][:, L + 2:L + 3], 0.0), w=[b_xp[k]])
        npt = 0
        for ch in range(12):
            k = ch % 2
            r0 = C_XBC + ch * 128
            self.dma(xp[k][:, 2:L + 2], st.PJ[r0:r0 + 128, :], r=[self.pjbuf(st, r0)], w=[b_xp[k]])
            self.V(lambda: nc.vector.tensor_scalar(acc[:], xp[k][:, 0:L], cw[:, ch, 0:1], cb[:, ch:ch + 1], ALU.mult, ALU.add),
                   r=[b_xp[k], b_cp], w=[b_acc])
            for j in range(1, 4):
                self.V(lambda: nc.vector.scalar_tensor_tensor(acc[:], xp[k][:, j:j + L], cw[:, ch, j:j + 1], acc[:], ALU.mult, ALU.add),
                       r=[b_xp[k], b_cp, b_acc], w=[b_acc])
            self.A(lambda: nc.scalar.activation(out=act[k][:], in_=acc[:], func=AF.Silu), r=[b_acc], w=[b_act[k]])
            if ch >= 8:
                self.dma(st.bc_fm[(ch - 8) * 128:(ch - 7) * 128, :], act[k][:], r=[b_act[k]], w=[st.b_bc_fm[ch - 8]])
            if ch < 10:
                for t0 in range(0, NT, 4):
                    pk = npt % 2; npt += 1
                    nt4 = min(4, NT - t0)
                    for q in range(nt4):
                        t = t0 + q
                        self.T(lambda: nc.tensor.matmul(pt[pk][:, q, :], act[k][:, t * 128:(t + 1) * 128], self.ident_f[:], start=True, stop=True),
                               r=[b_act[k], self.b_const], w=[b_pt[pk]])
                    if npt % 2 == 0:
                        self.A(lambda: nc.scalar.copy(tms[:, t0:t0 + nt4, :], pt[pk][:, 0:nt4, :]), r=[b_pt[pk]], w=[b_tms])
                    else:
                        self.G(lambda: nc.gpsimd.tensor_copy(tms[:, t0:t0 + nt4, :], pt[pk][:, 0:nt4, :]), r=[b_pt[pk]], w=[b_tms]) if False else \
                            self.V(lambda: nc.vector.tensor_copy(tms[:, t0:t0 + nt4, :], pt[pk][:, 0:nt4, :]), r=[b_pt[pk]], w=[b_tms])
                self.dma(st.xbc_tm[:, ch * 128:(ch + 1) * 128].rearrange("(t p) c -> p t c", p=128), tms[:], r=[b_tms], w=[st.b_xbc_tm[ch]])
    self.fw.barrier()
    with ExitStack() as es:
        rowp = self.sb(es, "rowp", [128, 64]); b_rowp = Buf()
        drow = self.sb(es, "drow", [128, 2048]); b_drow = Buf()
        self.dma(rowp[:], self.m2_row[i].partition_broadcast(128), w=[b_rowp])
        self.dma(drow[:], self.m2_drow[i].partition_broadcast(128), w=[b_drow])
        self.A(lambda: nc.scalar.activation(out=rowp[:, 32:64], in_=rowp[:, 32:64], func=AF.Exp), r=[b_rowp], w=[b_rowp])
        self.V(lambda: nc.vector.tensor_scalar(rowp[:, 32:64], rowp[:, 32:64], -1.0, None, ALU.mult), r=[b_rowp], w=[b_rowp])
        DT = self.sb(es, "DT", [128, NT, 32]); b_DT = Buf()
        LA = self.sb(es, "LA", [128, NT, 32]); b_LA = Buf()
        self.dma(DT[:], st.dt_tm.rearrange("(t p) c -> p t c", p=128), r=[st.b_dt_tm], w=[b_DT])
        self.V(lambda: nc.vector.tensor_tensor(DT[:], DT[:], rowp[:, 0:32].unsqueeze(1).to_broadcast([128, NT, 32]), ALU.add), r=[b_DT, b_rowp], w=[b_DT])
        self.A(lambda: nc.scalar.activation(out=DT[:], in_=DT[:], func=AF.Exp), r=[b_DT], w=[b_DT])
        self.A(lambda: nc.scalar.activation(out=DT[:], in_=DT[:], func=AF.Ln, bias=1.0), r=[b_DT], w=[b_DT])
        self.V(lambda: nc.vector.tensor_tensor(LA[:], DT[:], rowp[:, 32:64].unsqueeze(1).to_broadcast([128, NT, 32]), ALU.mult), r=[b_DT, b_rowp], w=[b_LA])
        XT = [self.sb(es, f"XT{k}", [128, 1280]) for k in range(2)]; b_XT = [Buf() for _ in range(2)]
        BCf = [self.sb(es, f"BCf{k}", [64, 8, 128]) for k in range(2)]; b_BCf = [Buf() for _ in range(2)]
        acs = self.sb(es, "acs", [128, 48]); b_acs = Buf()
        din = self.sb(es, "din", [128, 16]); b_din = Buf()
        dch = self.sb(es, "dch", [64, 16]); b_dch = Buf()
        CBs = self.sb(es, "CBs", [128, 4, 128]); b_CBs = Buf()
        arg = self.sb(es, "arg", [128, 8, 128]); b_arg = Buf()
        MT = self.sb(es, "MT", [128, 8, 128], BF16); b_MT = Buf()
        Bd = self.sb(es, "Bd", [128, 16, 64], BF16); b_Bd = Buf()
        xs = self.sb(es, "xs", [128, 1024], BF16); b_xs = Buf()
        yt = self.sb(es, "yt", [128, 1024]); b_yt = Buf()
        yfl = self.sb(es, "yfl", [128, 1024]); b_yfl = Buf()
        zt = self.sb(es, "zt", [128, 1024]); b_zt = Buf()
        ybf = self.sb(es, "ybf16", [128, 1024], BF16); b_ybf = Buf()
        ssq = self.sb(es, "ssqm", [128, 2]); b_ssq = Buf()
        ystg = self.sb(es, "ystg", [128, 8, 512], BF16); b_ystg = Buf()
        etmp = self.sb(es, "etmp", [64, 512]); b_etmp = Buf()
        pc = self.ps(es, "pc", [128, 32]); b_pc = Buf()
        cbp = self.ps(es, "cbp", [128, 4, 128]); b_cbp = Buf()
        abc = self.ps(es, "abc", [128, 8, 128]); b_abc = Buf()
        yd = self.ps(es, "yd", [128, 512]); b_yd = Buf()
        yo = self.ps(es, "yo", [128, 512]); b_yo = Buf()
        stp = self.ps(es, "stp", [64, 512]); b_stp = Buf()
        ptr = self.ps(es, "ptr2", [128, 8, 128], BF16); b_ptr = Buf()
        grp = min(4, NT)
        for d in range(2):
            ent = self.ssd_st[:, d, s, :]
            b_ent = self.b_ssd_st[d][s]
            if st.kind == "c":
                self.V(lambda: nc.vector.memset(ent, 0.0), w=[b_ent])
            tri = triU if d == 0 else triL
            mb = mbF if d == 0 else mbB
            order = range(NT) if d == 0 else range(NT - 1, -1, -1)
            for ci, c in enumerate(order):
                k = ci % 2
                tok = slice(c * 128, (c + 1) * 128)
                self.dma(XT[k][:], st.xbc_tm[tok, :], r=st.b_xbc_tm, w=[b_XT[k]])
                self.dma(BCf[k][:], st.bc_fm[:, tok].rearrange("(k n) t -> n k t", n=64), r=st.b_bc_fm, w=[b_BCf[k]])
                la = LA[:, c, d * 16:(d + 1) * 16]
                dtv = DT[:, c, d * 16:(d + 1) * 16]
                self.T(lambda: nc.tensor.matmul(pc[:, 0:16], tri, la, start=True, stop=True), r=[self.b_ssd_c, b_LA], w=[b_pc])
                self.T(lambda: nc.tensor.matmul(pc[:, 16:32], self.ones_f[:], la, start=True, stop=True), r=[self.b_const, b_LA], w=[b_pc])
                for g in range(4):
                    self.T(lambda: nc.tensor.matmul(cbp[:, g, :], BCf[k][:, g, :], BCf[k][:, 4 + g, :], start=True, stop=True),
                           r=[b_BCf[k]], w=[b_cbp])
                self.V(lambda: nc.vector.tensor_copy(acs[:, 0:32], pc[:, 0:32]), r=[b_pc], w=[b_acs])
                self.A(lambda: nc.scalar.copy(CBs[:], cbp[:]), r=[b_cbp], w=[b_CBs])
                self.V(lambda: nc.vector.tensor_tensor(din[:], acs[:, 16:32], acs[:, 0:16], ALU.subtract), r=[b_acs], w=[b_din])
                self.A(lambda: nc.scalar.activation(out=din[:], in_=din[:], func=AF.Exp), r=[b_din], w=[b_din])
                self.A(lambda: nc.scalar.activation(out=acs[:, 32:48], in_=acs[:, 0:16], func=AF.Exp), r=[b_acs], w=[b_acs])
                self.A(lambda: nc.scalar.activation(out=dch[:], in_=acs[0:64, 16:32], func=AF.Exp), r=[b_acs], w=[b_dch])
                Btm = XT[k][:, 1024:1280].rearrange("p (g n) -> p g n", g=4).unsqueeze(2).to_broadcast([128, 4, 4, 64])
                self.V(lambda: nc.vector.tensor_tensor(Bd[:].rearrange("p (g j) n -> p g j n", g=4), Btm,
                                                       din[:].rearrange("p (g j) -> p g j", g=4).unsqueeze(3).to_broadcast([128, 4, 4, 64]), ALU.mult),
                       r=[b_XT[k], b_din], w=[b_Bd])
                self.G(lambda: nc.gpsimd.tensor_tensor(xs[:].rearrange("p (h c) -> p h c", h=16), XT[k][:, 0:1024].rearrange("p (h c) -> p h c", h=16),
                                                       dtv.unsqueeze(2).to_broadcast([128, 16, 64]), ALU.mult),
                       r=[b_XT[k], b_DT], w=[b_xs])
                for hh in range(2):
                    hs = slice(hh * 8, hh * 8 + 8)
                    for hq in range(8):
                        h = hh * 8 + hq
                        self.T(lambda: nc.tensor.matmul(abc[:, hq, :], LA[:, c, d * 16 + h:d * 16 + h + 1].to_broadcast([128, 128]), tri, start=True, stop=False),
                               r=[b_LA, self.b_ssd_c], w=[b_abc])
                        self.T(lambda: nc.tensor.matmul(abc[:, hq, :], self.ident_f[:], mb, start=False, stop=True),
                               r=[self.b_const, self.b_ssd_c], w=[b_abc])
                    self.V(lambda: nc.vector.tensor_tensor(arg[:], abc[:], acs[:, hh * 8:hh * 8 + 8].unsqueeze(2).to_broadcast([128, 8, 128]), ALU.subtract),
                           r=[b_abc, b_acs], w=[b_arg])
                    self.A(lambda: nc.scalar.activation(out=arg[:], in_=arg[:], func=AF.Exp), r=[b_arg], w=[b_arg])
                    self.G(lambda: nc.gpsimd.tensor_tensor(MT[:].rearrange("p (g j) q -> p g j q", g=2), arg[:].rearrange("p (g j) q -> p g j q", g=2),
                                                           CBs[:, hh * 2:hh * 2 + 2, :].unsqueeze(2).to_broadcast([128, 2, 4, 128]), ALU.mult),
                           r=[b_arg, b_CBs], w=[b_MT])
                    for hq in range(8):
                        h = hh * 8 + hq
                        self.T(lambda: nc.tensor.matmul(yd[:, hq * 64:(hq + 1) * 64], MT[:, hq, :], xs[:, h * 64:(h + 1) * 64], start=True, stop=True),
                               r=[b_MT, b_xs], w=[b_yd])
                    for gq in range(2):
                        g = hh * 2 + gq
                        self.T(lambda: nc.tensor.matmul(yo[:, gq * 256:(gq + 1) * 256], BCf[k][:, 4 + g, :], ent[:, g * 256:(g + 1) * 256], start=True, stop=True),
                               r=[b_BCf[k], b_ent], w=[b_yo])
                    for hq in range(8):
                        h = hh * 8 + hq
                        self.T(lambda: nc.tensor.matmul(stp[:, hq * 64:(hq + 1) * 64], Bd[:, h, :], xs[:, h * 64:(h + 1) * 64], start=True, stop=True),
                               r=[b_Bd, b_xs], w=[b_stp])
                    ysl = yt[:, hh * 512:(hh + 1) * 512]
                    self.V(lambda: nc.vector.tensor_tensor(ysl.rearrange("p (h c) -> p h c", h=8), yo[:].rearrange("p (h c) -> p h c", h=8),
                                                           acs[:, 32 + hh * 8:32 + hh * 8 + 8].unsqueeze(2).to_broadcast([128, 8, 64]), ALU.mult),
                           r=[b_yo, b_acs], w=[b_yt])
                    self.V(lambda: nc.vector.tensor_tensor(ysl, ysl, yd[:], ALU.add), r=[b_yt, b_yd], w=[b_yt])
                    esl = ent[:, hh * 512:(hh + 1) * 512]
                    self.V(lambda: nc.vector.tensor_tensor(etmp[:].rearrange("p (h c) -> p h c", h=8), esl.rearrange("p (h c) -> p h c", h=8),
                                                           dch[:, hs].unsqueeze(2).to_broadcast([64, 8, 64]), ALU.mult),
                           r=[b_ent, b_dch], w=[b_etmp])
                    self.V(lambda: nc.vector.tensor_tensor(esl, etmp[:], stp[:], ALU.add), r=[b_etmp, b_stp], w=[b_ent])
                if d == 0:
                    self.dma(st.yf[tok, :], yt[:], r=[b_yt], w=[st.b_yf[c]], q="act")
                else:
                    self.dma(yfl[:], st.yf[tok, :], r=[st.b_yf[c]], w=[b_yfl])
                    self.dma(zt[:], st.z_tm[tok, :], r=[st.b_z_tm[c]], w=[b_zt])
                    self.G(lambda: nc.gpsimd.tensor_tensor(yfl[:], yfl[:], yt[:], ALU.add), r=[b_yfl, b_yt], w=[b_yfl])
                    self.G(lambda: nc.gpsimd.tensor_tensor(yt[:], XT[k][:, 0:1024], drow[:, 0:1024], ALU.mult), r=[b_XT[k], b_drow], w=[b_yt])
                    self.G(lambda: nc.gpsimd.tensor_tensor(yfl[:], yfl[:], yt[:], ALU.add), r=[b_yfl, b_yt], w=[b_yfl])
                    self.A(lambda: nc.scalar.activation(out=zt[:], in_=zt[:], func=AF.Silu), r=[b_zt], w=[b_zt])
                    self.G(lambda: nc.gpsimd.tensor_tensor(yfl[:], yfl[:], zt[:], ALU.mult), r=[b_yfl, b_zt], w=[b_yfl])
                    self.A(lambda: nc.scalar.activation(out=zt[:], in_=yfl[:], func=AF.Square, accum_out=ssq[:, 0:1]), r=[b_yfl], w=[b_zt, b_ssq])
                    self.A(lambda: nc.scalar.activation(out=ssq[:, 1:2], in_=ssq[:, 0:1], func=AF.Sqrt, scale=1.0 / 1024, bias=1e-6), r=[b_ssq], w=[b_ssq])
                    self.V(lambda: nc.vector.reciprocal(ssq[:, 1:2], ssq[:, 1:2]), r=[b_ssq], w=[b_ssq])
                    self.V(lambda: nc.vector.scalar_tensor_tensor(ybf[:], yfl[:], ssq[:, 1:2], drow[:, 1024:2048], ALU.mult, ALU.mult),
                           r=[b_yfl, b_ssq, b_drow], w=[b_ybf])
                    for kc in range(8):
                        self.T(lambda: nc.tensor.transpose(ptr[:, kc, :], ybf[:, kc * 128:(kc + 1) * 128], self.ident_b[:]),
                               r=[b_ybf, self.b_const], w=[b_ptr])
                    q = c % grp
                    self.A(lambda: nc.scalar.copy(ystg[:, :, q * 128:(q + 1) * 128], ptr[:]), r=[b_ptr], w=[b_ystg])
                    if q == 0:
                        c0 = c * 128
                        self.dma(st.yssd[:, c0:c0 + grp * 128].rearrange("(kc p) t -> p kc t", p=128), ystg[:, :, 0:grp * 128],
                                 r=[b_ystg], w=st.b_yssd, q="act")
    self.fw.barrier()


KB.ssd_setup = _ssd_setup
KB.ph_ssd = _ph_ssd


def prep_ssd(inp):
    o = {}
    cw = np.asarray(inp["m2_conv_w"], np.float32)
    o["m2_cw"] = np.ascontiguousarray(cw.reshape(4, 4, 12, 128).transpose(0, 3, 2, 1))
    o["m2_cb"] = np.ascontiguousarray(np.asarray(inp["m2_conv_b"], np.float32).reshape(4, 12, 128).transpose(0, 2, 1))
    o["m2_row"] = np.ascontiguousarray(np.concatenate([np.asarray(inp["m2_dt_bias"], np.float32).reshape(4, 1, 32),
                                                       np.asarray(inp["m2_a_log"], np.float32).reshape(4, 1, 32)], axis=2))
    drep = np.repeat(np.asarray(inp["m2_d"], np.float32), 64, axis=1)
    o["m2_drow"] = np.ascontiguousarray(np.concatenate([drep, np.asarray(inp["m2_norm_g"], np.float32)], axis=1).reshape(4, 1, 2048))
    q = np.arange(128)
    triU = (q[:, None] <= q[None, :]).astype(np.float32)
    triL = (q[:, None] >= q[None, :]).astype(np.float32)
    mbF = np.where(q[None, :] >= q[:, None], 0.0, -30000.0).astype(np.float32)
    mbB = np.where(q[None, :] <= q[:, None], 0.0, -30000.0).astype(np.float32)
    o["ssd_const"] = np.ascontiguousarray(np.stack([triU, triL, mbF, mbB], axis=1))
    return o


def _m5_setup(self):
    self.s5_wglu = self.inp("s5_w_glu", [4, 512, 2048])
    self.m2_wout = self.inp("m2_w_out", [4, 1024, 1024])
    self.lru_wout = self.inp("lru_w_out", [4, 512, 1024])
    self.w_o = self.inp("w_o", [4, 1024, 1024])
    self.wg_bf = self.scratch("wg_bf", [128, 8, 3072], BF16)
    self.b_wg_bf = Buf("wg_bf")


def _ph_castgates(self, i):
    nc = self.nc
    with ExitStack() as es:
        wf = [self.sb(es, f"cgf{k}", [128, 8, 512]) for k in range(2)]; b_wf = [Buf() for _ in range(2)]
        wb = [self.sb(es, f"cgb{k}", [128, 8, 512], BF16) for k in range(2)]; b_wb = [Buf() for _ in range(2)]
        for n in range(6):
            k = n % 2
            c0 = C_G + n * 512
            self.dma(wf[k][:], self.w_in[i][:, c0:c0 + 512].rearrange("(kc p) n -> p kc n", p=128), w=[b_wf[k]])
            if k == 0:
                self.G(lambda: nc.gpsimd.tensor_copy(wb[k][:], wf[k][:]), r=[b_wf[k]], w=[b_wb[k]])
            else:
                self.A(lambda: nc.scalar.copy(wb[k][:], wf[k][:]), r=[b_wf[k]], w=[b_wb[k]])
            self.dma(self.wg_bf[:, :, n * 512:(n + 1) * 512], wb[k][:], r=[b_wb[k]], w=[self.b_wg_bf])
    self.fw.barrier()


def _load_cast(self, es, dst, b_dst, src_ap, nk, ncols, stg, b_stg, cnt):
    nc = self.nc
    for c0 in range(0, ncols, 512):
        k = cnt[0] % 2; cnt[0] += 1
        self.dma(stg[k][:, :nk, :], src_ap[:, c0:c0 + 512].rearrange("(kc p) n -> p kc n", p=128), w=[b_stg[k]])
        if k == 0:
            self.G(lambda: nc.gpsimd.tensor_copy(dst[:, :nk, c0:c0 + 512], stg[k][:, :nk, :]), r=[b_stg[k]], w=[b_dst])
        else:
            self.A(lambda: nc.scalar.copy(dst[:, :nk, c0:c0 + 512], stg[k][:, :nk, :]), r=[b_stg[k]], w=[b_dst])


def _ph_m5(self, i, kind):
    nc = self.nc
    sts = [self.streams[(kind, s)] for s in range(self.n_samp)]
    L = sts[0].L; TB = sts[0].TB; NB = sts[0].NB
    with ExitStack() as es:
        Wglu = self.sb(es, "Wglu", [128, 4, 2048], BF16); b_Wglu = Buf()
        Wm2 = self.sb(es, "Wm2", [128, 8, 1024], BF16); b_Wm2 = Buf()
        Wlru = self.sb(es, "Wlru", [128, 4, 1024], BF16); b_Wlru = Buf()
        Wo = self.sb(es, "Wo", [128, 8, 1024], BF16); b_Wo = Buf()
        with ExitStack() as es_w:
            stg = [self.sb(es_w, f"m5stg{k}", [128, 8, 512]) for k in range(2)]; b_stg = [Buf() for _ in range(2)]
            cnt = [0]
            _load_cast(self, es_w, Wglu, b_Wglu, self.s5_wglu[i], 4, 2048, stg, b_stg, cnt)
            _load_cast(self, es_w, Wm2, b_Wm2, self.m2_wout[i], 8, 1024, stg, b_stg, cnt)
            _load_cast(self, es_w, Wlru, b_Wlru, self.lru_wout[i], 4, 1024, stg, b_stg, cnt)
            _load_cast(self, es_w, Wo, b_Wo, self.w_o[i], 8, 1024, stg, b_stg, cnt)
            self.fw.barrier()
        Wg = [self.sb(es, f"Wg{k}", [128, 8, 3, 128], BF16) for k in range(2)]; b_Wg = [Buf() for _ in range(2)]
        hTb = self.sb(es, "hTb", [128, 8, TB], BF16); b_hTb = Buf()
        y5b = self.sb(es, "y5b", [128, 4, TB], BF16); b_y5b = Buf()
        ymb = self.sb(es, "ymb", [128, 8, TB], BF16); b_ymb = Buf()
        ylb = self.sb(es, "ylb", [128, 4, TB], BF16); b_ylb = Buf()
        gt = [self.sb(es, f"gt{k}", [128, TB]) for k in range(3)]; b_gt = [Buf() for _ in range(3)]
        sg = self.sb(es, "sgm5", [128, TB]); b_sg = Buf()
        mt = self.sb(es, "mt", [128, TB]); b_mt = Buf()
        tt = self.sb(es, "ttm5", [128, TB]); b_tt = Buf()
        mrg = self.sb(es, "mrg", [128, 8, TB], BF16); b_mrg = Buf()
        xt = [self.sb(es, f"xtm5{k}", [128, D]) for k in range(2)]; b_xt = [Buf() for _ in range(2)]
        tmp = self.sb(es, "tmpm5", [128, 512]); b_tmp = Buf()
        pp = [self.ps(es, f"pp{k}", [128, 512]) for k in range(8)]; b_pp = [Buf() for _ in range(8)]
        npp = [0]

        def nxt():
            k = npp[0] % 8; npp[0] += 1
            return pp[k], b_pp[k]

        ng = 0; nx = 0
        for st in sts:
            src = st.src if st.first else st.res
            gate_row = self.gb[:, st.v, 0, :]
            for tb in range(NB):
                ts_ = slice(tb * TB, (tb + 1) * TB)
                self.dma(hTb[:], st.hTs[:, :, ts_], r=[st.b_hTs], w=[b_hTb])
                self.dma(y5b[:], st.ys5[:, ts_].rearrange("(kc p) t -> p kc t", p=128), r=st.b_ys5, w=[b_y5b])
                self.dma(ymb[:], st.yssd[:, ts_].rearrange("(kc p) t -> p kc t", p=128), r=st.b_yssd, w=[b_ymb])
                self.dma(ylb[:], st.ylru[:, ts_].rearrange("(kc p) t -> p kc t", p=128), r=st.b_ylru, w=[b_ylb])
                for nch in range(8):
                    kg = ng % 2; ng += 1
                    ncs = slice(nch * 128, (nch + 1) * 128)
                    self.dma(Wg[kg][:], self.wg_bf.rearrange("p k (j n) -> p k j n", j=3)[:, :, :, ncs], r=[self.b_wg_bf], w=[b_Wg[kg]])
                    for j in range(3):
                        p_, bp_ = nxt()
                        for kc in range(8):
                            self.T(lambda: nc.tensor.matmul(p_[:, :TB], Wg[kg][:, kc, j, :], hTb[:, kc, :], start=(kc == 0), stop=(kc == 7)),
                                   r=[b_Wg[kg], b_hTb], w=[bp_])
                        self.A(lambda: nc.scalar.activation(out=gt[j][:], in_=p_[:, :TB], func=AF.Sigmoid), r=[bp_], w=[b_gt[j]])
                    pv_, bpv = nxt(); pg_, bpg = nxt()
                    for kc in range(4):
                        self.T(lambda: nc.tensor.matmul(pv_[:, :TB], Wglu[:, kc, ncs], y5b[:, kc, :], start=(kc == 0), stop=(kc == 3)),
                               r=[b_Wglu, b_y5b], w=[bpv])
                    for kc in range(4):
                        self.T(lambda: nc.tensor.matmul(pg_[:, :TB], Wglu[:, kc, 1024 + nch * 128:1024 + (nch + 1) * 128], y5b[:, kc, :],
                                                        start=(kc == 0), stop=(kc == 3)),
                               r=[b_Wglu, b_y5b], w=[bpg])
                    self.A(lambda: nc.scalar.activation(out=sg[:], in_=pg_[:, :TB], func=AF.Sigmoid), r=[bpg], w=[b_sg])
                    self.V(lambda: nc.vector.tensor_tensor(mt[:], pv_[:, :TB], sg[:], ALU.mult), r=[bpv, b_sg], w=[b_mt])
                    self.G(lambda: nc.gpsimd.tensor_tensor(mt[:], mt[:], gt[0][:], ALU.mult), r=[b_mt, b_gt[0]], w=[b_mt])
                    pb_, bpb = nxt()
                    for kc in range(8):
                        self.T(lambda: nc.tensor.matmul(pb_[:, :TB], Wm2[:, kc, ncs], ymb[:, kc, :], start=(kc == 0), stop=(kc == 7)),
                               r=[b_Wm2, b_ymb], w=[bpb])
                    self.V(lambda: nc.vector.tensor_tensor(tt[:], pb_[:, :TB], gt[1][:], ALU.mult), r=[bpb, b_gt[1]], w=[b_tt])
                    self.G(lambda: nc.gpsimd.tensor_tensor(mt[:], mt[:], tt[:], ALU.add), r=[b_mt, b_tt], w=[b_mt])
                    pc_, bpc = nxt()
                    for kc in range(4):
                        self.T(lambda: nc.tensor.matmul(pc_[:, :TB], Wlru[:, kc, ncs], ylb[:, kc, :], start=(kc == 0), stop=(kc == 3)),
                               r=[b_Wlru, b_ylb], w=[bpc])
                    self.V(lambda: nc.vector.tensor_tensor(tt[:], pc_[:, :TB], gt[2][:], ALU.mult), r=[bpc, b_gt[2]], w=[b_tt])
                    self.G(lambda: nc.gpsimd.tensor_tensor(mrg[:, nch, :], mt[:], tt[:], ALU.add), r=[b_mt, b_tt], w=[b_mrg])
                for q in range(TB // 128):
                    t = tb * (TB // 128) + q
                    kx = nx % 2; nx += 1
                    self.dma(xt[kx][:], src[t * 128:(t + 1) * 128, :], r=[st.b_res[t]], w=[b_xt[kx]])
                    for nb in range(2):
                        po_, bpo = nxt()
                        for kc in range(8):
                            self.T(lambda: nc.tensor.matmul(po_[:], mrg[:, kc, q * 128:(q + 1) * 128], Wo[:, kc, nb * 512:(nb + 1) * 512],
                                                            start=(kc == 0), stop=(kc == 7)),
                                   r=[b_mrg, b_Wo], w=[bpo])
                        self.V(lambda: nc.vector.tensor_tensor(tmp[:], po_[:], gate_row[:, nb * 512:(nb + 1) * 512], ALU.mult), r=[bpo, self.b_gb], w=[b_tmp])
                        self.G(lambda: nc.gpsimd.tensor_tensor(xt[kx][:, nb * 512:(nb + 1) * 512], xt[kx][:, nb * 512:(nb + 1) * 512], tmp[:], ALU.add),
                               r=[b_xt[kx], b_tmp], w=[b_xt[kx]])
                    self.dma(st.res[t * 128:(t + 1) * 128, :], xt[kx][:], r=[b_xt[kx]], w=[st.b_res[t]])
            st.first = False
    self.fw.barrier()


KB.m5_setup = _m5_setup
KB.ph_castgates = _ph_castgates
KB.ph_m5 = _ph_m5


def _moe_setup(self):
    self.w_router = self.inp("w_routerT", [4, 128, 8, 16])
    self.moe_w1 = self.inp("moe_w1", [4, 16, 1024, 1024])
    self.moe_w3 = self.inp("moe_w3", [4, 16, 1024, 1024])
    self.moe_w2 = self.inp("moe_w2", [4, 16, 1024, 1024])
    self.iota_in = self.inp("iota_c", [128, 4])
    self.reg_bc = {256: self.nc.gpsimd.to_reg(255), 4096: self.nc.gpsimd.to_reg(4095)}
    for st in self.streams.values():
        L = st.L
        st.h2 = self.scratch(f"h2_{st.kind}{st.s}", [L, 1024], BF16)
        st.b_h2 = Buf()
        st.affd = self.scratch(f"affd_{st.kind}{st.s}", [L, 16])
        st.b_affd = Buf()
        st.cum = self.scratch(f"cum_{st.kind}{st.s}", [16, L])
        st.b_cum = Buf()


def _rowbcast(self, es, dst, b_dst, col8, rbufs, pbank, b_pbank, dgs, b_dgs):
    nc = self.nc
    for half in range(2):
        for q in range(4):
            kc = half * 4 + q
            d = q % 2
            self.G(lambda: nc.gpsimd.tensor_scalar(dgs[d][:], self.ident_f[:], col8[:, kc:kc + 1], None, ALU.mult),
                   r=[self.b_const] + rbufs, w=[b_dgs[d]])
            self.T(lambda: nc.tensor.matmul(pbank[:, q * 128:(q + 1) * 128], self.ones_f[:], dgs[d][:], start=True, stop=True),
                   r=[self.b_const, b_dgs[d]], w=[b_pbank])
        self.A(lambda: nc.scalar.copy(dst[:, half * 512:(half + 1) * 512], pbank[:]), r=[b_pbank], w=[b_dst])


def _ph_moe(self, i, kind):
    nc = self.nc
    sts = [self.streams[(kind, s)] for s in range(self.n_samp)]
    L = sts[0].L; NT = sts[0].NT
    KCAP = L // 8
    NCT = max(1, KCAP // 128)
    with ExitStack() as es0:
        affT = self.sb(es0, "affT", [64, L]); b_affT = Buf()
        self.V(lambda: nc.vector.memset(affT[:], 0.0), w=[b_affT])
        with ExitStack() as es:
            Wr = self.sb(es, "Wr", [128, 8, 16]); b_Wr = Buf()
            self.dma(Wr[:], self.w_router[i], w=[b_Wr])
            scrow = self.sb(es, "scrow", [128, D]); shrow = self.sb(es, "shrow", [128, D]); b_rows = Buf()
            dgs = [self.sb(es, f"dgs{k}", [128, 128]) for k in range(2)]; b_dgs = [Buf() for _ in range(2)]
            pbk = self.ps(es, "pbk", [128, 512]); b_pbk = Buf()
            xt = [self.sb(es, f"xte{k}", [128, D]) for k in range(2)]; b_xt = [Buf() for _ in range(2)]
            h2b = [self.sb(es, f"h2b{k}", [128, D], BF16) for k in range(2)]; b_h2b = [Buf() for _ in range(2)]
            junk = self.sb(es, "junke", [128, D]); b_junk = Buf()
            ssq = [self.sb(es, f"ssqe{k}", [128, 2]) for k in range(2)]; b_ssq = [Buf() for _ in range(2)]
            h2T = self.sb(es, "h2T", [128, 8, 128]); b_h2T = Buf()
            ptr = [self.ps(es, f"ptre{k}", [128, 4, 128]) for k in range(2)]; b_ptr = [Buf() for _ in range(2)]
            plg = self.ps(es, "plg", [128, 16]); b_plg = Buf()
            paf = self.ps(es, "paf", [64, 512]); b_paf = Buf()
            AFF = self.sb(es, "AFF", [128, NT, 16]); b_AFF = Buf()
            sm = self.sb(es, "sm", [128, 4]); b_sm = Buf()
            ex = self.sb(es, "ex", [128, 16]); b_ex = Buf()
            for s, st in enumerate(sts):
                _rowbcast(self, es, scrow, b_rows, self.nsc[:, st.v, 2, :], [self.b_nsc], pbk, b_pbk, dgs, b_dgs)
                _rowbcast(self, es, shrow, b_rows, self.nsc[:, st.v, 3, :], [self.b_nsc], pbk, b_pbk, dgs, b_dgs)
                for t in range(NT):
                    k = t % 2
                    self.dma(xt[k][:], st.res[t * 128:(t + 1) * 128, :], r=[st.b_res[t]], w=[b_xt[k]])
                    self.A(lambda: nc.scalar.activation(out=junk[:], in_=xt[k][:], func=AF.Square, accum_out=ssq[k][:, 0:1]),
                           r=[b_xt[k]], w=[b_junk, b_ssq[k]])
                    self.A(lambda: nc.scalar.activation(out=ssq[k][:, 1:2], in_=ssq[k][:, 0:1], func=AF.Sqrt, scale=1.0 / D, bias=1e-6),
                           r=[b_ssq[k]], w=[b_ssq[k]])
                    self.V(lambda: nc.vector.reciprocal(ssq[k][:, 1:2], ssq[k][:, 1:2]), r=[b_ssq[k]], w=[b_ssq[k]])
                    self.V(lambda: nc.vector.scalar_tensor_tensor(xt[k][:], xt[k][:], ssq[k][:, 1:2], scrow[:], ALU.mult, ALU.mult),
                           r=[b_xt[k], b_ssq[k], b_rows], w=[b_xt[k]])
                    self.G(lambda: nc.gpsimd.tensor_tensor(xt[k][:], xt[k][:], shrow[:], ALU.add), r=[b_xt[k], b_rows], w=[b_xt[k]])
                    self.A(lambda: nc.scalar.copy(h2b[k][:], xt[k][:]), r=[b_xt[k]], w=[b_h2b[k]])
                    self.dma(st.h2[t * 128:(t + 1) * 128, :], h2b[k][:], r=[b_h2b[k]], w=[st.b_h2])
                    for half in range(2):
                        for q in range(4):
                            kc = half * 4 + q
                            self.T(lambda: nc.tensor.matmul(ptr[half][:, q, :], xt[k][:, kc * 128:(kc + 1) * 128], self.ident_f[:], start=True, stop=True),
                                   r=[b_xt[k], self.b_const], w=[b_ptr[half]])
                        if half == 0:
                            self.V(lambda: nc.vector.tensor_copy(h2T[:, 0:4, :], ptr[0][:]), r=[b_ptr[0]], w=[b_h2T])
                        else:
                            self.A(lambda: nc.scalar.copy(h2T[:, 4:8, :], ptr[1][:]), r=[b_ptr[1]], w=[b_h2T])
                    for kc in range(8):
                        self.T(lambda: nc.tensor.matmul(plg[:], h2T[:, kc, :], Wr[:, kc, :], start=(kc == 0), stop=(kc == 7)),
                               r=[b_h2T, b_Wr], w=[b_plg])
                    self.V(lambda: nc.vector.reduce_max(sm[:, 0:1], plg[:], axis=AX.X), r=[b_plg], w=[b_sm])
                    self.V(lambda: nc.vector.tensor_scalar(sm[:, 1:2], sm[:, 0:1], -1.0, None, ALU.mult), r=[b_sm], w=[b_sm])
                    self.A(lambda: nc.scalar.activation(out=ex[:], in_=plg[:], func=AF.Exp, bias=sm[:, 1:2], accum_out=sm[:, 2:3]),
                           r=[b_plg, b_sm], w=[b_ex, b_sm])
                    self.V(lambda: nc.vector.reciprocal(sm[:, 3:4], sm[:, 2:3]), r=[b_sm], w=[b_sm])
                    self.V(lambda: nc.vector.tensor_scalar(AFF[:, t, :], ex[:], sm[:, 3:4], None, ALU.mult), r=[b_ex, b_sm], w=[b_AFF])
                self.dma(st.affd.rearrange("(t p) e -> p t e", p=128), AFF[:], r=[b_AFF], w=[st.b_affd])
                TBt = min(4, NT)
                for t0 in range(0, NT, TBt):
                    for q in range(TBt):
                        self.T(lambda: nc.tensor.matmul(paf[s * 32:s * 32 + 16, q * 128:(q + 1) * 128], AFF[:, t0 + q, :], self.ident_f[:], start=True, stop=True),
                               r=[b_AFF, self.b_const], w=[b_paf])
                    self.V(lambda: nc.vector.tensor_copy(affT[s * 32:s * 32 + 16, t0 * 128:(t0 + TBt) * 128], paf[s * 32:s * 32 + 16, 0:TBt * 128]),
                           r=[b_paf], w=[b_affT])
        self.fw.barrier()
        with ExitStack() as es:
            msk = self.sb(es, "msk", [64, L]); b_msk = Buf()
            cum = self.sb(es, "cumt", [64, L]); b_cumt = Buf()
            lh = self.sb(es, "lh", [64, 8]); b_lh = Buf()
            lo, hi, mid, cnt, ge, tmp = (lh[:, j:j + 1] for j in range(6))
            self.V(lambda: nc.vector.memset(lo, 0.0), w=[b_lh])
            self.V(lambda: nc.vector.memset(hi, 1.0), w=[b_lh])
            for it in range(34):
                self.V(lambda: nc.vector.tensor_tensor(mid, lo, hi, ALU.add), r=[b_lh], w=[b_lh])
                self.V(lambda: nc.vector.tensor_scalar(mid, mid, 0.5, None, ALU.mult), r=[b_lh], w=[b_lh])
                self.V(lambda: nc.vector.tensor_scalar(msk[:], affT[:], mid, None, ALU.is_ge), r=[b_affT, b_lh], w=[b_msk])
                self.V(lambda: nc.vector.reduce_sum(cnt, msk[:], axis=AX.X), r=[b_msk], w=[b_lh])
                self.V(lambda: nc.vector.tensor_scalar(ge, cnt, float(KCAP) - 0.5, None, ALU.is_ge), r=[b_lh], w=[b_lh])
                self.V(lambda: nc.vector.tensor_tensor(tmp, mid, lo, ALU.subtract), r=[b_lh], w=[b_lh])
                self.V(lambda: nc.vector.tensor_tensor(tmp, tmp, ge, ALU.mult), r=[b_lh], w=[b_lh])
                self.V(lambda: nc.vector.tensor_tensor(lo, lo, tmp, ALU.add), r=[b_lh], w=[b_lh])
                self.V(lambda: nc.vector.tensor_tensor(tmp, hi, mid, ALU.subtract), r=[b_lh], w=[b_lh])
                self.V(lambda: nc.vector.tensor_tensor(tmp, tmp, ge, ALU.mult), r=[b_lh], w=[b_lh])
                self.V(lambda: nc.vector.tensor_tensor(hi, mid, tmp, ALU.add), r=[b_lh], w=[b_lh])
            self.V(lambda: nc.vector.tensor_scalar(msk[:], affT[:], lo, None, ALU.is_ge), r=[b_affT, b_lh], w=[b_msk])
            self.V(lambda: nc.vector.tensor_tensor_scan(cum[:], self.ones_f[0:64, 0:1].to_broadcast([64, L]), msk[:], 0.0, ALU.mult, ALU.add),
                   r=[b_msk, self.b_const], w=[b_cumt])
            for s, st in enumerate(sts):
                self.dma(st.cum, cum[s * 32:s * 32 + 16, :], r=[b_cumt], w=[st.b_cum])
        self.fw.barrier()
    with ExitStack() as es:
        wst = [self.sb(es, f"wst{k}", [128, 8, 512]) for k in range(2)]; b_wst = [Buf() for _ in range(2)]
        W1 = self.sb(es, "W1", [128, 8, 1024], BF16); b_W1 = Buf()
        W3 = self.sb(es, "W3", [128, 8, 1024], BF16); b_W3 = Buf()
        W2 = self.sb(es, "W2", [128, 8, 1024], BF16); b_W2 = Buf()
        iot = self.sb(es, "iot", [128, 4]); b_iot = Buf()
        self.dma(iot[:], self.iota_in, w=[b_iot])
        cumb = self.sb(es, "cumb", [128, L]); b_cumb = Buf()
        cmpj = self.sb(es, "cmpj", [128, L]); b_cmpj = Buf()
        idxf = self.sb(es, "idxf", [128, 4]); b_idxf = Buf()
        idxi = self.sb(es, "idxi", [128, 4], I32); b_idxi = Buf()
        xg = [self.sb(es, f"xg{k}", [128, D], BF16) for k in range(2)]; b_xg = [Buf() for _ in range(2)]
        wts = self.sb(es, "wts", [128, 4, 16]); b_wts = Buf()
        xsT = self.sb(es, "xsT", [128, 8, NCT * 128], BF16); b_xsT = Buf()
        hidT = self.sb(es, "hidT", [128, 8, NCT * 128], BF16); b_hidT = Buf()
        sgl = self.sb(es, "sgl", [128, NCT * 128]); b_sgl = Buf()
        yo = [self.sb(es, f"yoe{k}", [128, D]) for k in range(2)]; b_yo = [Buf() for _ in range(2)]
        ptx = self.ps(es, "ptx", [128, 8, 128], BF16); b_ptx = Buf()
        pe_ = [self.ps(es, f"pex{k}", [128, 512]) for k in range(6)]; b_pe = [Buf() for _ in range(6)]
        b_scat = [Buf() for _ in sts]
        for k in range(2):
            self.V(lambda: nc.vector.memset(xg[k][:], 0.0), w=[b_xg[k]])
        self.V(lambda: nc.vector.memset(wts[:], 0.0), w=[b_wts])
        CN = NCT * 128
        npe = [0]

        def nxt():
            k = npe[0] % 6; npe[0] += 1
            return pe_[k], b_pe[k]

        cnt = [0]; nxg = 0; nyo = 0
        for e in range(16):
            _load_cast(self, es, W1, b_W1, self.moe_w1[i, e], 8, 1024, wst, b_wst, cnt)
            _load_cast(self, es, W3, b_W3, self.moe_w3[i, e], 8, 1024, wst, b_wst, cnt)
            _load_cast(self, es, W2, b_W2, self.moe_w2[i, e], 8, 1024, wst, b_wst, cnt)
            for s, st in enumerate(sts):
                self.dma(cumb[:], st.cum[e:e + 1, :].partition_broadcast(128), r=[st.b_cum], w=[b_cumb])
                for ct in range(NCT):
                    self.V(lambda: nc.vector.tensor_scalar(cmpj[:], cumb[:], iot[:, ct:ct + 1], None, ALU.is_le), r=[b_cumb, b_iot], w=[b_cmpj])
                    self.V(lambda: nc.vector.reduce_sum(idxf[:, ct:ct + 1], cmpj[:], axis=AX.X), r=[b_cmpj], w=[b_idxf])
                self.V(lambda: nc.vector.tensor_copy(idxi[:, 0:NCT], idxf[:, 0:NCT]), r=[b_idxf], w=[b_idxi])
                for ct in range(NCT):
                    kx = nxg % 2; nxg += 1
                    self.fw.idma(reads=[b_idxi, st.b_h2], writes=[b_xg[kx]],
                                 out=xg[kx][:], out_offset=None, in_=st.h2[:, :],
                                 in_offset=bass.IndirectOffsetOnAxis(ap=idxi[:, ct:ct + 1], axis=0),
                                 bounds_check=self.reg_bc[L], oob_is_err=False)
                    self.fw.idma(reads=[b_idxi, st.b_affd], writes=[b_wts],
                                 out=wts[:, ct, :], out_offset=None, in_=st.affd[:, :],
                                 in_offset=bass.IndirectOffsetOnAxis(ap=idxi[:, ct:ct + 1], axis=0),
                                 bounds_check=self.reg_bc[L], oob_is_err=False)
                    for kc in range(8):
                        self.T(lambda: nc.tensor.transpose(ptx[:, kc, :], xg[kx][:, kc * 128:(kc + 1) * 128], self.ident_b[:]),
                               r=[b_xg[kx], self.b_const], w=[b_ptx])
                    self.A(lambda: nc.scalar.copy(xsT[:, :, ct * 128:(ct + 1) * 128], ptx[:]), r=[b_ptx], w=[b_xsT])
                for fch in range(8):
                    fs = slice(fch * 128, (fch + 1) * 128)
                    p1, bp1 = nxt(); p3, bp3 = nxt()
                    for kc in range(8):
                        self.T(lambda: nc.tensor.matmul(p1[:, :CN], W1[:, kc, fs], xsT[:, kc, :], start=(kc == 0), stop=(kc == 7)),
                               r=[b_W1, b_xsT], w=[bp1])
                    for kc in range(8):
                        self.T(lambda: nc.tensor.matmul(p3[:, :CN], W3[:, kc, fs], xsT[:, kc, :], start=(kc == 0), stop=(kc == 7)),
                               r=[b_W3, b_xsT], w=[bp3])
                    self.A(lambda: nc.scalar.activation(out=sgl[:], in_=p1[:, :CN], func=AF.Silu), r=[bp1], w=[b_sgl])
                    self.V(lambda: nc.vector.tensor_tensor(hidT[:, fch, :], p3[:, :CN], sgl[:], ALU.mult), r=[bp3, b_sgl], w=[b_hidT])
                g2 = self.gb[:, st.v, 1, :]
                for ct in range(NCT):
                    ky = nyo % 2; nyo += 1
                    for nb in range(2):
                        po, bpo = nxt()
                        for fch in range(8):
                            self.T(lambda: nc.tensor.matmul(po[:], hidT[:, fch, ct * 128:(ct + 1) * 128], W2[:, fch, nb * 512:(nb + 1) * 512],
                                                            start=(fch == 0), stop=(fch == 7)),
                                   r=[b_hidT, b_W2], w=[bpo])
                        self.V(lambda: nc.vector.scalar_tensor_tensor(yo[ky][:, nb * 512:(nb + 1) * 512], po[:], wts[:, ct, e:e + 1],
                                                                      g2[:, nb * 512:(nb + 1) * 512], ALU.mult, ALU.mult),
                               r=[bpo, b_wts, self.b_gb], w=[b_yo[ky]])
                    self.fw.idma(reads=[b_idxi, b_yo[ky]], writes=[b_scat[s]],
                                 out=st.res[:, :], out_offset=bass.IndirectOffsetOnAxis(ap=idxi[:, ct:ct + 1], axis=0),
                                 in_=yo[ky][:], in_offset=None, bounds_check=self.reg_bc[L], oob_is_err=False, compute_op=ALU.add)
    self.fw.barrier()


def _ph_final(self, st, out_ap, fng_in):
    nc = self.nc
    with ExitStack() as es:
        grow = self.sb(es, "fgrow", [128, D]); b_grow = Buf()
        self.dma(grow[:], fng_in.partition_broadcast(128), w=[b_grow])
        xt = [self.sb(es, f"xtf{k}", [128, D]) for k in range(3)]; b_xt = [Buf() for _ in range(3)]
        junk = self.sb(es, "junkf", [128, D]); b_junk = Buf()
        ssq = [self.sb(es, f"ssqf{k}", [128, 2]) for k in range(3)]; b_ssq = [Buf() for _ in range(3)]
        for t in range(st.NT):
            k = t % 3
            self.dma(xt[k][:], st.res[t * 128:(t + 1) * 128, :], r=[st.b_res[t]], w=[b_xt[k]])
            self.A(lambda: nc.scalar.activation(out=junk[:], in_=xt[k][:], func=AF.Square, accum_out=ssq[k][:, 0:1]), r=[b_xt[k]], w=[b_junk, b_ssq[k]])
            self.A(lambda: nc.scalar.activation(out=ssq[k][:, 1:2], in_=ssq[k][:, 0:1], func=AF.Sqrt, scale=1.0 / D, bias=1e-6), r=[b_ssq[k]], w=[b_ssq[k]])
            self.V(lambda: nc.vector.reciprocal(ssq[k][:, 1:2], ssq[k][:, 1:2]), r=[b_ssq[k]], w=[b_ssq[k]])
            self.V(lambda: nc.vector.scalar_tensor_tensor(xt[k][:], xt[k][:], ssq[k][:, 1:2], grow[:], ALU.mult, ALU.mult),
                   r=[b_xt[k], b_ssq[k], b_grow], w=[b_xt[k]])
            self.dma(out_ap[t * 128:(t + 1) * 128, :], xt[k][:], r=[b_xt[k]])
    self.fw.barrier()


KB.moe_setup = _moe_setup
KB.ph_moe = _ph_moe
KB.ph_final = _ph_final


def prep_moe(inp):
    o = {}
    wr = np.asarray(inp["moe_w_router"], np.float32)
    o["w_routerT"] = np.ascontiguousarray(wr.reshape(4, 8, 128, 16).transpose(0, 2, 1, 3))
    for k in ("moe_w1", "moe_w3", "moe_w2"):
        o[k] = np.ascontiguousarray(inp[k], np.float32)
    o["iota_c"] = np.ascontiguousarray((np.arange(128)[:, None] + 128 * np.arange(4)[None, :]).astype(np.float32))
    return o


def _layer(self, i, last=False):
    S = self.n_samp
    self.ph_mod(i)
    self.ph_s5_params(i)
    self.ph_castgates(i)
    kinds = ("c", "x")
    for kind in kinds:
        for s in range(S):
            self.ph_norm_proj(i, self.streams[(kind, s)])
    for kind in kinds:
        self.ph_lru(i, kind)
    for kind in kinds:
        self.ph_s5(i, kind)
    with ExitStack() as es_s:
        self.ssd_st = self.sb(es_s, "ssd_st", [64, 2, S, 1024]); self.b_ssd_st = [[Buf() for _ in range(S)] for _ in range(2)]
        for s in range(S):
            for kind in kinds:
                self.ph_ssd(i, self.streams[(kind, s)])
        self.fw.barrier()
    for kind in kinds:
        if kind == "c" and last:
            continue
        self.ph_m5(i, kind)
        self.ph_moe(i, kind)


KB.layer = _layer


def prep_all(inp):
    o = prep_common(inp)
    o.update(prep_s5(inp)); o.update(prep_ssd(inp)); o.update(prep_moe(inp))
    for k in ("s5_w_glu", "m2_w_out", "lru_w_out", "w_o"):
        o[k] = np.ascontiguousarray(inp[k], np.float32)
    o["fng"] = np.ascontiguousarray(np.asarray(inp["final_norm_g"], np.float32).reshape(1, 1024))
    return o


from concourse.bass_utils import run_bass_kernel_spmd

N_CORES = 8
N_SAMP = 2


def build_program():
    nc = bass.Bass("TRN2", target_bir_lowering=False)
    kb = KB(nc, n_samp=N_SAMP, dbg=False)
    kb.setup(); kb.s5_setup(); kb.ssd_setup(); kb.m5_setup(); kb.moe_setup()
    fng = kb.inp("fng", [1, D])
    out = nc.dram_tensor("out", [N_SAMP, 4096, D], F32, kind="ExternalOutput").ap()
    for i in range(4):
        kb.layer(i, last=(i == 3))
    for s in range(N_SAMP):
        kb.ph_final(kb.streams[("x", s)], out[s], fng)
    kb.fw.finish()
    return nc, kb


def kernel(**inputs):
    inp = {k: np.asarray(v) for k, v in inputs.items()}
    nc, kb = build_program()
    common = prep_all(inp)
    in_maps = []
    for core in range(N_CORES):
        m = dict(common)
        m.update(prep_core(inp, core, N_SAMP))
        in_maps.append({k: m[k] for k in kb.din})
    res = run_bass_kernel_spmd(nc, in_maps, core_ids=list(range(N_CORES)))
    outs = [np.asarray(r["out"], dtype=np.float32) for r in res.results]
    return np.concatenate(outs, axis=0)
```
